# Optimizing a Trainium2 kernel written in Bass

```python
import math
import jax, jax.numpy as jnp
from jax import lax
import numpy as np

D_MODEL = 1024
BATCH = 8
SEQ = 4096
DEPTH = 2

DSWA_PATTERNS = ((128, 1), (512, 4), (2048, 16))
N_GROUPS = 3
HEADS_PER_GROUP = 4
N_HEADS_A = N_GROUPS * HEADS_PER_GROUP
HEAD_DIM_A = 128
A_QKV_W = N_HEADS_A * HEAD_DIM_A
A_OUT = HEADS_PER_GROUP * HEAD_DIM_A
NUM_BUCKETS = 32
MAX_DISTANCE = 2048
N_HEADS_B = 8
HEAD_DIM_B = 128
B_W = N_HEADS_B * HEAD_DIM_B
CONV_WIDTH = 4
CHUNK = 64
D_FF = 2816
N_EXPERTS = 8
TOP_K = 2
D_FF_EXPERT = 3584
MOE_BLOCK = 512
N_DENSE = (DEPTH + 1) // 2
N_MOE = DEPTH // 2
ALPHA = (2 * DEPTH) ** 0.25
BETA = (8 * DEPTH) ** -0.25
LN_EPS = 1e-5
RMS_EPS = 1e-6
IN_SIZES = (A_QKV_W, A_QKV_W, A_QKV_W, 3 * B_W, B_W, N_HEADS_B, N_HEADS_B, D_MODEL, D_MODEL)
N_IN = sum(IN_SIZES)
IN_SPLITS = tuple(int(s) for s in np.cumsum(IN_SIZES)[:-1])

kernel_name = "hybrid_dilated_attn_gated_deltanet_moe_deepnorm"


def layer_norm(x, g, b):
    xf = x.astype(jnp.float32)
    mu = jnp.mean(xf, axis=-1, keepdims=True)
    var = jnp.mean(jnp.square(xf - mu), axis=-1, keepdims=True)
    return ((xf - mu) * lax.rsqrt(var + LN_EPS) * g.astype(jnp.float32) + b.astype(jnp.float32)).astype(x.dtype)


def t5_causal_bucket(dist):
    num_exact = NUM_BUCKETS // 2
    d = jnp.maximum(dist, 1).astype(jnp.float32)
    large = num_exact + (jnp.log(d / num_exact) / math.log(MAX_DISTANCE / num_exact)
                         * (NUM_BUCKETS - num_exact)).astype(jnp.int32)
    large = jnp.minimum(large, NUM_BUCKETS - 1)
    return jnp.where(dist < num_exact, dist, large)


def dilated_window_attention(q, k, v, bias_table, window, dilation):
    B, S, H, Dh = q.shape
    blk = window // dilation
    n = S // dilation
    nb = -(-n // blk)
    n_pad = nb * blk

    def to_strided(t):
        t = t.reshape(B, n, dilation, H, Dh).transpose(0, 2, 1, 3, 4)
        t = jnp.pad(t, ((0, 0), (0, 0), (0, n_pad - n), (0, 0), (0, 0)))
        return t.reshape(B, dilation, nb, blk, H, Dh)

    qs, ks, vs = to_strided(q), to_strided(k), to_strided(v)

    def with_prev(t):
        prev = jnp.pad(t, ((0, 0), (0, 0), (1, 0), (0, 0), (0, 0), (0, 0)))[:, :, :-1]
        return jnp.concatenate([prev, t], axis=3)

    kk, vv = with_prev(ks), with_prev(vs)
    qi = jnp.arange(blk)[:, None] + blk
    kj = jnp.arange(2 * blk)[None, :]
    delta = qi - kj
    band = (delta >= 0) & (delta <= blk)
    mask = band[None] & ((jnp.arange(nb)[:, None, None] > 0) | (kj[None] >= blk))
    bias = bias_table[t5_causal_bucket(jnp.maximum(delta, 0) * dilation)]
    bias = bias.transpose(2, 0, 1).astype(jnp.float32)

    s = jnp.einsum('brcqhd,brckhd->brchqk', qs, kk).astype(jnp.float32) * (Dh ** -0.5)
    s = s + bias[None, None, None]
    s = jnp.where(mask[None, None, :, None], s, -jnp.inf)
    lse = jax.nn.logsumexp(s, axis=-1)
    p = jnp.exp(s - lse[..., None])
    o = jnp.einsum('brchqk,brckhd->brcqhd', p.astype(v.dtype), vv)

    o = o.reshape(B, dilation, n_pad, H, Dh)[:, :, :n].transpose(0, 2, 1, 3, 4).reshape(B, S, H, Dh)
    lse = lse.transpose(0, 1, 2, 4, 3).reshape(B, dilation, n_pad, H)[:, :, :n]
    lse = lse.transpose(0, 2, 1, 3).reshape(B, S, H)
    return o, lse


def causal_depthwise_conv(x, w):
    return lax.conv_general_dilated(x, w[:, None, :].astype(x.dtype), window_strides=(1,),
                                    padding=[(CONV_WIDTH - 1, 0)],
                                    dimension_numbers=('NWC', 'WIO', 'NWC'),
                                    feature_group_count=x.shape[-1])


def gated_delta_rule(q, k, v, g, beta):
    B, S, H, Dk = q.shape
    Dv = v.shape[-1]
    nc = S // CHUNK
    f32 = jnp.float32

    def chunks(t):
        t = t.astype(f32).reshape(B, nc, CHUNK, H, *t.shape[3:])
        return jnp.moveaxis(t, 3, 1)

    q, k, v, g, beta = chunks(q), chunks(k), chunks(v), chunks(g), chunks(beta)
    G = jnp.cumsum(g, axis=-1)
    idx = jnp.arange(CHUNK)
    incl = idx[:, None] >= idx[None, :]
    strict = idx[:, None] > idx[None, :]
    decay = jnp.exp(jnp.where(incl, G[..., :, None] - G[..., None, :], -jnp.inf))
    kk = jnp.einsum('bhntd,bhnsd->bhnts', k, k)
    A = jnp.where(strict, beta[..., None] * kk * decay, 0.0)
    eye = jnp.eye(CHUNK, dtype=f32)
    rhs = jnp.concatenate([beta[..., None] * v, (beta * jnp.exp(G))[..., None] * k], axis=-1)
    sol = lax.linalg.triangular_solve(eye + A, rhs, left_side=True, lower=True, unit_diagonal=True)
    u0, w = sol[..., :Dv], sol[..., Dv:]
    qk = jnp.einsum('bhntd,bhnsd->bhnts', q, k) * decay
    q_dec = q * jnp.exp(G)[..., None]
    k_dec = k * jnp.exp(G[..., -1:] - G)[..., None]
    g_last = jnp.exp(G[..., -1])

    def step(state, xs):
        u0_c, w_c, qk_c, qdec_c, kdec_c, gl_c = xs
        u = u0_c - jnp.einsum('bhtk,bhkv->bhtv', w_c, state)
        o = jnp.einsum('bhtk,bhkv->bhtv', qdec_c, state) + jnp.einsum('bhts,bhsv->bhtv', qk_c, u)
        state = gl_c[..., None, None] * state + jnp.einsum('bhsk,bhsv->bhkv', kdec_c, u)
        return state, o

    xs = tuple(jnp.moveaxis(t, 2, 0) for t in (u0, w, qk, q_dec, k_dec, g_last))
    _, o = lax.scan(step, jnp.zeros((B, H, Dk, Dv), f32), xs)
    return o.transpose(1, 0, 3, 2, 4).reshape(B, S, H, Dv)


def l2norm(t):
    return t * lax.rsqrt(jnp.sum(jnp.square(t), axis=-1, keepdims=True) + RMS_EPS)


def hybrid_mixer(x, rel_bias, w_in, conv_w, a_log, dt_bias, o_norm_w, w_oa, w_ob, w_out):
    B, S, _ = x.shape
    f32 = jnp.float32
    proj = x @ w_in
    qa, ka, va, qkv_b, z, b_raw, a_raw, gate_a, gate_b = jnp.split(proj, IN_SPLITS, axis=-1)

    qa = qa.reshape(B, S, N_HEADS_A, HEAD_DIM_A)
    ka = ka.reshape(B, S, N_HEADS_A, HEAD_DIM_A)
    va = va.reshape(B, S, N_HEADS_A, HEAD_DIM_A)
    outs, lses = [], []
    for gi, (window, dilation) in enumerate(DSWA_PATTERNS):
        hs = slice(gi * HEADS_PER_GROUP, (gi + 1) * HEADS_PER_GROUP)
        o, lse = dilated_window_attention(qa[:, :, hs], ka[:, :, hs], va[:, :, hs], rel_bias[:, hs], window, dilation)
        outs.append(o)
        lses.append(lse)
    wgt = jax.nn.softmax(jnp.stack(lses), axis=0)
    y_a = jnp.einsum('gbsh,gbshd->bshd', wgt, jnp.stack(outs).astype(f32))
    y_a = y_a.reshape(B, S, A_OUT).astype(x.dtype)

    qkv_b = jax.nn.silu(causal_depthwise_conv(qkv_b, conv_w))
    qb, kb, vb = jnp.split(qkv_b, 2 * B_W // 2 * np.array([1, 2]) // 2 * 1 if False else (B_W, 2 * B_W), axis=-1)
    qb = l2norm(qb.reshape(B, S, N_HEADS_B, HEAD_DIM_B).astype(f32)) * (HEAD_DIM_B ** -0.5)
    kb = l2norm(kb.reshape(B, S, N_HEADS_B, HEAD_DIM_B).astype(f32))
    vb = vb.reshape(B, S, N_HEADS_B, HEAD_DIM_B)
    beta = jax.nn.sigmoid(b_raw.astype(f32))
    g = -jnp.exp(a_log.astype(f32)) * jax.nn.softplus(a_raw.astype(f32) + dt_bias.astype(f32))
    ob = gated_delta_rule(qb, kb, vb, g, beta)
    ob = ob * lax.rsqrt(jnp.mean(jnp.square(ob), axis=-1, keepdims=True) + RMS_EPS) * o_norm_w.astype(f32)
    ob = ob * jax.nn.silu(z.reshape(B, S, N_HEADS_B, HEAD_DIM_B).astype(f32))
    y_b = ob.reshape(B, S, B_W).astype(x.dtype)

    merged = jax.nn.sigmoid(gate_a) * (y_a @ w_oa) + jax.nn.sigmoid(gate_b) * (y_b @ w_ob)
    return merged @ w_out


def swiglu(x, w_gate, w_up, w_down):
    return (jax.nn.silu(x @ w_gate) * (x @ w_up)) @ w_down


def moe_swiglu(x, w_router, w_gate, w_up, w_down):
    B, S, D = x.shape
    xt = x.reshape(-1, D)
    N = xt.shape[0]
    logits = (xt @ w_router).astype(jnp.float32)
    top_logit, top_idx = lax.top_k(logits, TOP_K)
    gates = jax.nn.softmax(top_logit, axis=-1)
    A = N * TOP_K
    e_flat = top_idx.reshape(-1).astype(jnp.int32)
    tok_flat = jnp.arange(A, dtype=jnp.int32) // TOP_K
    gate_flat = gates.reshape(-1)
    order = jnp.argsort(e_flat)
    e_sorted = e_flat[order]
    counts = jnp.zeros((N_EXPERTS,), jnp.int32).at[e_flat].add(1)
    padded = (counts + MOE_BLOCK - 1) // MOE_BLOCK * MOE_BLOCK
    start = jnp.cumsum(counts) - counts
    pend = jnp.cumsum(padded)
    pstart = pend - padded
    dest = pstart[e_sorted] + (jnp.arange(A, dtype=jnp.int32) - start[e_sorted])
    n_blocks = -(-A // MOE_BLOCK) + N_EXPERTS
    P = n_blocks * MOE_BLOCK
    tok_buf = jnp.zeros((P,), jnp.int32).at[dest].set(tok_flat[order])
    gate_buf = jnp.zeros((P,), jnp.float32).at[dest].set(gate_flat[order])
    block_expert = jnp.minimum(jnp.searchsorted(pend, jnp.arange(n_blocks, dtype=jnp.int32) * MOE_BLOCK,
                                                side='right'), N_EXPERTS - 1)

    def expert_block(args):
        toks, e = args
        xb = xt[toks]
        return (jax.nn.silu(xb @ w_gate[e]) * (xb @ w_up[e])) @ w_down[e]

    yb = lax.map(expert_block, (tok_buf.reshape(n_blocks, MOE_BLOCK), block_expert))
    yb = yb.reshape(P, D) * gate_buf[:, None].astype(x.dtype)
    return jnp.zeros_like(xt).at[tok_buf].add(yb).reshape(B, S, D)


def setup_inputs(seed: int = 0) -> dict:
    key = jax.random.key(seed)
    ks = jax.random.split(key, 24)
    f32 = jnp.float32

    def nrm(k, shape, scale):
        return jax.random.normal(k, shape, f32) * scale

    dt = jnp.exp(jax.random.uniform(ks[5], (DEPTH, N_HEADS_B), f32, math.log(1e-3), math.log(1e-1)))
    return {
        "x": nrm(ks[0], (BATCH, SEQ, D_MODEL), 1.0),
        "rel_bias": nrm(ks[1], (NUM_BUCKETS, N_HEADS_A), 0.2),
        "w_in": nrm(ks[2], (DEPTH, D_MODEL, N_IN), D_MODEL ** -0.5),
        "conv_w": nrm(ks[3], (DEPTH, CONV_WIDTH, 3 * B_W), CONV_WIDTH ** -0.5),
        "a_log": jnp.log(jax.random.uniform(ks[4], (DEPTH, N_HEADS_B), f32, 1.0, 16.0)),
        "dt_bias": dt + jnp.log(-jnp.expm1(-dt)),
        "o_norm_w": 1.0 + nrm(ks[6], (DEPTH, HEAD_DIM_B), 0.1),
        "w_oa": nrm(ks[7], (DEPTH, A_OUT, D_MODEL), BETA * A_OUT ** -0.5),
        "w_ob": nrm(ks[8], (DEPTH, B_W, D_MODEL), BETA * B_W ** -0.5),
        "w_out": nrm(ks[9], (DEPTH, D_MODEL, D_MODEL), BETA * D_MODEL ** -0.5),
        "ln1_g": 1.0 + nrm(ks[10], (DEPTH, D_MODEL), 0.1),
        "ln1_b": nrm(ks[11], (DEPTH, D_MODEL), 0.01),
        "ffn_w_gate": nrm(ks[12], (N_DENSE, D_MODEL, D_FF), D_MODEL ** -0.5),
        "ffn_w_up": nrm(ks[13], (N_DENSE, D_MODEL, D_FF), D_MODEL ** -0.5),
        "ffn_w_down": nrm(ks[14], (N_DENSE, D_FF, D_MODEL), BETA * D_FF ** -0.5),
        "moe_router": nrm(ks[15], (N_MOE, D_MODEL, N_EXPERTS), D_MODEL ** -0.5),
        "moe_w_gate": nrm(ks[16], (N_MOE, N_EXPERTS, D_MODEL, D_FF_EXPERT), D_MODEL ** -0.5),
        "moe_w_up": nrm(ks[17], (N_MOE, N_EXPERTS, D_MODEL, D_FF_EXPERT), D_MODEL ** -0.5),
        "moe_w_down": nrm(ks[18], (N_MOE, N_EXPERTS, D_FF_EXPERT, D_MODEL), BETA * D_FF_EXPERT ** -0.5),
        "ln2_g": 1.0 + nrm(ks[19], (DEPTH, D_MODEL), 0.1),
        "ln2_b": nrm(ks[20], (DEPTH, D_MODEL), 0.01),
    }


def reference(x, rel_bias, w_in, conv_w, a_log, dt_bias, o_norm_w, w_oa, w_ob, w_out, ln1_g, ln1_b,
              ffn_w_gate, ffn_w_up, ffn_w_down, moe_router, moe_w_gate, moe_w_up, moe_w_down, ln2_g, ln2_b):
    for layer in range(DEPTH):
        mix = hybrid_mixer(x, rel_bias, w_in[layer], conv_w[layer], a_log[layer], dt_bias[layer],
                           o_norm_w[layer], w_oa[layer], w_ob[layer], w_out[layer])
        x = layer_norm(ALPHA * x + mix, ln1_g[layer], ln1_b[layer])
        j = layer // 2
        if layer % 2 == 0:
            f = swiglu(x, ffn_w_gate[j], ffn_w_up[j], ffn_w_down[j])
        else:
            f = moe_swiglu(x, moe_router[j], moe_w_gate[j], moe_w_up[j], moe_w_down[j])
        x = layer_norm(ALPHA * x + f, ln2_g[layer], ln2_b[layer])
    return x
```

```python
import numpy as np
from contextlib import ExitStack
import concourse.bass as bass
import concourse.mybir as mybir
from concourse.bass_utils import run_bass_kernel_spmd

F32 = mybir.dt.float32
BF16 = mybir.dt.bfloat16
AF = mybir.ActivationFunctionType
ALU = mybir.AluOpType
AX = mybir.AxisListType

S_LEN = 4096
DM = 1024
NTILE = 32
N_IN = 10768
DEPTH = 2
D_FF = 2816
D_FFE = 3584
NEXP = 8
ALPHA = (2 * DEPTH) ** 0.25
LN_EPS = 1e-5
RMS_EPS = 1e-6
NEG = -30000.0
ENGS = ('pe', 'dve', 'act', 'pool', 'sp')
import os
ATT_LIMIT = int(os.environ.get('ATT_LIMIT', '9'))
DN_LIMIT = int(os.environ.get('DN_LIMIT', '9'))
FFN_LIMIT = int(os.environ.get('FFN_LIMIT', '9'))
SKIP_MIXER = int(os.environ.get('SKIP_MIXER', '0'))


class Sched:
    def __init__(self, nc, ctx):
        self.nc = nc
        self.ctx = ctx
        self.ops = {e: [] for e in ENGS}
        self.cnt = {e: 0 for e in ENGS}
        self.seen = {e: {} for e in ENGS}
        self.res = {}
        self.sems = {}
        for e in ('pe', 'dve', 'act', 'pool'):
            self.sems[e] = ctx.enter_context(nc.semaphore("sem_" + e))
        self.dcnt = {}

    def _sem(self, key):
        if key not in self.sems:
            self.sems[key] = self.ctx.enter_context(self.nc.semaphore("semd_" + str(key)))
            self.dcnt[key] = 0
        return self.sems[key]

    def _deps(self, eng, reads, writes):
        need = {}

        def add(k, v):
            if need.get(k, 0) < v:
                need[k] = v
        for r in reads:
            st = self.res.get(r)
            if st and st['w']:
                add(*st['w'])
        for w in writes:
            st = self.res.get(w)
            if st:
                if st['w'] and st['w'][0] != eng:
                    add(*st['w'])
                for k, v in st['r'].items():
                    if k != eng:
                        add(k, v)
        out = []
        for k, v in need.items():
            if self.seen[eng].get(k, 0) < v:
                self.seen[eng][k] = v
                out.append((k, v))
        return out

    def op(self, eng, fn, reads=(), writes=()):
        psr = [r for r in reads if isinstance(r, tuple) and r[0] == 'ps' and r not in writes]
        if psr:
            writes = list(writes) + psr
        waits = self._deps(eng, reads, writes)
        self.cnt[eng] += 1
        n = self.cnt[eng]
        self.ops[eng].append((waits, fn, eng, 1))
        for r in reads:
            st = self.res.setdefault(r, {'w': None, 'r': {}})
            st['r'][eng] = n
        for w in writes:
            self.res[w] = {'w': (eng, n), 'r': {}}
        return n

    def dma(self, eng, out, in_, key, reads=(), writes=(), **kw):
        self._sem(key)
        waits = self._deps(eng, reads, writes)
        self.dcnt[key] += 16
        n = self.dcnt[key]
        self.ops[eng].append((waits, lambda e: e.dma_start(out=out, in_=in_, **kw), key, 16))
        for r in reads:
            st = self.res.setdefault(r, {'w': None, 'r': {}})
            st['r'][key] = n
        for w in writes:
            self.res[w] = {'w': (key, n), 'r': {}}
        return n

    def dma_fn(self, eng, fn, key, reads=(), writes=()):
        self._sem(key)
        waits = self._deps(eng, reads, writes)
        self.dcnt[key] += 16
        n = self.dcnt[key]
        self.ops[eng].append((waits, fn, key, 16))
        for r in reads:
            st = self.res.setdefault(r, {'w': None, 'r': {}})
            st['r'][key] = n
        for w in writes:
            self.res[w] = {'w': (key, n), 'r': {}}
        return n

    def barrier(self):
        cur = {e: self.cnt[e] for e in ('pe', 'dve', 'act', 'pool')}
        cur.update(self.dcnt)
        for e in ENGS:
            waits = []
            for k, v in cur.items():
                if k != e and v > 0 and self.seen[e].get(k, 0) < v:
                    self.seen[e][k] = v
                    waits.append((k, v))
            if waits:
                self.ops[e].append((waits, None, None, 0))
        self.res = {}

    def emit(self):
        nc = self.nc
        sems = self.sems
        ops = self.ops

        def run(e, lst):
            for waits, fn, sk, inc in lst:
                for k, v in waits:
                    e.wait_ge(sems[k], v)
                if fn is not None:
                    fn(e).then_inc(sems[sk], inc)
        with nc.Block() as block:
            @block.tensor
            def _(e):
                run(e, ops['pe'])

            @block.vector
            def _(e):
                run(e, ops['dve'])

            @block.scalar
            def _(e):
                run(e, ops['act'])

            @block.gpsimd
            def _(e):
                run(e, ops['pool'])

            @block.sync
            def _(e):
                run(e, ops['sp'])


class Arena:
    def __init__(self, t, nwords):
        self.t = t
        self.n = nwords
        self.off = 0

    def alloc(self, nelem, dtype=F32):
        nw = nelem if dtype == F32 else (nelem + 1) // 2
        assert self.off + nw <= self.n, f"arena overflow {self.off}+{nw}>{self.n}"
        ap = self.t[:, self.off:self.off + nw]
        self.off += nw
        return ap if dtype == F32 else ap.bitcast(dtype)

    def mark(self):
        return self.off

    def release(self, m):
        self.off = m


def _w_in_perm():
    cols = []
    for hs in range(4):
        for g in range(3):
            h = g * 4 + hs
            for base in (0, 1536, 3072):
                cols.extend(range(base + h * 128, base + (h + 1) * 128))
    for h in range(8):
        for base in (4608, 5632, 6656, 7680):
            cols.extend(range(base + h * 128, base + (h + 1) * 128))
    cols.extend(range(8704, 8720))
    cols.extend(range(8720, 10768))
    assert len(cols) == N_IN
    return np.asarray(cols)


OFF_DN = 12 * 384
OFF_BA = OFF_DN + 8 * 512
OFF_GA = OFF_BA + 16
OFF_GB = OFF_GA + 1024


def _t5_bucket(dist):
    dist = np.asarray(dist, np.int64)
    d = np.maximum(dist, 1).astype(np.float32)
    large = 16 + (np.log(d / np.float32(16)) / np.float32(np.log(2048 / 16)) * np.float32(16)).astype(np.int32)
    large = np.minimum(large, 31)
    return np.where(dist < 16, dist, large)


def _bias_tables(rel_bias):
    out = np.full((128, 12, 256), NEG, np.float32)
    kj = np.arange(128)[:, None]
    qi = np.arange(128)[None, :]
    for h in range(12):
        d = (1, 4, 16)[h // 4]
        delta0 = qi - kj
        b0 = rel_bias[_t5_bucket(np.maximum(delta0, 0) * d), h]
        out[:, h, 0:128] = np.where(delta0 >= 0, b0, NEG)
        delta1 = qi + 128 - kj
        b1 = rel_bias[_t5_bucket(delta1 * d), h]
        out[:, h, 128:256] = np.where(delta1 <= 128, b1, NEG)
    return out


def _const_mats():
    r = np.arange(128)[:, None]
    c = np.arange(128)[None, :]
    same = (r // 64) == (c // 64)
    m = np.zeros((128, 9, 128), np.float32)
    m[:, 0] = (r == c)
    m[:, 1] = 1.0
    m[:, 2] = (r <= c) & same
    m[:, 3] = (r > c)
    m[:, 4] = np.where((r > c) & same, 0.0, NEG)
    m[:, 5] = np.where((c >= r) & same, 0.0, NEG)
    m[:, 6] = same
    m[:, 7] = (r < 64) * np.ones((1, 128))
    m[:, 8] = (r >= 64) * np.ones((1, 128))
    return m


class Prog:
    def __init__(self, dbg=(), stop_after=None):
        self.dbg = set(dbg)
        self.stop_after = stop_after
        self.nc = nc = bass.Bass("TRN2", target_bir_lowering=False)
        nc.allow_low_precision("bf16 matmul operands with fp32 PSUM accumulation")
        self.ctx = ExitStack()
        with self.ctx:
            self._build()

    def I(self, name):
        if name not in self.ins:
            self.ins[name] = self.nc.dram_tensor(name, list(self.in_shapes[name]), F32, kind="ExternalInput").ap()
        return self.ins[name]

    def mm(self, out, lhsT, rhs, start, stop, reads=(), writes=()):
        self.s.op('pe', lambda e: e.matmul(out, lhsT=lhsT, rhs=rhs, start=start, stop=stop), reads=reads, writes=writes)

    def tr(self, out, in_, reads=(), writes=()):
        ident = self.cm[:, 0, :]
        self.s.op('pe', lambda e: e.transpose(out, in_, ident), reads=reads, writes=writes)

    def copy(self, eng, out, in_, reads=(), writes=()):
        if eng == 'act':
            self.s.op('act', lambda e: e.activation(out=out, in_=in_, func=AF.Copy), reads=reads, writes=writes)
        else:
            self.s.op(eng, lambda e: e.tensor_copy(out=out, in_=in_), reads=reads, writes=writes)

    def act(self, out, in_, func, reads=(), writes=(), **kw):
        self.s.op('act', lambda e: e.activation(out=out, in_=in_, func=func, **kw), reads=reads, writes=writes)

    def tt(self, out, in0, in1, op, reads=(), writes=(), eng='dve'):
        self.s.op(eng, lambda e: e.tensor_tensor(out=out, in0=in0, in1=in1, op=op), reads=reads, writes=writes)

    def ts(self, out, in0, s1, s2, op0, op1=None, reads=(), writes=(), eng='dve', **kw):
        if op1 is None:
            self.s.op(eng, lambda e: e.tensor_scalar(out=out, in0=in0, scalar1=s1, scalar2=None, op0=op0, **kw), reads=reads, writes=writes)
        else:
            self.s.op(eng, lambda e: e.tensor_scalar(out=out, in0=in0, scalar1=s1, scalar2=s2, op0=op0, op1=op1, **kw), reads=reads, writes=writes)

    def stt(self, out, in0, scalar, in1, op0, op1, reads=(), writes=(), eng='dve', **kw):
        self.s.op(eng, lambda e: e.scalar_tensor_tensor(out=out, in0=in0, scalar=scalar, in1=in1, op0=op0, op1=op1, **kw), reads=reads, writes=writes)

    def dump(self, name, ap_sb, shape, dt, key, reads):
        o = self.nc.dram_tensor("dbg_" + name, list(shape), dt, kind="ExternalOutput").ap()
        self.s.dma('sp', o, ap_sb, key='dbg', reads=reads, writes=[('dbgout', name)])
        self.dbg_names.append(name)

    def _build(self):
        nc, ctx = self.nc, self.ctx
        self.s = s = Sched(nc, ctx)
        self.dbg_names = []
        self.in_shapes = {
            "x": [S_LEN, DM], "cmat": [128, 9, 128], "biasT": [128, 12, 256], "w_in_r": [DEPTH, 128, 8, N_IN],
            "conv_r": [DEPTH, 128, 96], "headp": [DEPTH, 128, 16], "onw": [DEPTH, 128, 128],
            "w_oa_r": [DEPTH, 128, 4, DM], "w_ob_r": [DEPTH, 128, 8, DM], "w_out_r": [DEPTH, 128, 8, DM],
            "ln_r": [DEPTH, 4, 128, DM], "ffn_g_r": [D_FF // 128, 128, 8, 128], "ffn_u_r": [D_FF // 128, 128, 8, 128],
            "ffn_d": [D_FF, DM], "moe_router_r": [128, 8, NEXP], "moe_g_r": [NEXP, D_FFE // 128, 128, 8, 128],
            "moe_u_r": [NEXP, D_FFE // 128, 128, 8, 128], "moe_d": [NEXP, D_FFE, DM],
        }
        self.ins = {}
        self.y_out = nc.dram_tensor("y", [S_LEN, DM], F32, kind="ExternalOutput").ap()
        self.x1_d = nc.dram_tensor("x1_scr", [S_LEN, DM], F32).ap()
        self.x2_d = nc.dram_tensor("x2_scr", [S_LEN, DM], F32).ap()
        self.ya_d = nc.dram_tensor("ya_scr", [4, 128, S_LEN], BF16).ap()
        self.yb_d = nc.dram_tensor("yb_scr", [8, 128, S_LEN], BF16).ap()
        NW = 53200
        big = ctx.enter_context(nc.sbuf_tensor("arena", [128, NW], F32))
        self.ar = ar = Arena(big, NW)
        self.ps = [ctx.enter_context(nc.psum_tensor(f"ps{i}", [128, 512], F32)) for i in range(8)]
        self.cm = ar.alloc(9 * 128).rearrange("p (k c) -> p k c", k=9)
        self.ones_bf = ar.alloc(128, BF16)
        s.dma('sp', self.cm, self.I('cmat'), key='c0', writes=['cm'])
        self.copy('dve', self.ones_bf, self.cm[:, 1, :], reads=['cm'], writes=['ones_bf'])
        self.Gt = ar.alloc(32 * NEXP).rearrange("p (i e) -> p i e", e=NEXP)
        self.cst = ar.alloc(4)
        self.m_xT = ar.mark()
        self.xT = ar.alloc(8 * S_LEN, BF16).rearrange("p (k t) -> p k t", k=8)
        for j, v in enumerate((128.0 * RMS_EPS, RMS_EPS, LN_EPS, 0.0)):
            s.op('dve', lambda e, j=j, v=v: e.memset(self.cst[:, j:j + 1], v), writes=['cst'])
        s.barrier()

        self.phase_make_xT(self.I('x'))
        s.barrier()
        if self.stop_after == 'xT':
            self.dump("xT", self.xT[:, 0, :], [128, S_LEN], BF16, 'dbg', [])
            self.finish()
            return
        for l in range(DEPTH):
            if SKIP_MIXER:
                self.phase_ffn(l)
                s.barrier()
                break
            self.phase_attention(l)
            s.barrier()
            if self.stop_after == ('att', l):
                break
            self.phase_deltanet(l)
            s.barrier()
            if ('yb', l) in self.dbg:
                for h in range(8):
                    self.dump(f"yb{l}_{h}", self.yb_d[h], [128, S_LEN], BF16, 'dbg', [])
            if self.stop_after == ('dn', l):
                break
            self.phase_merge(l)
            s.barrier()
            if ('x1', l) in self.dbg:
                self.dump(f"x1_{l}", self.x1_d, [S_LEN, DM], F32, 'dbg', [])
            if self.stop_after == ('mix', l):
                break
            self.phase_ffn(l)
            s.barrier()
            if ('x2', l) in self.dbg:
                self.dump(f"x2_{l}", self.x2_d if l == 0 else self.y_out, [S_LEN, DM], F32, 'dbg', [])
            if self.stop_after == ('ffn', l):
                break
        self.finish()

    def finish(self):
        s = self.s
        s.barrier()
        s.emit()

    def transposes_to_xT(self, src, src_key, i, bank0, eng_pair=('act', 'dve'), xf32=None):
        for half in range(2):
            b = bank0 + half
            for q in range(4):
                kc = half * 4 + q
                self.tr(self.ps[b][:, q * 128:(q + 1) * 128], src[:, kc * 128:(kc + 1) * 128],
                        reads=[src_key], writes=[('ps', b)])
            self.copy(eng_pair[half], self.xT[:, half * 4:(half + 1) * 4, i * 128:(i + 1) * 128],
                      self.ps[b][:, :].rearrange("p (k t) -> p k t", k=4),
                      reads=[('ps', b)], writes=[('xT', i)])
            if xf32 is not None:
                self.copy(eng_pair[1 - half], xf32[0][:, half * 4:(half + 1) * 4, :],
                          self.ps[b][:, :].rearrange("p (k t) -> p k t", k=4),
                          reads=[('ps', b)], writes=[xf32[1]])

    def phase_make_xT(self, src):
        s, ar = self.s, self.ar
        m = ar.mark()
        xl = [ar.alloc(DM) for _ in range(2)]
        for i in range(NTILE):
            sl = i % 2
            s.dma('sp', xl[sl], src[i * 128:(i + 1) * 128, :], key=f'xl{sl}', writes=[('xl', sl)])
            self.transposes_to_xT(xl[sl], ('xl', sl), i, bank0=(i % 2) * 2)
        ar.release(m)

    def phase_attention(self, l):
        s, ar, ps, xT = self.s, self.ar, self.ps, self.xT
        m = ar.mark()
        biasT = ar.alloc(12 * 256).rearrange("p (h c) -> p h c", h=12)
        wu = [ar.alloc(8 * 384, BF16).rearrange("p (k c) -> p k c", k=8) for _ in range(2)]
        qT = ar.alloc(S_LEN, BF16)
        kT = ar.alloc(S_LEN, BF16)
        V = ar.alloc(32 * 128, BF16).rearrange("p (b e) -> p b e", b=32)
        OD = ar.alloc(2 * S_LEN).rearrange("p (two t) -> p two t", two=2)
        PT = [ar.alloc(256, BF16) for _ in range(4)]
        tmp = [ar.alloc(256) for _ in range(2)]
        yst = ar.alloc(S_LEN, BF16)
        s.dma('sp', biasT, self.I('biasT'), key='c0', writes=['biasT'])
        scale = 128.0 ** -0.5
        for hs in range(4):
            for g in range(3):
                d = (1, 4, 16)[g]
                nb = 32 // d
                h = g * 4 + hs
                u = hs * 3 + g
                sl = u % 2
                s.dma('pool', wu[sl], self.I('w_in_r')[l][:, :, u * 384:(u + 1) * 384], key=f'wu{sl}', writes=[('wu', sl)])
                for tt in range(8):
                    for X in range(2):
                        bank = X
                        for kc in range(8):
                            self.mm(ps[bank][:, :], wu[sl][:, kc, X * 128:(X + 1) * 128], xT[:, kc, tt * 512:(tt + 1) * 512],
                                    kc == 0, kc == 7, reads=[('wu', sl)], writes=[('ps', bank)])
                        dst = (qT if X == 0 else kT).rearrange("p (r i) -> p r i", r=d)[:, :, (512 // d) * tt:(512 // d) * (tt + 1)]
                        src = ps[bank][:, :].rearrange("p (j r) -> p r j", r=d)
                        self.copy('act' if X == 0 else 'dve', dst, src, reads=[('ps', bank)], writes=['qT' if X == 0 else 'kT'])
                if ATT_LIMIT < 1:
                    continue
                for b4 in range(8):
                    bank = 2 + b4 % 2
                    for bb in range(4):
                        b = b4 * 4 + bb
                        r, c = divmod(b, nb)
                        t0 = 128 * c * d + r
                        for kc in range(8):
                            self.mm(ps[bank][:, bb * 128:(bb + 1) * 128], xT[:, kc, t0:t0 + 127 * d + 1:d], wu[sl][:, kc, 256:384],
                                    kc == 0, kc == 7, reads=[('wu', sl)], writes=[('ps', bank)])
                    self.copy('act' if b4 % 2 == 0 else 'dve', V[:, b4 * 4:(b4 + 1) * 4, :],
                              ps[bank][:, :].rearrange("p (b e) -> p b e", b=4), reads=[('ps', bank)], writes=[('V', b4)])
                if ATT_LIMIT < 2:
                    continue
                ODv = OD.rearrange("p two (i r) -> p two i r", r=d)

                def s_stage(b):
                    r, c = divmod(b, nb)
                    nq = 256 if c + 1 < nb else 128
                    bank = 4 + b % 2
                    sps = ps[bank][:, 0:nq]
                    self.mm(sps, kT[:, b * 128:(b + 1) * 128], qT[:, b * 128:b * 128 + nq], True, True,
                            reads=['qT', 'kT'], writes=[('ps', bank)])
                    tsl = b % 2
                    self.stt(tmp[tsl][:, :nq], sps, scale, biasT[:, h, :nq], ALU.mult, ALU.add,
                             reads=[('ps', bank), 'biasT'], writes=[('tmp', tsl)])
                    self.act(PT[b % 4][:, :nq], tmp[tsl][:, :nq], AF.Exp, reads=[('tmp', tsl)], writes=[('PT', b % 4)])

                def pv_stage(b):
                    r, c = divmod(b, nb)
                    bank = 6 + b % 2
                    ops_ = ps[bank][:, 0:256]
                    for which in range(2):
                        o_ = ops_[:, which * 128:(which + 1) * 128]
                        if c > 0:
                            lh = V[:, b - 1, :] if which == 0 else self.ones_bf
                            self.mm(o_, lh, PT[(b - 1) % 4][:, 128:256], True, False,
                                    reads=[('V', (b - 1) // 4), ('PT', (b - 1) % 4)], writes=[('ps', bank)])
                        lh = V[:, b, :] if which == 0 else self.ones_bf
                        self.mm(o_, lh, PT[b % 4][:, 0:128], c == 0, True,
                                reads=[('V', b // 4), ('PT', b % 4)], writes=[('ps', bank)])
                    view = ODv[:, :, 128 * c:128 * c + 128, r]
                    if g == 0:
                        regs = [('OD', b // 4)]
                    elif g == 1:
                        regs = [('OD', c)]
                    else:
                        regs = [('OD', 4 * c + j) for j in range(4)]
                    src = ops_.rearrange("p (two t) -> p two t", two=2)
                    if g == 0:
                        self.copy('act', view, src, reads=[('ps', bank)], writes=regs)
                    else:
                        self.tt(view, src, view, ALU.add, reads=[('ps', bank)] + regs, writes=regs)

                s_stage(0)
                for b in range(32):
                    if b + 1 < 32:
                        s_stage(b + 1)
                    pv_stage(b)
            if ATT_LIMIT < 3:
                self.dump(f"q{hs}", qT, [128, S_LEN], BF16, 'dbg', ['qT'])
                self.dump(f"v{hs}", V.rearrange("p b e -> p (b e)"), [128, S_LEN], BF16, 'dbg', [('V', i) for i in range(8)])
                continue
            for rg in range(8):
                cs = slice(rg * 512, (rg + 1) * 512)
                self.s.op('dve', lambda e, cs=cs: e.reciprocal(out=OD[:, 1, cs], in_=OD[:, 1, cs]), reads=[('OD', rg)], writes=[('OD', rg)])
                self.tt(yst[:, cs], OD[:, 0, cs], OD[:, 1, cs], ALU.mult, reads=[('OD', rg)], writes=['yst'])
            s.dma('sp', self.ya_d[hs], yst, key='yst', reads=['yst'], writes=[('ya_d', hs)])
            if ('ya', l) in self.dbg:
                self.dump(f"ya{l}_{hs}", yst, [128, S_LEN], BF16, 'dbg', ['yst'])
        ar.release(m)


    def phase_deltanet(self, l):
        import itertools
        s, ar, ps, xT, cm = self.s, self.ar, self.ps, self.xT, self.cm
        m = ar.mark()
        ident, ones, U, Ls, MS, MIT, BD, H0, H1 = (cm[:, k, :] for k in range(9))
        cw = ar.alloc(96)
        hp = ar.alloc(16)
        onw = ar.alloc(128)
        negA = ar.alloc(8)
        s.dma('sp', cw, self.I('conv_r')[l], key='c0', writes=['cw'])
        s.dma('sp', hp, self.I('headp')[l], key='c0', writes=['hp'])
        s.dma('sp', onw, self.I('onw')[l], key='c0', writes=['onw'])
        self.ts(onw, onw, 128.0 ** 0.5, None, ALU.mult, reads=['onw'], writes=['onw'])
        self.act(negA, hp[:, 0:8], AF.Exp, reads=['hp'], writes=['negA'])
        self.ts(negA, negA, -1.0, None, ALU.mult, reads=['negA'], writes=['negA'])
        wba = ar.alloc(8 * 16, BF16).rearrange("p (k c) -> p k c", k=8)
        s.dma('pool', wba, self.I('w_in_r')[l][:, :, OFF_BA:OFF_BA + 16], key='wba', writes=['wba'])
        a8 = lambda: ar.alloc(256).rearrange("p (i h) -> p i h", h=8)
        BETA, NB, Gs, EG, ED, BEG = a8(), a8(), a8(), a8(), a8(), a8()
        GLB = ar.alloc(512).rearrange("p (c h) -> p c h", h=8)
        m_small = ar.mark()
        GG = ar.alloc(512).rearrange("p (i c) -> p i c", c=16)
        ba = ar.alloc(512).rearrange("p (i c) -> p i c", c=16)
        for i in range(32):
            for kc in range(8):
                self.mm(ps[0][:, i * 16:(i + 1) * 16], xT[:, kc, i * 128:(i + 1) * 128], wba[:, kc, :], kc == 0, kc == 7,
                        reads=['wba'], writes=[('ps', 0)])
        self.copy('dve', ba, ps[0][:, :].rearrange("p (i c) -> p i c", c=16), reads=[('ps', 0)], writes=['ba'])
        bc = lambda v: v.unsqueeze(1).to_broadcast([128, 32, 8])
        self.act(BETA, ba[:, :, 0:8], AF.Sigmoid, reads=['ba'], writes=['BETA'])
        self.ts(NB, BETA, -1.0, None, ALU.mult, reads=['BETA'], writes=['NB'])
        self.tt(Gs, ba[:, :, 8:16], bc(hp[:, 8:16]), ALU.add, reads=['ba', 'hp'], writes=['Gs'])
        self.act(Gs, Gs, AF.Exp, reads=['Gs'], writes=['Gs'])
        self.act(Gs, Gs, AF.Ln, bias=1.0, reads=['Gs'], writes=['Gs'])
        self.tt(Gs, Gs, bc(negA), ALU.mult, reads=['Gs', 'negA'], writes=['Gs'])
        for i in range(32):
            self.mm(ps[1][:, i * 16:i * 16 + 8], U, Gs[:, i, :], True, True, reads=['Gs'], writes=[('ps', 1)])
            self.mm(ps[1][:, i * 16 + 8:i * 16 + 16], BD, Gs[:, i, :], True, True, reads=['Gs'], writes=[('ps', 1)])
            self.mm(ps[2][:, (2 * i) * 8:(2 * i) * 8 + 8], H0, Gs[:, i, :], True, True, reads=['Gs'], writes=[('ps', 2)])
            self.mm(ps[2][:, (2 * i + 1) * 8:(2 * i + 1) * 8 + 8], H1, Gs[:, i, :], True, True, reads=['Gs'], writes=[('ps', 2)])
        self.copy('dve', GG, ps[1][:, :].rearrange("p (i c) -> p i c", c=16), reads=[('ps', 1)], writes=['GG'])
        self.act(GLB, ps[2][:, :].rearrange("p (c h) -> p c h", h=8), AF.Exp, reads=[('ps', 2)], writes=['GLB'])
        self.act(EG, GG[:, :, 0:8], AF.Exp, reads=['GG'], writes=['EG'])
        self.tt(ED, GG[:, :, 8:16], GG[:, :, 0:8], ALU.subtract, reads=['GG'], writes=['ED'])
        self.act(ED, ED, AF.Exp, reads=['ED'], writes=['ED'])
        self.tt(BEG, BETA, EG, ALU.mult, reads=['BETA', 'EG'], writes=['BEG'])

        s.barrier()
        ar.release(m_small)
        bankrot = itertools.cycle(range(2, 8))
        w_in_r = self.I('w_in_r')
        evrot = itertools.cycle(('act', 'dve'))
        m4 = lambda: [ar.alloc(128) for _ in range(4)]

        def make_bufs(hs):
            B = dict(hs=hs)
            B['wu'] = ar.alloc(8 * 512, BF16).rearrange("p (k c) -> p k c", k=8)
            B['S'] = ar.alloc(128)
            B['pre'] = [ar.alloc(3 + 512) for _ in range(3)]
            B['hist'] = [ar.alloc(4) for _ in range(3)]
            B['cv'] = [ar.alloc(512) for _ in range(3)]
            B['sets'] = [dict(u0=m4(), wT=m4(), qkD=m4(), Kdec=m4(), QTn=ar.alloc(512),
                              zs=ar.alloc(512).rearrange("p (a e) -> p a e", a=4), idx=k) for k in range(2)]
            for nm in ('gU', 'Ds', 'DTi', 'Za', 'Ya', 'TTa', 'Kbg', 'Vb'):
                B[nm] = m4()
            B['ub'], B['osb'], B['onb'] = ar.alloc(128), ar.alloc(128), ar.alloc(128)
            B['ssq'], B['rn1'] = ar.alloc(1), ar.alloc(1)
            B['ybst'] = [ar.alloc(512, BF16)] * 2
            return B

        def stage(mms, evs):
            b = next(bankrot)
            for pp in range(4):
                mms(pp, ps[b][:, pp * 128:(pp + 1) * 128], ('ps', b))
            for pp in range(4):
                evs(pp, ps[b][:, pp * 128:(pp + 1) * 128], ('ps', b))

        def prep(h, sg, st, B):
            hs = B['hs']
            K = lambda *a: (hs,) + a
            t0 = sg * 512
            k_ = st['idx']
            W = B['wu']
            wk = K('wu')
            pre, cv, hist = B['pre'], B['cv'], B['hist']
            gU, Ds, DTi, Kbg, Vb = B['gU'], B['Ds'], B['DTi'], B['Kbg'], B['Vb']
            for X in range(3):
                bank = X % 2
                for kc in range(8):
                    self.mm(ps[bank][:, :], W[:, kc, X * 128:(X + 1) * 128], xT[:, kc, t0:t0 + 512], kc == 0, kc == 7,
                            reads=[wk], writes=[('ps', bank)])
                if sg > 0:
                    self.copy('dve', pre[X][:, 0:3], hist[X][:, 0:3], reads=[K('hist', X)], writes=[K('pre', X)])
                else:
                    self.s.op('dve', lambda e, X=X: e.memset(pre[X][:, 0:3], 0.0), writes=[K('pre', X)])
                self.copy('act', pre[X][:, 3:515], ps[bank][:, :], reads=[('ps', bank)], writes=[K('pre', X)])
                wc = lambda k: cw[:, h * 12 + X * 4 + k:h * 12 + X * 4 + k + 1]
                self.ts(cv[X], pre[X][:, 0:512], wc(0), None, ALU.mult, reads=[K('pre', X)], writes=[K('cv', X)])
                for k in range(1, 4):
                    self.stt(cv[X], pre[X][:, k:k + 512], wc(k), cv[X], ALU.mult, ALU.add,
                             reads=[K('pre', X), K('cv', X)], writes=[K('cv', X)])
                self.copy('dve', hist[X][:, 0:3], pre[X][:, 512:515], reads=[K('pre', X)], writes=[K('hist', X)])
                self.act(cv[X], cv[X], AF.Silu, reads=[K('cv', X)], writes=[K('cv', X)])
                yield
            for X in range(2):
                sqb = pre[X][:, 3:515]
                self.act(sqb, cv[X], AF.Square, scale=(128.0 ** 0.5 if X == 0 else 1.0), reads=[K('cv', X)], writes=[K('pre', X)])
                self.mm(ps[X][:, :], ones, sqb, True, True, reads=[K('pre', X)], writes=[('ps', X)])
                self.act(sqb, ps[X][:, :], AF.Sqrt, bias=self.cst[:, X:X + 1], reads=[('ps', X)], writes=[K('pre', X)])
                self.s.op('dve', lambda e, sqb=sqb: e.reciprocal(out=sqb, in_=sqb), reads=[K('pre', X)], writes=[K('pre', X)])
                if X == 0:
                    self.tt(st['QTn'], cv[0], sqb, ALU.mult, reads=[K('cv', 0), K('pre', 0)], writes=[K('QTn', k_)])
                else:
                    self.tt(cv[1], cv[1], sqb, ALU.mult, reads=[K('cv', 1), K('pre', 1)], writes=[K('cv', 1)])
                yield
            KTn, VTs, QTn = cv[1], cv[2], st['QTn']
            b = next(bankrot)
            for kc in range(8):
                self.mm(ps[b][:, :], W[:, kc, 384:512], xT[:, kc, t0:t0 + 512], kc == 0, kc == 7, reads=[wk], writes=[('ps', b)])
            self.act(st['zs'], ps[b][:, :].rearrange("p (a e) -> p a e", a=4), AF.Silu, reads=[('ps', b)], writes=[K('zs', k_)])
            yield
            ti = lambda pp: sg * 4 + pp
            col = lambda A_, pp: A_[:, ti(pp), h:h + 1]
            pc = lambda pp: slice(pp * 128, (pp + 1) * 128)
            stage(lambda pp, q, bk: self.tr(q, KTn[:, pc(pp)], reads=[K('cv', 1)], writes=[bk]),
                  lambda pp, q, bk: (self.ts(Kbg[pp], q, col(BEG, pp), None, ALU.mult, reads=[bk], writes=[K('Kbg', pp)]),
                                     self.act(st['Kdec'][pp], q, AF.Identity, scale=col(ED, pp), reads=[bk], writes=[K('Kdec', k_, pp)])))
            yield
            stage(lambda pp, q, bk: self.tr(q, VTs[:, pc(pp)], reads=[K('cv', 2)], writes=[bk]),
                  lambda pp, q, bk: self.ts(Vb[pp], q, col(BETA, pp), None, ALU.mult, reads=[bk], writes=[K('Vb', pp)]))
            yield
            for pp in range(4):
                self.ts(gU[pp], U, col(Gs, pp), None, ALU.mult, writes=[K('gU', pp)])
            stage(lambda pp, q, bk: (self.mm(q, gU[pp], Ls, True, False, reads=[K('gU', pp)], writes=[bk]),
                                     self.mm(q, ident, MS, False, True, writes=[bk])),
                  lambda pp, q, bk: self.act(Ds[pp], q, AF.Exp, reads=[bk], writes=[K('Ds', pp)]))
            yield
            stage(lambda pp, q, bk: (self.mm(q, Ls, gU[pp], True, False, reads=[K('gU', pp)], writes=[bk]),
                                     self.mm(q, ident, MIT, False, True, writes=[bk])),
                  lambda pp, q, bk: self.act(DTi[pp], q, AF.Exp, reads=[bk], writes=[K('DTi', pp)]))
            yield
            stage(lambda pp, q, bk: self.mm(q, KTn[:, pc(pp)], QTn[:, pc(pp)], True, True, reads=[K('cv', 1), K('QTn', k_)], writes=[bk]),
                  lambda pp, q, bk: self.tt(st['qkD'][pp], q, DTi[pp], ALU.mult, reads=[bk, K('DTi', pp)], writes=[K('qkD', k_, pp)]))
            yield
            Z, Y, TT = [B['Za'], gU], [B['Ya'], Ds], [B['TTa'], DTi]
            Zk = [lambda pp: K('Za', pp), lambda pp: K('gU', pp)]
            Yk = [lambda pp: K('Ya', pp), lambda pp: K('Ds', pp)]
            TTk = [lambda pp: K('TTa', pp), lambda pp: K('DTi', pp)]
            stage(lambda pp, q, bk: self.mm(q, KTn[:, pc(pp)], KTn[:, pc(pp)], True, True, reads=[K('cv', 1)], writes=[bk]),
                  lambda pp, q, bk: self.stt(Z[0][pp], q, col(NB, pp), Ds[pp], ALU.mult, ALU.mult,
                                             reads=[bk, K('Ds', pp)], writes=[Zk[0](pp)]))
            yield
            stage(lambda pp, q, bk: self.tr(q, Z[0][pp], reads=[Zk[0](pp)], writes=[bk]),
                  lambda pp, q, bk: (self.copy('act', Y[0][pp], q, reads=[bk], writes=[Yk[0](pp)]),
                                     self.tt(TT[0][pp], q, ident, ALU.add, reads=[bk], writes=[TTk[0](pp)])))
            yield
            cur = 0
            for k in range(1, 6):
                nxt = 1 - cur
                stage(lambda pp, q, bk: self.mm(q, Y[cur][pp], Z[cur][pp], True, True, reads=[Yk[cur](pp), Zk[cur](pp)], writes=[bk]),
                      lambda pp, q, bk: self.copy(next(evrot), Z[nxt][pp], q, reads=[bk], writes=[Zk[nxt](pp)]))
                if k < 5:
                    stage(lambda pp, q, bk: self.mm(q, Z[cur][pp], Y[cur][pp], True, True, reads=[Yk[cur](pp), Zk[cur](pp)], writes=[bk]),
                          lambda pp, q, bk: self.copy(next(evrot), Y[nxt][pp], q, reads=[bk], writes=[Yk[nxt](pp)]))
                yield
                stage(lambda pp, q, bk: self.mm(q, Z[nxt][pp], TT[cur][pp], True, True, reads=[Zk[nxt](pp), TTk[cur](pp)], writes=[bk]),
                      lambda pp, q, bk: self.tt(TT[nxt][pp], q, TT[cur][pp], ALU.add, reads=[bk, TTk[cur](pp)], writes=[TTk[nxt](pp)]))
                cur = nxt
                yield
            TTf, TTfk = TT[cur], TTk[cur]
            stage(lambda pp, q, bk: self.mm(q, TTf[pp], Vb[pp], True, True, reads=[TTfk(pp), K('Vb', pp)], writes=[bk]),
                  lambda pp, q, bk: self.copy(next(evrot), st['u0'][pp], q, reads=[bk], writes=[K('u0', k_, pp)]))
            yield
            stage(lambda pp, q, bk: self.mm(q, Kbg[pp], TTf[pp], True, True, reads=[TTfk(pp), K('Kbg', pp)], writes=[bk]),
                  lambda pp, q, bk: self.copy(next(evrot), st['wT'][pp], q, reads=[bk], writes=[K('wT', k_, pp)]))
            yield

        def scan(h, sg, st, B, ysl):
            hs = B['hs']
            K = lambda *a: (hs,) + a
            t0 = sg * 512
            k_ = st['idx']
            Sst, ub, osb, onb, ssq, rn1, ybst = B['S'], B['ub'], B['osb'], B['onb'], B['ssq'], B['rn1'], B['ybst']
            for pp in range(4):
                i = sg * 4 + pp
                for c in range(2):
                    rows = slice(64 * c, 64 * c + 64)
                    b1 = next(bankrot)
                    self.mm(ps[b1][rows, 0:128], st['wT'][pp][:, 64 * c:64 * c + 64], Sst, True, True,
                            reads=[K('wT', k_, pp), K('S')], writes=[('ps', b1)])
                    self.mm(ps[b1][rows, 128:256], st['QTn'][:, pp * 128 + 64 * c:pp * 128 + 64 * c + 64], Sst, True, True,
                            reads=[K('QTn', k_), K('S')], writes=[('ps', b1)])
                    self.tt(ub[rows, :], st['u0'][pp][rows, :], ps[b1][rows, 0:128], ALU.subtract,
                            reads=[('ps', b1), K('u0', k_, pp)], writes=[K('ub')])
                    self.act(osb[rows, :], ps[b1][rows, 128:256], AF.Identity, scale=EG[rows, i, h:h + 1],
                             reads=[('ps', b1)], writes=[K('osb')])
                    yield
                    b2 = next(bankrot)
                    self.mm(ps[b2][:, 0:128], st['Kdec'][pp][rows, :], ub[rows, :], True, True,
                            reads=[K('ub'), K('Kdec', k_, pp)], writes=[('ps', b2)])
                    self.stt(Sst, Sst, GLB[:, 2 * i + c, h:h + 1], ps[b2][:, 0:128], ALU.mult, ALU.add,
                             reads=[('ps', b2), K('S')], writes=[K('S')])
                    yield
                b3 = next(bankrot)
                self.mm(ps[b3][:, 0:128], st['qkD'][pp], ub, True, True, reads=[K('ub'), K('qkD', k_, pp)], writes=[('ps', b3)])
                self.tt(osb, osb, ps[b3][:, 0:128], ALU.add, reads=[('ps', b3), K('osb')], writes=[K('osb')])
                self.s.op('dve', lambda e, ssq=ssq: e.memset(ssq, 0.0), writes=[K('ssq')])
                self.act(onb, osb, AF.Square, accum_out=ssq, reads=[K('osb'), K('ssq')], writes=[K('onb'), K('ssq')])
                self.act(rn1, ssq, AF.Sqrt, bias=self.cst[:, 0:1], reads=[K('ssq')], writes=[K('rn1')])
                self.s.op('dve', lambda e, rn1=rn1: e.reciprocal(out=rn1, in_=rn1), reads=[K('rn1')], writes=[K('rn1')])
                self.stt(onb, osb, rn1[:, 0:1], onw, ALU.mult, ALU.mult, reads=[K('osb'), K('rn1')], writes=[K('onb')])
                b4 = next(bankrot)
                self.tr(ps[b4][:, 0:128], onb, reads=[K('onb')], writes=[('ps', b4)])
                self.tt(ybst[ysl][:, pp * 128:(pp + 1) * 128], ps[b4][:, 0:128], st['zs'][:, pp, :], ALU.mult,
                        reads=[('ps', b4), K('zs', k_)], writes=[K('ybst')])
                yield
            self.s.dma('sp', self.yb_d[h][:, t0:t0 + 512], ybst[ysl], key=f'ybst{hs}', reads=[K('ybst')], writes=[('yb_d', h, sg)])

        def merged(g1, g2):
            gens = [g for g in (g1, g2) if g is not None]
            while gens:
                for g in list(gens):
                    try:
                        next(g)
                        yield
                    except StopIteration:
                        gens.remove(g)

        def chain(h, B):
            hs = B['hs']
            s.dma('pool', B['wu'], w_in_r[l][:, :, OFF_DN + h * 512:OFF_DN + (h + 1) * 512], key=f'dwu{hs}', writes=[(hs, 'wu')])
            self.s.op('dve', lambda e: e.memset(B['S'], 0.0), reads=[(hs, 'S')], writes=[(hs, 'S')])
            nsg = 8 if DN_LIMIT >= 3 else 1
            yield from prep(h, 0, B['sets'][0], B)
            for sg in range(nsg):
                nxt = prep(h, sg + 1, B['sets'][(sg + 1) % 2], B) if sg + 1 < nsg else None
                yield from merged(scan(h, sg, B['sets'][sg % 2], B, sg % 2), nxt)

        NPAR = 2
        bufs = [make_bufs(k) for k in range(NPAR)]
        heads = list(range(8 if DN_LIMIT >= 9 else (NPAR if DN_LIMIT >= 1 else 0)))
        for h0 in range(0, len(heads), NPAR):
            gens = [chain(h0 + k, bufs[k]) for k in range(NPAR) if h0 + k < len(heads)]
            for _ in merged(*gens) if len(gens) == 2 else gens[0]:
                pass
        ar.release(m)

    def ln_tile(self, res, res_key, add_views, add_keys, gam, bet, out, out_key, tbufs, statss, ls):
        tbuf, stats = tbufs[ls], statss[ls]
        kt, kst = ('ln_t', ls), ('ln_st', ls)
        s1, nm, ss, rs = (stats[:, j:j + 1] for j in range(4))
        for hf in range(2):
            cs = slice(hf * 512, (hf + 1) * 512)
            self.stt(tbuf[:, cs], res[:, cs], ALPHA, add_views[hf], ALU.mult, ALU.add,
                     reads=[res_key, add_keys[hf]], writes=[kt])
        self.s.op('dve', lambda e: e.memset(stats[:, 0:4], 0.0), writes=[kst])
        self.act(out, tbuf, AF.Identity, accum_out=s1, reads=[kt, kst], writes=[out_key, kst])
        self.ts(nm, s1, -1.0 / DM, None, ALU.mult, reads=[kst], writes=[kst])
        self.act(out, tbuf, AF.Square, bias=nm, accum_out=ss, reads=[kt, kst], writes=[out_key, kst])
        self.act(rs, ss, AF.Sqrt, bias=self.cst[:, 2:3], scale=1.0 / DM, reads=[kst], writes=[kst])
        self.s.op('dve', lambda e: e.reciprocal(out=rs, in_=rs), reads=[kst], writes=[kst])
        self.ts(tbuf, tbuf, nm, rs, ALU.add, ALU.mult, reads=[kt, kst], writes=[kt])
        self.tt(tbuf, tbuf, gam, ALU.mult, reads=[kt, 'lnp'], writes=[kt])
        self.tt(out, tbuf, bet, ALU.add, reads=[kt, 'lnp'], writes=[out_key])

    def phase_merge(self, l):
        import itertools
        s, ar, ps, xT = self.s, self.ar, self.ps, self.xT
        m = ar.mark()
        moe = (l % 2 == 1)
        w_in_r = self.I('w_in_r')
        Wga = ar.alloc(8 * DM, BF16).rearrange("p (k c) -> p k c", k=8)
        Wgb = ar.alloc(8 * DM, BF16).rearrange("p (k c) -> p k c", k=8)
        Woa = ar.alloc(4 * DM, BF16).rearrange("p (k c) -> p k c", k=4)
        Wob = ar.alloc(8 * DM, BF16).rearrange("p (k c) -> p k c", k=8)
        Wout = ar.alloc(8 * DM, BF16).rearrange("p (k c) -> p k c", k=8)
        gam, bet = ar.alloc(DM), ar.alloc(DM)
        s.dma('pool', Wga, w_in_r[l][:, :, OFF_GA:OFF_GA + DM], key='mw0', writes=['Wga'])
        s.dma('pool', Woa, self.I('w_oa_r')[l], key='mw1', writes=['Woa'])
        s.dma('pool', Wgb, w_in_r[l][:, :, OFF_GB:OFF_GB + DM], key='mw2', writes=['Wgb'])
        s.dma('pool', Wob, self.I('w_ob_r')[l], key='mw3', writes=['Wob'])
        s.dma('pool', Wout, self.I('w_out_r')[l], key='mw4', writes=['Wout'])
        s.dma('sp', gam, self.I('ln_r')[l][0], key='c0', writes=['lnp'])
        s.dma('sp', bet, self.I('ln_r')[l][1], key='c0', writes=['lnp'])
        TB = 512
        yaT = [ar.alloc(4 * TB, BF16).rearrange("p (k t) -> p k t", k=4)] * 2
        ybT = [ar.alloc(8 * TB, BF16).rearrange("p (k t) -> p k t", k=8) for _ in range(2)]
        mT = ar.alloc(8 * TB, BF16).rearrange("p (k t) -> p k t", k=8)
        sg = [ar.alloc(TB) for _ in range(2)]
        xres = [ar.alloc(DM)] * 2
        tbufs = [ar.alloc(DM) for _ in range(2)]
        xo = [ar.alloc(DM) for _ in range(2)]
        statss = [ar.alloc(8) for _ in range(2)]
        if moe:
            x1Tf = ar.alloc(8 * 128).rearrange("p (k t) -> p k t", k=8)
            wr = ar.alloc(8 * NEXP).rearrange("p (k e) -> p k e", k=8)
            s.dma('sp', wr, self.I('moe_router_r'), key='c0', writes=['wr'])
            L, Lm, mk, E = ar.alloc(8), ar.alloc(8), ar.alloc(8), ar.alloc(8)
            gst = ar.alloc(8)
        src_res = self.I('x') if l == 0 else self.x2_d
        brot = itertools.cycle(range(0, 4))
        ya_v = self.ya_d.rearrange("k p t -> p k t")
        yb_v = self.yb_d.rearrange("k p t -> p k t")
        nblk = S_LEN // TB
        for blk in range(nblk):
            bs = slice(blk * TB, (blk + 1) * TB)
            sl = blk % 2
            s.dma('sp', yaT[sl], ya_v[:, :, bs], key='ya0', writes=[('yaT', 0)])
            s.dma('sp', ybT[sl], yb_v[:, :, bs], key=f'yb{sl}', writes=[('ybT', sl)])
            for oc in range(8):
                ocs = slice(oc * 128, (oc + 1) * 128)
                for br in range(2):
                    Wg, Wo, yT, nk = (Wga, Woa, yaT[sl], 4) if br == 0 else (Wgb, Wob, ybT[sl], 8)
                    wgk, wok, yk = (('Wga', 'Woa', ('yaT', 0)) if br == 0 else ('Wgb', 'Wob', ('ybT', sl)))
                    b1 = next(brot)
                    for kc in range(8):
                        self.mm(ps[b1][:, 0:TB], Wg[:, kc, ocs], xT[:, kc, bs], kc == 0, kc == 7,
                                reads=[wgk, ('xTb', blk)], writes=[('ps', b1)])
                    self.act(sg[br], ps[b1][:, 0:TB], AF.Sigmoid, reads=[('ps', b1)], writes=[('sg', br)])
                    b2 = next(brot)
                    for kc in range(nk):
                        self.mm(ps[b2][:, 0:TB], Wo[:, kc, ocs], yT[:, kc, :], kc == 0, kc == nk - 1,
                                reads=[wok, yk], writes=[('ps', b2)])
                    self.tt(sg[br], sg[br], ps[b2][:, 0:TB], ALU.mult, reads=[('sg', br), ('ps', b2)], writes=[('sg', br)])
                    if br == 1:
                        self.tt(mT[:, oc, :], sg[0], sg[1], ALU.add, reads=[('sg', 0), ('sg', 1)], writes=['mT'])
            for sub in range(TB // 128):
                i = blk * (TB // 128) + sub
                xs = i % 2
                s.dma('sp', xres[xs], src_res[i * 128:(i + 1) * 128, :], key='xr0', writes=[('xres', 0)])
                for hf in range(2):
                    for kc in range(8):
                        self.mm(ps[4 + hf][:, :], mT[:, kc, sub * 128:(sub + 1) * 128], Wout[:, kc, hf * 512:(hf + 1) * 512],
                                kc == 0, kc == 7, reads=['mT', 'Wout'], writes=[('ps', 4 + hf)])
                self.ln_tile(xres[xs], ('xres', 0), [ps[4][:, :], ps[5][:, :]], [('ps', 4), ('ps', 5)], gam, bet,
                             xo[xs], ('xo', xs), tbufs, statss, xs)
                s.dma('sp', self.x1_d[i * 128:(i + 1) * 128, :], xo[xs], key=f'xo{xs}', reads=[('xo', xs)], writes=[('x1_d', i)])
                for hf in range(2):
                    b = 6 + hf
                    for q in range(4):
                        kc = hf * 4 + q
                        self.tr(ps[b][:, q * 128:(q + 1) * 128], xo[xs][:, kc * 128:(kc + 1) * 128], reads=[('xo', xs)], writes=[('ps', b)])
                    self.copy('act' if hf == 0 else 'dve', xT[:, hf * 4:(hf + 1) * 4, i * 128:(i + 1) * 128],
                              ps[b][:, :].rearrange("p (k t) -> p k t", k=4), reads=[('ps', b)], writes=[('xTb', blk)])
                    if moe:
                        self.copy('dve' if hf == 0 else 'act', x1Tf[:, hf * 4:(hf + 1) * 4, :],
                                  ps[b][:, :].rearrange("p (k t) -> p k t", k=4), reads=[('ps', b)], writes=['x1Tf'])
                if moe:
                    for kc in range(8):
                        self.mm(ps[6][:, 0:NEXP], x1Tf[:, kc, :], wr[:, kc, :], kc == 0, kc == 7, reads=['x1Tf', 'wr'], writes=[('ps', 6)])
                    self.copy('dve', L, ps[6][:, 0:NEXP], reads=[('ps', 6)], writes=['L'])
                    m1, m2, den = gst[:, 0:1], gst[:, 1:2], gst[:, 2:3]
                    self.s.op('dve', lambda e, m1=m1: e.tensor_reduce(out=m1, in_=L, axis=AX.X, op=ALU.max), reads=['L'], writes=['gst'])
                    self.ts(Lm, L, m1, None, ALU.subtract, reads=['L', 'gst'], writes=['Lm'])
                    self.ts(mk, Lm, 0.0, None, ALU.is_equal, reads=['Lm'], writes=['mk'])
                    self.stt(mk, mk, -1e30, Lm, ALU.mult, ALU.add, reads=['mk', 'Lm'], writes=['mk'])
                    self.s.op('dve', lambda e, m2=m2: e.tensor_reduce(out=m2, in_=mk, axis=AX.X, op=ALU.max), reads=['mk'], writes=['gst'])
                    self.ts(mk, Lm, m2, None, ALU.is_ge, reads=['Lm', 'gst'], writes=['mk'])
                    self.act(E, Lm, AF.Exp, reads=['Lm'], writes=['E'])
                    self.tt(E, E, mk, ALU.mult, reads=['E', 'mk'], writes=['E'])
                    self.s.op('dve', lambda e, den=den: e.tensor_reduce(out=den, in_=E, axis=AX.X, op=ALU.add), reads=['E'], writes=['gst'])
                    self.s.op('dve', lambda e, den=den: e.reciprocal(out=den, in_=den), reads=['gst'], writes=['gst'])
                    self.ts(self.Gt[:, i, :], E, den, None, ALU.mult, reads=['E', 'gst'], writes=['Gt'])
        ar.release(m)

    def phase_ffn(self, l):
        import itertools
        s, ar, ps, xT = self.s, self.ar, self.ps, self.xT
        m = ar.mark()
        moe = (l % 2 == 1)
        last = (l == DEPTH - 1)
        if moe:
            nexp, nch = NEXP, D_FFE // 128
            gsrc = lambda e, c0, n: self.I('moe_g_r')[e][c0:c0 + n].rearrange("c p k j -> p c (k j)")
            usrc = lambda e, c0, n: self.I('moe_u_r')[e][c0:c0 + n].rearrange("c p k j -> p c (k j)")
            dsrc = lambda e, c0, n: self.I('moe_d')[e][c0 * 128:(c0 + n) * 128, :].rearrange("(c p) n -> p c n", p=128)
        else:
            nexp, nch = 1, D_FF // 128
            gsrc = lambda e, c0, n: self.I('ffn_g_r')[c0:c0 + n].rearrange("c p k j -> p c (k j)")
            usrc = lambda e, c0, n: self.I('ffn_u_r')[c0:c0 + n].rearrange("c p k j -> p c (k j)")
            dsrc = lambda e, c0, n: self.I('ffn_d')[c0 * 128:(c0 + n) * 128, :].rearrange("(c p) n -> p c n", p=128)
        GC = 4
        TBK = 1024
        acc = ar.alloc(8 * DM).rearrange("p (a n) -> p a n", a=8)
        hT = [ar.alloc(GC * TBK, BF16).rearrange("p (c t) -> p c t", c=GC) for _ in range(2)]
        Wg = [ar.alloc(GC * 1024, BF16).rearrange("p (c k j) -> p c k j", c=GC, k=8) for _ in range(2)]
        Wu = [ar.alloc(GC * 1024, BF16).rearrange("p (c k j) -> p c k j", c=GC, k=8) for _ in range(2)]
        Wd = [ar.alloc(GC * DM, BF16).rearrange("p (c n) -> p c n", c=GC) for _ in range(2)]
        sgb = [ar.alloc(512) for _ in range(2)]
        gam, bet = ar.alloc(DM), ar.alloc(DM)
        xres = [ar.alloc(DM) for _ in range(2)]
        tbufs = [ar.alloc(DM) for _ in range(2)]
        xo = [ar.alloc(DM) for _ in range(2)]
        statss = [ar.alloc(8) for _ in range(2)]
        s.dma('sp', gam, self.I('ln_r')[l][2], key='c0', writes=['lnp'])
        s.dma('sp', bet, self.I('ln_r')[l][3], key='c0', writes=['lnp'])
        groups = [(c0, min(GC, nch - c0)) for c0 in range(0, nch, GC)]
        gurot = itertools.cycle([(0, 1), (2, 3)])
        drot = itertools.cycle([4, 5])
        dst = self.y_out if last else self.x2_d
        nslot = 0
        for tb in range(S_LEN // TBK if FFN_LIMIT >= 9 else 1):
            first = True
            for e in range(nexp):
                for (c0, n) in (groups if FFN_LIMIT >= 2 else groups[:1]):
                    sl = nslot % 2
                    nslot += 1
                    s.dma('pool', Wg[sl][:, 0:n].rearrange("p c k j -> p c (k j)"), gsrc(e, c0, n), key=f'fg{sl}', writes=[('Wg', sl)])
                    s.dma('pool', Wu[sl][:, 0:n].rearrange("p c k j -> p c (k j)"), usrc(e, c0, n), key=f'fu{sl}', writes=[('Wu', sl)])
                    s.dma('pool', Wd[sl][:, 0:n], dsrc(e, c0, n), key=f'fd{sl}', writes=[('Wd', sl)])
                    for c in range(n):
                        for hf in range(2):
                            ts_ = slice(tb * TBK + hf * 512, tb * TBK + (hf + 1) * 512)
                            bg, bu = next(gurot)
                            for kc in range(8):
                                self.mm(ps[bg][:, :], Wg[sl][:, c, kc, :], xT[:, kc, ts_], kc == 0, kc == 7,
                                        reads=[('Wg', sl), ('xTb', tb)], writes=[('ps', bg)])
                            for kc in range(8):
                                self.mm(ps[bu][:, :], Wu[sl][:, c, kc, :], xT[:, kc, ts_], kc == 0, kc == 7,
                                        reads=[('Wu', sl), ('xTb', tb)], writes=[('ps', bu)])
                            k2 = (c * 2 + hf) % 2
                            self.act(sgb[k2], ps[bg][:, :], AF.Silu, reads=[('ps', bg)], writes=[('sgb', k2)])
                            self.tt(hT[sl][:, c, hf * 512:(hf + 1) * 512], sgb[k2], ps[bu][:, :], ALU.mult,
                                    reads=[('sgb', k2), ('ps', bu)], writes=[('hT', sl)])
                    for sub in range(8 if FFN_LIMIT >= 1 else 0):
                        i = tb * 8 + sub
                        for hf in range(2):
                            bd = next(drot)
                            for c in range(n):
                                self.mm(ps[bd][:, :], hT[sl][:, c, sub * 128:(sub + 1) * 128], Wd[sl][:, c, hf * 512:(hf + 1) * 512],
                                        c == 0, c == n - 1, reads=[('hT', sl), ('Wd', sl)], writes=[('ps', bd)])
                            av = acc[:, sub, hf * 512:(hf + 1) * 512]
                            ak = ('acc', sub, hf)
                            if moe:
                                gcol = self.Gt[:, i, e:e + 1]
                                if first:
                                    self.ts(av, ps[bd][:, :], gcol, None, ALU.mult, reads=[('ps', bd)], writes=[ak])
                                else:
                                    self.stt(av, ps[bd][:, :], gcol, av, ALU.mult, ALU.add, reads=[('ps', bd), ak], writes=[ak])
                            else:
                                if first:
                                    self.copy('dve', av, ps[bd][:, :], reads=[('ps', bd)], writes=[ak])
                                else:
                                    self.tt(av, av, ps[bd][:, :], ALU.add, reads=[('ps', bd), ak], writes=[ak])
                    first = False
            for sub in range(8 if FFN_LIMIT >= 3 else 0):
                i = tb * 8 + sub
                xs = i % 2
                s.dma('sp', xres[xs], self.x1_d[i * 128:(i + 1) * 128, :], key=f'xr{xs}', writes=[('xres', xs)])
                self.ln_tile(xres[xs], ('xres', xs), [acc[:, sub, 0:512], acc[:, sub, 512:1024]], [('acc', sub, 0), ('acc', sub, 1)],
                             gam, bet, xo[xs], ('xo', xs), tbufs, statss, xs)
                s.dma('sp', dst[i * 128:(i + 1) * 128, :], xo[xs], key=f'xo{xs}', reads=[('xo', xs)], writes=[('dst', i)])
                if not last:
                    for hf in range(2):
                        b = 6 + hf
                        for q in range(4):
                            kc = hf * 4 + q
                            self.tr(ps[b][:, q * 128:(q + 1) * 128], xo[xs][:, kc * 128:(kc + 1) * 128], reads=[('xo', xs)], writes=[('ps', b)])
                        self.copy('act' if hf == 0 else 'dve', xT[:, hf * 4:(hf + 1) * 4, i * 128:(i + 1) * 128],
                                  ps[b][:, :].rearrange("p (k t) -> p k t", k=4), reads=[('ps', b)], writes=[('xTb', tb)])
        ar.release(m)


def _prep_shared(inp):
    f = lambda a: np.ascontiguousarray(a, dtype=np.float32)
    perm = _w_in_perm()
    w_in = inp["w_in"]
    w_in_r = np.stack([w_in[l][:, perm].reshape(8, 128, N_IN).transpose(1, 0, 2) for l in range(DEPTH)])
    cw = inp["conv_w"]
    conv_r = np.stack([cw[l].reshape(4, 3, 8, 128).transpose(3, 2, 1, 0).reshape(128, 96) for l in range(DEPTH)])
    headp = np.stack([np.broadcast_to(np.concatenate([inp["a_log"][l], inp["dt_bias"][l]])[None, :], (128, 16)) for l in range(DEPTH)])
    onw = np.stack([np.broadcast_to(inp["o_norm_w"][l][None, :], (128, 128)) for l in range(DEPTH)])
    kt = lambda w, kc: w.reshape(kc, 128, w.shape[1]).transpose(1, 0, 2)
    w_oa_r = np.stack([kt(inp["w_oa"][l], 4) for l in range(DEPTH)])
    w_ob_r = np.stack([kt(inp["w_ob"][l], 8) for l in range(DEPTH)])
    w_out_r = np.stack([kt(inp["w_out"][l], 8) for l in range(DEPTH)])
    ln_r = np.stack([np.stack([np.broadcast_to(inp[k][l][None, :], (128, DM)) for k in ("ln1_g", "ln1_b", "ln2_g", "ln2_b")]) for l in range(DEPTH)])
    ct = lambda w: w.reshape(8, 128, w.shape[1] // 128, 128).transpose(2, 1, 0, 3)
    shared = {
        "cmat": _const_mats(),
        "biasT": _bias_tables(np.asarray(inp["rel_bias"], np.float32)),
        "w_in_r": w_in_r, "conv_r": conv_r, "headp": headp, "onw": onw,
        "w_oa_r": w_oa_r, "w_ob_r": w_ob_r, "w_out_r": w_out_r, "ln_r": ln_r,
        "ffn_g_r": ct(inp["ffn_w_gate"][0]), "ffn_u_r": ct(inp["ffn_w_up"][0]), "ffn_d": inp["ffn_w_down"][0],
        "moe_router_r": inp["moe_router"][0].reshape(8, 128, NEXP).transpose(1, 0, 2),
        "moe_g_r": np.stack([ct(inp["moe_w_gate"][0][e]) for e in range(NEXP)]),
        "moe_u_r": np.stack([ct(inp["moe_w_up"][0][e]) for e in range(NEXP)]),
        "moe_d": inp["moe_w_down"][0],
    }
    return {k: f(v) for k, v in shared.items()}


def kernel(**inputs):
    inp = {k: np.asarray(v) for k, v in inputs.items()}
    shared = _prep_shared(inp)
    prog = Prog()
    x = np.ascontiguousarray(inp["x"], dtype=np.float32)
    shared = {k: v for k, v in shared.items() if k in prog.ins}
    in_maps = [dict(shared, x=x[b]) for b in range(8)]
    res = run_bass_kernel_spmd(prog.nc, in_maps, core_ids=list(range(8)))
    return np.stack([np.asarray(res.results[b]["y"], dtype=np.float32) for b in range(8)])
```

```python
import numpy as np
from contextlib import ExitStack
import concourse.bass as bass
import concourse.mybir as mybir
from concourse.bass_utils import run_bass_kernel_spmd

F32 = mybir.dt.float32
BF16 = mybir.dt.bfloat16
AF = mybir.ActivationFunctionType
ALU = mybir.AluOpType
AX = mybir.AxisListType

S_LEN = 4096
DM = 1024
NTILE = 32
N_IN = 10768
DEPTH = 2
D_FF = 2816
D_FFE = 3584
NEXP = 8
ALPHA = (2 * DEPTH) ** 0.25
LN_EPS = 1e-5
RMS_EPS = 1e-6
NEG = -30000.0
ENGS = ('pe', 'dve', 'act', 'pool', 'sp')
import os
ATT_LIMIT = int(os.environ.get('ATT_LIMIT', '9'))
DN_LIMIT = int(os.environ.get('DN_LIMIT', '9'))
FFN_LIMIT = int(os.environ.get('FFN_LIMIT', '9'))
SKIP_MIXER = int(os.environ.get('SKIP_MIXER', '0'))


class Sched:
    def __init__(self, nc, ctx):
        self.nc = nc
        self.ctx = ctx
        self.ops = {e: [] for e in ENGS}
        self.cnt = {e: 0 for e in ENGS}
        self.seen = {e: {} for e in ENGS}
        self.res = {}
        self.sems = {}
        for e in ('pe', 'dve', 'act', 'pool'):
            self.sems[e] = ctx.enter_context(nc.semaphore("sem_" + e))
        self.dcnt = {}

    def _sem(self, key):
        if key not in self.sems:
            self.sems[key] = self.ctx.enter_context(self.nc.semaphore("semd_" + str(key)))
            self.dcnt[key] = 0
        return self.sems[key]

    def _deps(self, eng, reads, writes):
        need = {}

        def add(k, v):
            if need.get(k, 0) < v:
                need[k] = v
        for r in reads:
            st = self.res.get(r)
            if st and st['w']:
                add(*st['w'])
        for w in writes:
            st = self.res.get(w)
            if st:
                if st['w'] and st['w'][0] != eng:
                    add(*st['w'])
                for k, v in st['r'].items():
                    if k != eng:
                        add(k, v)
        out = []
        for k, v in need.items():
            if self.seen[eng].get(k, 0) < v:
                self.seen[eng][k] = v
                out.append((k, v))
        return out

    def op(self, eng, fn, reads=(), writes=()):
        psr = [r for r in reads if isinstance(r, tuple) and r[0] == 'ps' and r not in writes]
        if psr:
            writes = list(writes) + psr
        waits = self._deps(eng, reads, writes)
        self.cnt[eng] += 1
        n = self.cnt[eng]
        self.ops[eng].append((waits, fn, eng, 1))
        for r in reads:
            st = self.res.setdefault(r, {'w': None, 'r': {}})
            st['r'][eng] = n
        for w in writes:
            self.res[w] = {'w': (eng, n), 'r': {}}
        return n

    def dma(self, eng, out, in_, key, reads=(), writes=(), **kw):
        self._sem(key)
        waits = self._deps(eng, reads, writes)
        self.dcnt[key] += 16
        n = self.dcnt[key]
        self.ops[eng].append((waits, lambda e: e.dma_start(out=out, in_=in_, **kw), key, 16))
        for r in reads:
            st = self.res.setdefault(r, {'w': None, 'r': {}})
            st['r'][key] = n
        for w in writes:
            self.res[w] = {'w': (key, n), 'r': {}}
        return n

    def dma_fn(self, eng, fn, key, reads=(), writes=()):
        self._sem(key)
        waits = self._deps(eng, reads, writes)
        self.dcnt[key] += 16
        n = self.dcnt[key]
        self.ops[eng].append((waits, fn, key, 16))
        for r in reads:
            st = self.res.setdefault(r, {'w': None, 'r': {}})
            st['r'][key] = n
        for w in writes:
            self.res[w] = {'w': (key, n), 'r': {}}
        return n

    def barrier(self):
        cur = {e: self.cnt[e] for e in ('pe', 'dve', 'act', 'pool')}
        cur.update(self.dcnt)
        for e in ENGS:
            waits = []
            for k, v in cur.items():
                if k != e and v > 0 and self.seen[e].get(k, 0) < v:
                    self.seen[e][k] = v
                    waits.append((k, v))
            if waits:
                self.ops[e].append((waits, None, None, 0))
        self.res = {}

    def emit(self):
        nc = self.nc
        sems = self.sems
        ops = self.ops

        def run(e, lst):
            for waits, fn, sk, inc in lst:
                for k, v in waits:
                    e.wait_ge(sems[k], v)
                if fn is not None:
                    fn(e).then_inc(sems[sk], inc)
        with nc.Block() as block:
            @block.tensor
            def _(e):
                run(e, ops['pe'])

            @block.vector
            def _(e):
                run(e, ops['dve'])

            @block.scalar
            def _(e):
                run(e, ops['act'])

            @block.gpsimd
            def _(e):
                run(e, ops['pool'])

            @block.sync
            def _(e):
                run(e, ops['sp'])


class Arena:
    def __init__(self, t, nwords):
        self.t = t
        self.n = nwords
        self.off = 0

    def alloc(self, nelem, dtype=F32):
        nw = nelem if dtype == F32 else (nelem + 1) // 2
        assert self.off + nw <= self.n, f"arena overflow {self.off}+{nw}>{self.n}"
        ap = self.t[:, self.off:self.off + nw]
        self.off += nw
        return ap if dtype == F32 else ap.bitcast(dtype)

    def mark(self):
        return self.off

    def release(self, m):
        self.off = m


def _w_in_perm():
    cols = []
    for hs in range(4):
        for g in range(3):
            h = g * 4 + hs
            for base in (0, 1536, 3072):
                cols.extend(range(base + h * 128, base + (h + 1) * 128))
    for h in range(8):
        for base in (4608, 5632, 6656, 7680):
            cols.extend(range(base + h * 128, base + (h + 1) * 128))
    cols.extend(range(8704, 8720))
    cols.extend(range(8720, 10768))
    assert len(cols) == N_IN
    return np.asarray(cols)


OFF_DN = 12 * 384
OFF_BA = OFF_DN + 8 * 512
OFF_GA = OFF_BA + 16
OFF_GB = OFF_GA + 1024


def _t5_bucket(dist):
    dist = np.asarray(dist, np.int64)
    d = np.maximum(dist, 1).astype(np.float32)
    large = 16 + (np.log(d / np.float32(16)) / np.float32(np.log(2048 / 16)) * np.float32(16)).astype(np.int32)
    large = np.minimum(large, 31)
    return np.where(dist < 16, dist, large)


def _bias_tables(rel_bias):
    out = np.full((128, 12, 256), NEG, np.float32)
    kj = np.arange(128)[:, None]
    qi = np.arange(128)[None, :]
    for h in range(12):
        d = (1, 4, 16)[h // 4]
        delta0 = qi - kj
        b0 = rel_bias[_t5_bucket(np.maximum(delta0, 0) * d), h]
        out[:, h, 0:128] = np.where(delta0 >= 0, b0, NEG)
        delta1 = qi + 128 - kj
        b1 = rel_bias[_t5_bucket(delta1 * d), h]
        out[:, h, 128:256] = np.where(delta1 <= 128, b1, NEG)
    return out


def _const_mats():
    r = np.arange(128)[:, None]
    c = np.arange(128)[None, :]
    same = (r // 64) == (c // 64)
    m = np.zeros((128, 9, 128), np.float32)
    m[:, 0] = (r == c)
    m[:, 1] = 1.0
    m[:, 2] = (r <= c) & same
    m[:, 3] = (r > c)
    m[:, 4] = np.where((r > c) & same, 0.0, NEG)
    m[:, 5] = np.where((c >= r) & same, 0.0, NEG)
    m[:, 6] = same
    m[:, 7] = (r < 64) * np.ones((1, 128))
    m[:, 8] = (r >= 64) * np.ones((1, 128))
    return m


class Prog:
    def __init__(self, dbg=(), stop_after=None):
        self.dbg = set(dbg)
        self.stop_after = stop_after
        self.nc = nc = bass.Bass("TRN2", target_bir_lowering=False)
        nc.allow_low_precision("bf16 matmul operands with fp32 PSUM accumulation")
        self.ctx = ExitStack()
        with self.ctx:
            self._build()

    def I(self, name):
        if name not in self.ins:
            self.ins[name] = self.nc.dram_tensor(name, list(self.in_shapes[name]), F32, kind="ExternalInput").ap()
        return self.ins[name]

    def mm(self, out, lhsT, rhs, start, stop, reads=(), writes=()):
        self.s.op('pe', lambda e: e.matmul(out, lhsT=lhsT, rhs=rhs, start=start, stop=stop), reads=reads, writes=writes)

    def tr(self, out, in_, reads=(), writes=()):
        ident = self.cm[:, 0, :]
        self.s.op('pe', lambda e: e.transpose(out, in_, ident), reads=reads, writes=writes)

    def copy(self, eng, out, in_, reads=(), writes=()):
        if eng == 'act':
            self.s.op('act', lambda e: e.activation(out=out, in_=in_, func=AF.Copy), reads=reads, writes=writes)
        else:
            self.s.op(eng, lambda e: e.tensor_copy(out=out, in_=in_), reads=reads, writes=writes)

    def act(self, out, in_, func, reads=(), writes=(), **kw):
        self.s.op('act', lambda e: e.activation(out=out, in_=in_, func=func, **kw), reads=reads, writes=writes)

    def tt(self, out, in0, in1, op, reads=(), writes=(), eng='dve'):
        self.s.op(eng, lambda e: e.tensor_tensor(out=out, in0=in0, in1=in1, op=op), reads=reads, writes=writes)

    def ts(self, out, in0, s1, s2, op0, op1=None, reads=(), writes=(), eng='dve', **kw):
        if op1 is None:
            self.s.op(eng, lambda e: e.tensor_scalar(out=out, in0=in0, scalar1=s1, scalar2=None, op0=op0, **kw), reads=reads, writes=writes)
        else:
            self.s.op(eng, lambda e: e.tensor_scalar(out=out, in0=in0, scalar1=s1, scalar2=s2, op0=op0, op1=op1, **kw), reads=reads, writes=writes)

    def stt(self, out, in0, scalar, in1, op0, op1, reads=(), writes=(), eng='dve', **kw):
        self.s.op(eng, lambda e: e.scalar_tensor_tensor(out=out, in0=in0, scalar=scalar, in1=in1, op0=op0, op1=op1, **kw), reads=reads, writes=writes)

    def dump(self, name, ap_sb, shape, dt, key, reads):
        o = self.nc.dram_tensor("dbg_" + name, list(shape), dt, kind="ExternalOutput").ap()
        self.s.dma('sp', o, ap_sb, key='dbg', reads=reads, writes=[('dbgout', name)])
        self.dbg_names.append(name)

    def _build(self):
        nc, ctx = self.nc, self.ctx
        self.s = s = Sched(nc, ctx)
        self.dbg_names = []
        self.in_shapes = {
            "x": [S_LEN, DM], "cmat": [128, 9, 128], "biasT": [128, 12, 256], "w_in_r": [DEPTH, 128, 8, N_IN],
            "conv_r": [DEPTH, 128, 96], "headp": [DEPTH, 128, 16], "onw": [DEPTH, 128, 128],
            "w_oa_r": [DEPTH, 128, 4, DM], "w_ob_r": [DEPTH, 128, 8, DM], "w_out_r": [DEPTH, 128, 8, DM],
            "ln_r": [DEPTH, 4, 128, DM], "ffn_g_r": [D_FF // 128, 128, 8, 128], "ffn_u_r": [D_FF // 128, 128, 8, 128],
            "ffn_d": [D_FF, DM], "moe_router_r": [128, 8, NEXP], "moe_g_r": [NEXP, D_FFE // 128, 128, 8, 128],
            "moe_u_r": [NEXP, D_FFE // 128, 128, 8, 128], "moe_d": [NEXP, D_FFE, DM],
        }
        self.ins = {}
        self.y_out = nc.dram_tensor("y", [S_LEN, DM], F32, kind="ExternalOutput").ap()
        self.x1_d = nc.dram_tensor("x1_scr", [S_LEN, DM], F32).ap()
        self.x2_d = nc.dram_tensor("x2_scr", [S_LEN, DM], F32).ap()
        self.ya_d = nc.dram_tensor("ya_scr", [4, 128, S_LEN], BF16).ap()
        self.yb_d = nc.dram_tensor("yb_scr", [8, 128, S_LEN], BF16).ap()
        NW = 53200
        big = ctx.enter_context(nc.sbuf_tensor("arena", [128, NW], F32))
        self.ar = ar = Arena(big, NW)
        self.ps = [ctx.enter_context(nc.psum_tensor(f"ps{i}", [128, 512], F32)) for i in range(8)]
        self.cm = ar.alloc(9 * 128).rearrange("p (k c) -> p k c", k=9)
        self.ones_bf = ar.alloc(128, BF16)
        s.dma('sp', self.cm, self.I('cmat'), key='c0', writes=['cm'])
        self.copy('dve', self.ones_bf, self.cm[:, 1, :], reads=['cm'], writes=['ones_bf'])
        self.Gt = ar.alloc(32 * NEXP).rearrange("p (i e) -> p i e", e=NEXP)
        self.cst = ar.alloc(4)
        self.m_xT = ar.mark()
        self.xT = ar.alloc(8 * S_LEN, BF16).rearrange("p (k t) -> p k t", k=8)
        for j, v in enumerate((128.0 * RMS_EPS, RMS_EPS, LN_EPS, 0.0)):
            s.op('dve', lambda e, j=j, v=v: e.memset(self.cst[:, j:j + 1], v), writes=['cst'])
        s.barrier()

        self.phase_make_xT(self.I('x'))
        s.barrier()
        if self.stop_after == 'xT':
            self.dump("xT", self.xT[:, 0, :], [128, S_LEN], BF16, 'dbg', [])
            self.finish()
            return
        for l in range(DEPTH):
            if SKIP_MIXER:
                self.phase_ffn(l)
                s.barrier()
                break
            self.phase_attention(l)
            s.barrier()
            if self.stop_after == ('att', l):
                break
            self.phase_deltanet(l)
            s.barrier()
            if ('yb', l) in self.dbg:
                for h in range(8):
                    self.dump(f"yb{l}_{h}", self.yb_d[h], [128, S_LEN], BF16, 'dbg', [])
            if self.stop_after == ('dn', l):
                break
            self.phase_merge(l)
            s.barrier()
            if ('x1', l) in self.dbg:
                self.dump(f"x1_{l}", self.x1_d, [S_LEN, DM], F32, 'dbg', [])
            if self.stop_after == ('mix', l):
                break
            self.phase_ffn(l)
            s.barrier()
            if ('x2', l) in self.dbg:
                self.dump(f"x2_{l}", self.x2_d if l == 0 else self.y_out, [S_LEN, DM], F32, 'dbg', [])
            if self.stop_after == ('ffn', l):
                break
        self.finish()

    def finish(self):
        s = self.s
        s.barrier()
        s.emit()

    def transposes_to_xT(self, src, src_key, i, bank0, eng_pair=('act', 'dve'), xf32=None):
        for half in range(2):
            b = bank0 + half
            for q in range(4):
                kc = half * 4 + q
                self.tr(self.ps[b][:, q * 128:(q + 1) * 128], src[:, kc * 128:(kc + 1) * 128],
                        reads=[src_key], writes=[('ps', b)])
            self.copy(eng_pair[half], self.xT[:, half * 4:(half + 1) * 4, i * 128:(i + 1) * 128],
                      self.ps[b][:, :].rearrange("p (k t) -> p k t", k=4),
                      reads=[('ps', b)], writes=[('xT', i)])
            if xf32 is not None:
                self.copy(eng_pair[1 - half], xf32[0][:, half * 4:(half + 1) * 4, :],
                          self.ps[b][:, :].rearrange("p (k t) -> p k t", k=4),
                          reads=[('ps', b)], writes=[xf32[1]])

    def phase_make_xT(self, src):
        s, ar = self.s, self.ar
        m = ar.mark()
        xl = [ar.alloc(DM) for _ in range(2)]
        for i in range(NTILE):
            sl = i % 2
            s.dma('sp', xl[sl], src[i * 128:(i + 1) * 128, :], key=f'xl{sl}', writes=[('xl', sl)])
            self.transposes_to_xT(xl[sl], ('xl', sl), i, bank0=(i % 2) * 2)
        ar.release(m)

    def phase_attention(self, l):
        s, ar, ps, xT = self.s, self.ar, self.ps, self.xT
        m = ar.mark()
        biasT = ar.alloc(12 * 256).rearrange("p (h c) -> p h c", h=12)
        wu = [ar.alloc(8 * 384, BF16).rearrange("p (k c) -> p k c", k=8) for _ in range(2)]
        qT = ar.alloc(S_LEN, BF16)
        kT = ar.alloc(S_LEN, BF16)
        V = ar.alloc(32 * 128, BF16).rearrange("p (b e) -> p b e", b=32)
        OD = ar.alloc(2 * S_LEN).rearrange("p (two t) -> p two t", two=2)
        PT = [ar.alloc(256, BF16) for _ in range(4)]
        tmp = [ar.alloc(256) for _ in range(2)]
        yst = ar.alloc(S_LEN, BF16)
        s.dma('sp', biasT, self.I('biasT'), key='c0', writes=['biasT'])
        scale = 128.0 ** -0.5
        for hs in range(4):
            for g in range(3):
                d = (1, 4, 16)[g]
                nb = 32 // d
                h = g * 4 + hs
                u = hs * 3 + g
                sl = u % 2
                s.dma('pool', wu[sl], self.I('w_in_r')[l][:, :, u * 384:(u + 1) * 384], key=f'wu{sl}', writes=[('wu', sl)])
                for tt in range(8):
                    for X in range(2):
                        bank = X
                        for kc in range(8):
                            self.mm(ps[bank][:, :], wu[sl][:, kc, X * 128:(X + 1) * 128], xT[:, kc, tt * 512:(tt + 1) * 512],
                                    kc == 0, kc == 7, reads=[('wu', sl)], writes=[('ps', bank)])
                        dst = (qT if X == 0 else kT).rearrange("p (r i) -> p r i", r=d)[:, :, (512 // d) * tt:(512 // d) * (tt + 1)]
                        src = ps[bank][:, :].rearrange("p (j r) -> p r j", r=d)
                        self.copy('act' if X == 0 else 'dve', dst, src, reads=[('ps', bank)], writes=['qT' if X == 0 else 'kT'])
                if ATT_LIMIT < 1:
                    continue
                for b4 in range(8):
                    bank = 2 + b4 % 2
                    for bb in range(4):
                        b = b4 * 4 + bb
                        r, c = divmod(b, nb)
                        t0 = 128 * c * d + r
                        for kc in range(8):
                            self.mm(ps[bank][:, bb * 128:(bb + 1) * 128], xT[:, kc, t0:t0 + 127 * d + 1:d], wu[sl][:, kc, 256:384],
                                    kc == 0, kc == 7, reads=[('wu', sl)], writes=[('ps', bank)])
                    self.copy('act' if b4 % 2 == 0 else 'dve', V[:, b4 * 4:(b4 + 1) * 4, :],
                              ps[bank][:, :].rearrange("p (b e) -> p b e", b=4), reads=[('ps', bank)], writes=[('V', b4)])
                if ATT_LIMIT < 2:
                    continue
                ODv = OD.rearrange("p two (i r) -> p two i r", r=d)

                def s_stage(b):
                    r, c = divmod(b, nb)
                    nq = 256 if c + 1 < nb else 128
                    bank = 4 + b % 2
                    sps = ps[bank][:, 0:nq]
                    self.mm(sps, kT[:, b * 128:(b + 1) * 128], qT[:, b * 128:b * 128 + nq], True, True,
                            reads=['qT', 'kT'], writes=[('ps', bank)])
                    tsl = b % 2
                    self.stt(tmp[tsl][:, :nq], sps, scale, biasT[:, h, :nq], ALU.mult, ALU.add,
                             reads=[('ps', bank), 'biasT'], writes=[('tmp', tsl)])
                    self.act(PT[b % 4][:, :nq], tmp[tsl][:, :nq], AF.Exp, reads=[('tmp', tsl)], writes=[('PT', b % 4)])

                def pv_stage(b):
                    r, c = divmod(b, nb)
                    bank = 6 + b % 2
                    ops_ = ps[bank][:, 0:256]
                    for which in range(2):
                        o_ = ops_[:, which * 128:(which + 1) * 128]
                        if c > 0:
                            lh = V[:, b - 1, :] if which == 0 else self.ones_bf
                            self.mm(o_, lh, PT[(b - 1) % 4][:, 128:256], True, False,
                                    reads=[('V', (b - 1) // 4), ('PT', (b - 1) % 4)], writes=[('ps', bank)])
                        lh = V[:, b, :] if which == 0 else self.ones_bf
                        self.mm(o_, lh, PT[b % 4][:, 0:128], c == 0, True,
                                reads=[('V', b // 4), ('PT', b % 4)], writes=[('ps', bank)])
                    view = ODv[:, :, 128 * c:128 * c + 128, r]
                    if g == 0:
                        regs = [('OD', b // 4)]
                    elif g == 1:
                        regs = [('OD', c)]
                    else:
                        regs = [('OD', 4 * c + j) for j in range(4)]
                    src = ops_.rearrange("p (two t) -> p two t", two=2)
                    if g == 0:
                        self.copy('act', view, src, reads=[('ps', bank)], writes=regs)
                    else:
                        self.tt(view, src, view, ALU.add, reads=[('ps', bank)] + regs, writes=regs)

                s_stage(0)
                for b in range(32):
                    if b + 1 < 32:
                        s_stage(b + 1)
                    pv_stage(b)
            if ATT_LIMIT < 3:
                self.dump(f"q{hs}", qT, [128, S_LEN], BF16, 'dbg', ['qT'])
                self.dump(f"v{hs}", V.rearrange("p b e -> p (b e)"), [128, S_LEN], BF16, 'dbg', [('V', i) for i in range(8)])
                continue
            for rg in range(8):
                cs = slice(rg * 512, (rg + 1) * 512)
                self.s.op('dve', lambda e, cs=cs: e.reciprocal(out=OD[:, 1, cs], in_=OD[:, 1, cs]), reads=[('OD', rg)], writes=[('OD', rg)])
                self.tt(yst[:, cs], OD[:, 0, cs], OD[:, 1, cs], ALU.mult, reads=[('OD', rg)], writes=['yst'])
            s.dma('sp', self.ya_d[hs], yst, key='yst', reads=['yst'], writes=[('ya_d', hs)])
            if ('ya', l) in self.dbg:
                self.dump(f"ya{l}_{hs}", yst, [128, S_LEN], BF16, 'dbg', ['yst'])
        ar.release(m)


    def phase_deltanet(self, l):
        import itertools
        s, ar, ps, xT, cm = self.s, self.ar, self.ps, self.xT, self.cm
        m = ar.mark()
        ident, ones, U, Ls, MS, MIT, BD, H0, H1 = (cm[:, k, :] for k in range(9))
        cw = ar.alloc(96)
        hp = ar.alloc(16)
        onw = ar.alloc(128)
        negA = ar.alloc(8)
        s.dma('sp', cw, self.I('conv_r')[l], key='c0', writes=['cw'])
        s.dma('sp', hp, self.I('headp')[l], key='c0', writes=['hp'])
        s.dma('sp', onw, self.I('onw')[l], key='c0', writes=['onw'])
        self.ts(onw, onw, 128.0 ** 0.5, None, ALU.mult, reads=['onw'], writes=['onw'])
        self.act(negA, hp[:, 0:8], AF.Exp, reads=['hp'], writes=['negA'])
        self.ts(negA, negA, -1.0, None, ALU.mult, reads=['negA'], writes=['negA'])
        wba = ar.alloc(8 * 16, BF16).rearrange("p (k c) -> p k c", k=8)
        s.dma('pool', wba, self.I('w_in_r')[l][:, :, OFF_BA:OFF_BA + 16], key='wba', writes=['wba'])
        a8 = lambda: ar.alloc(256).rearrange("p (i h) -> p i h", h=8)
        BETA, NB, Gs, EG, ED, BEG = a8(), a8(), a8(), a8(), a8(), a8()
        GLB = ar.alloc(512).rearrange("p (c h) -> p c h", h=8)
        m_small = ar.mark()
        GG = ar.alloc(512).rearrange("p (i c) -> p i c", c=16)
        ba = ar.alloc(512).rearrange("p (i c) -> p i c", c=16)
        for i in range(32):
            for kc in range(8):
                self.mm(ps[0][:, i * 16:(i + 1) * 16], xT[:, kc, i * 128:(i + 1) * 128], wba[:, kc, :], kc == 0, kc == 7,
                        reads=['wba'], writes=[('ps', 0)])
        self.copy('dve', ba, ps[0][:, :].rearrange("p (i c) -> p i c", c=16), reads=[('ps', 0)], writes=['ba'])
        bc = lambda v: v.unsqueeze(1).to_broadcast([128, 32, 8])
        self.act(BETA, ba[:, :, 0:8], AF.Sigmoid, reads=['ba'], writes=['BETA'])
        self.ts(NB, BETA, -1.0, None, ALU.mult, reads=['BETA'], writes=['NB'])
        self.tt(Gs, ba[:, :, 8:16], bc(hp[:, 8:16]), ALU.add, reads=['ba', 'hp'], writes=['Gs'])
        self.act(Gs, Gs, AF.Exp, reads=['Gs'], writes=['Gs'])
        self.act(Gs, Gs, AF.Ln, bias=1.0, reads=['Gs'], writes=['Gs'])
        self.tt(Gs, Gs, bc(negA), ALU.mult, reads=['Gs', 'negA'], writes=['Gs'])
        for i in range(32):
            self.mm(ps[1][:, i * 16:i * 16 + 8], U, Gs[:, i, :], True, True, reads=['Gs'], writes=[('ps', 1)])
            self.mm(ps[1][:, i * 16 + 8:i * 16 + 16], BD, Gs[:, i, :], True, True, reads=['Gs'], writes=[('ps', 1)])
            self.mm(ps[2][:, (2 * i) * 8:(2 * i) * 8 + 8], H0, Gs[:, i, :], True, True, reads=['Gs'], writes=[('ps', 2)])
            self.mm(ps[2][:, (2 * i + 1) * 8:(2 * i + 1) * 8 + 8], H1, Gs[:, i, :], True, True, reads=['Gs'], writes=[('ps', 2)])
        self.copy('dve', GG, ps[1][:, :].rearrange("p (i c) -> p i c", c=16), reads=[('ps', 1)], writes=['GG'])
        self.act(GLB, ps[2][:, :].rearrange("p (c h) -> p c h", h=8), AF.Exp, reads=[('ps', 2)], writes=['GLB'])
        self.act(EG, GG[:, :, 0:8], AF.Exp, reads=['GG'], writes=['EG'])
        self.tt(ED, GG[:, :, 8:16], GG[:, :, 0:8], ALU.subtract, reads=['GG'], writes=['ED'])
        self.act(ED, ED, AF.Exp, reads=['ED'], writes=['ED'])
        self.tt(BEG, BETA, EG, ALU.mult, reads=['BETA', 'EG'], writes=['BEG'])

        s.barrier()
        ar.release(m_small)
        bankrot = itertools.cycle(range(2, 8))
        w_in_r = self.I('w_in_r')
        evrot = itertools.cycle(('act', 'dve'))
        m4 = lambda: [ar.alloc(128) for _ in range(4)]

        def make_bufs(hs):
            B = dict(hs=hs)
            B['wu'] = ar.alloc(8 * 512, BF16).rearrange("p (k c) -> p k c", k=8)
            B['S'] = ar.alloc(128)
            B['pre'] = [ar.alloc(3 + 512) for _ in range(3)]
            B['hist'] = [ar.alloc(4) for _ in range(3)]
            B['cv'] = [ar.alloc(512) for _ in range(3)]
            B['sets'] = [dict(u0=m4(), wT=m4(), qkD=m4(), Kdec=m4(), QTn=ar.alloc(512),
                              zs=ar.alloc(512).rearrange("p (a e) -> p a e", a=4), idx=k) for k in range(2)]
            for nm in ('gU', 'Ds', 'DTi', 'Za', 'Ya', 'TTa', 'Kbg', 'Vb'):
                B[nm] = m4()
            B['ub'], B['osb'], B['onb'] = ar.alloc(128), ar.alloc(128), ar.alloc(128)
            B['ssq'], B['rn1'] = ar.alloc(1), ar.alloc(1)
            B['ybst'] = [ar.alloc(512, BF16)] * 2
            return B

        def stage(mms, evs):
            b = next(bankrot)
            for pp in range(4):
                mms(pp, ps[b][:, pp * 128:(pp + 1) * 128], ('ps', b))
            for pp in range(4):
                evs(pp, ps[b][:, pp * 128:(pp + 1) * 128], ('ps', b))

        def prep(h, sg, st, B):
            hs = B['hs']
            K = lambda *a: (hs,) + a
            t0 = sg * 512
            k_ = st['idx']
            W = B['wu']
            wk = K('wu')
            pre, cv, hist = B['pre'], B['cv'], B['hist']
            gU, Ds, DTi, Kbg, Vb = B['gU'], B['Ds'], B['DTi'], B['Kbg'], B['Vb']
            for X in range(3):
                bank = X % 2
                for kc in range(8):
                    self.mm(ps[bank][:, :], W[:, kc, X * 128:(X + 1) * 128], xT[:, kc, t0:t0 + 512], kc == 0, kc == 7,
                            reads=[wk], writes=[('ps', bank)])
                if sg > 0:
                    self.copy('dve', pre[X][:, 0:3], hist[X][:, 0:3], reads=[K('hist', X)], writes=[K('pre', X)])
                else:
                    self.s.op('dve', lambda e, X=X: e.memset(pre[X][:, 0:3], 0.0), writes=[K('pre', X)])
                self.copy('act', pre[X][:, 3:515], ps[bank][:, :], reads=[('ps', bank)], writes=[K('pre', X)])
                wc = lambda k: cw[:, h * 12 + X * 4 + k:h * 12 + X * 4 + k + 1]
                self.ts(cv[X], pre[X][:, 0:512], wc(0), None, ALU.mult, reads=[K('pre', X)], writes=[K('cv', X)])
                for k in range(1, 4):
                    self.stt(cv[X], pre[X][:, k:k + 512], wc(k), cv[X], ALU.mult, ALU.add,
                             reads=[K('pre', X), K('cv', X)], writes=[K('cv', X)])
                self.copy('dve', hist[X][:, 0:3], pre[X][:, 512:515], reads=[K('pre', X)], writes=[K('hist', X)])
                self.act(cv[X], cv[X], AF.Silu, reads=[K('cv', X)], writes=[K('cv', X)])
                yield
            for X in range(2):
                sqb = pre[X][:, 3:515]
                self.act(sqb, cv[X], AF.Square, scale=(128.0 ** 0.5 if X == 0 else 1.0), reads=[K('cv', X)], writes=[K('pre', X)])
                self.mm(ps[X][:, :], ones, sqb, True, True, reads=[K('pre', X)], writes=[('ps', X)])
                self.act(sqb, ps[X][:, :], AF.Sqrt, bias=self.cst[:, X:X + 1], reads=[('ps', X)], writes=[K('pre', X)])
                self.s.op('dve', lambda e, sqb=sqb: e.reciprocal(out=sqb, in_=sqb), reads=[K('pre', X)], writes=[K('pre', X)])
                if X == 0:
                    self.tt(st['QTn'], cv[0], sqb, ALU.mult, reads=[K('cv', 0), K('pre', 0)], writes=[K('QTn', k_)])
                else:
                    self.tt(cv[1], cv[1], sqb, ALU.mult, reads=[K('cv', 1), K('pre', 1)], writes=[K('cv', 1)])
                yield
            KTn, VTs, QTn = cv[1], cv[2], st['QTn']
            b = next(bankrot)
            for kc in range(8):
                self.mm(ps[b][:, :], W[:, kc, 384:512], xT[:, kc, t0:t0 + 512], kc == 0, kc == 7, reads=[wk], writes=[('ps', b)])
            self.act(st['zs'], ps[b][:, :].rearrange("p (a e) -> p a e", a=4), AF.Silu, reads=[('ps', b)], writes=[K('zs', k_)])
            yield
            ti = lambda pp: sg * 4 + pp
            col = lambda A_, pp: A_[:, ti(pp), h:h + 1]
            pc = lambda pp: slice(pp * 128, (pp + 1) * 128)
            stage(lambda pp, q, bk: self.tr(q, KTn[:, pc(pp)], reads=[K('cv', 1)], writes=[bk]),
                  lambda pp, q, bk: (self.ts(Kbg[pp], q, col(BEG, pp), None, ALU.mult, reads=[bk], writes=[K('Kbg', pp)]),
                                     self.act(st['Kdec'][pp], q, AF.Identity, scale=col(ED, pp), reads=[bk], writes=[K('Kdec', k_, pp)])))
            yield
            stage(lambda pp, q, bk: self.tr(q, VTs[:, pc(pp)], reads=[K('cv', 2)], writes=[bk]),
                  lambda pp, q, bk: self.ts(Vb[pp], q, col(BETA, pp), None, ALU.mult, reads=[bk], writes=[K('Vb', pp)]))
            yield
            for pp in range(4):
                self.ts(gU[pp], U, col(Gs, pp), None, ALU.mult, writes=[K('gU', pp)])
            stage(lambda pp, q, bk: (self.mm(q, gU[pp], Ls, True, False, reads=[K('gU', pp)], writes=[bk]),
                                     self.mm(q, ident, MS, False, True, writes=[bk])),
                  lambda pp, q, bk: self.act(Ds[pp], q, AF.Exp, reads=[bk], writes=[K('Ds', pp)]))
            yield
            stage(lambda pp, q, bk: (self.mm(q, Ls, gU[pp], True, False, reads=[K('gU', pp)], writes=[bk]),
                                     self.mm(q, ident, MIT, False, True, writes=[bk])),
                  lambda pp, q, bk: self.act(DTi[pp], q, AF.Exp, reads=[bk], writes=[K('DTi', pp)]))
            yield
            stage(lambda pp, q, bk: self.mm(q, KTn[:, pc(pp)], QTn[:, pc(pp)], True, True, reads=[K('cv', 1), K('QTn', k_)], writes=[bk]),
                  lambda pp, q, bk: self.tt(st['qkD'][pp], q, DTi[pp], ALU.mult, reads=[bk, K('DTi', pp)], writes=[K('qkD', k_, pp)]))
            yield
            Z, Y, TT = [B['Za'], gU], [B['Ya'], Ds], [B['TTa'], DTi]
            Zk = [lambda pp: K('Za', pp), lambda pp: K('gU', pp)]
            Yk = [lambda pp: K('Ya', pp), lambda pp: K('Ds', pp)]
            TTk = [lambda pp: K('TTa', pp), lambda pp: K('DTi', pp)]
            stage(lambda pp, q, bk: self.mm(q, KTn[:, pc(pp)], KTn[:, pc(pp)], True, True, reads=[K('cv', 1)], writes=[bk]),
                  lambda pp, q, bk: self.stt(Z[0][pp], q, col(NB, pp), Ds[pp], ALU.mult, ALU.mult,
                                             reads=[bk, K('Ds', pp)], writes=[Zk[0](pp)]))
            yield
            stage(lambda pp, q, bk: self.tr(q, Z[0][pp], reads=[Zk[0](pp)], writes=[bk]),
                  lambda pp, q, bk: (self.copy('act', Y[0][pp], q, reads=[bk], writes=[Yk[0](pp)]),
                                     self.tt(TT[0][pp], q, ident, ALU.add, reads=[bk], writes=[TTk[0](pp)])))
            yield
            cur = 0
            for k in range(1, 6):
                nxt = 1 - cur
                stage(lambda pp, q, bk: self.mm(q, Y[cur][pp], Z[cur][pp], True, True, reads=[Yk[cur](pp), Zk[cur](pp)], writes=[bk]),
                      lambda pp, q, bk: self.copy(next(evrot), Z[nxt][pp], q, reads=[bk], writes=[Zk[nxt](pp)]))
                if k < 5:
                    stage(lambda pp, q, bk: self.mm(q, Z[cur][pp], Y[cur][pp], True, True, reads=[Yk[cur](pp), Zk[cur](pp)], writes=[bk]),
                          lambda pp, q, bk: self.copy(next(evrot), Y[nxt][pp], q, reads=[bk], writes=[Yk[nxt](pp)]))
                yield
                stage(lambda pp, q, bk: self.mm(q, Z[nxt][pp], TT[cur][pp], True, True, reads=[Zk[nxt](pp), TTk[cur](pp)], writes=[bk]),
                      lambda pp, q, bk: self.tt(TT[nxt][pp], q, TT[cur][pp], ALU.add, reads=[bk, TTk[cur](pp)], writes=[TTk[nxt](pp)]))
                cur = nxt
                yield
            TTf, TTfk = TT[cur], TTk[cur]
            stage(lambda pp, q, bk: self.mm(q, TTf[pp], Vb[pp], True, True, reads=[TTfk(pp), K('Vb', pp)], writes=[bk]),
                  lambda pp, q, bk: self.copy(next(evrot), st['u0'][pp], q, reads=[bk], writes=[K('u0', k_, pp)]))
            yield
            stage(lambda pp, q, bk: self.mm(q, Kbg[pp], TTf[pp], True, True, reads=[TTfk(pp), K('Kbg', pp)], writes=[bk]),
                  lambda pp, q, bk: self.copy(next(evrot), st['wT'][pp], q, reads=[bk], writes=[K('wT', k_, pp)]))
            yield

        def scan(h, sg, st, B, ysl):
            hs = B['hs']
            K = lambda *a: (hs,) + a
            t0 = sg * 512
            k_ = st['idx']
            Sst, ub, osb, onb, ssq, rn1, ybst = B['S'], B['ub'], B['osb'], B['onb'], B['ssq'], B['rn1'], B['ybst']
            for pp in range(4):
                i = sg * 4 + pp
                for c in range(2):
                    rows = slice(64 * c, 64 * c + 64)
                    b1 = next(bankrot)
                    self.mm(ps[b1][rows, 0:128], st['wT'][pp][:, 64 * c:64 * c + 64], Sst, True, True,
                            reads=[K('wT', k_, pp), K('S')], writes=[('ps', b1)])
                    self.mm(ps[b1][rows, 128:256], st['QTn'][:, pp * 128 + 64 * c:pp * 128 + 64 * c + 64], Sst, True, True,
                            reads=[K('QTn', k_), K('S')], writes=[('ps', b1)])
                    self.tt(ub[rows, :], st['u0'][pp][rows, :], ps[b1][rows, 0:128], ALU.subtract,
                            reads=[('ps', b1), K('u0', k_, pp)], writes=[K('ub')])
                    self.act(osb[rows, :], ps[b1][rows, 128:256], AF.Identity, scale=EG[rows, i, h:h + 1],
                             reads=[('ps', b1)], writes=[K('osb')])
                    yield
                    b2 = next(bankrot)
                    self.mm(ps[b2][:, 0:128], st['Kdec'][pp][rows, :], ub[rows, :], True, True,
                            reads=[K('ub'), K('Kdec', k_, pp)], writes=[('ps', b2)])
                    self.stt(Sst, Sst, GLB[:, 2 * i + c, h:h + 1], ps[b2][:, 0:128], ALU.mult, ALU.add,
                             reads=[('ps', b2), K('S')], writes=[K('S')])
                    yield
                b3 = next(bankrot)
                self.mm(ps[b3][:, 0:128], st['qkD'][pp], ub, True, True, reads=[K('ub'), K('qkD', k_, pp)], writes=[('ps', b3)])
                self.tt(osb, osb, ps[b3][:, 0:128], ALU.add, reads=[('ps', b3), K('osb')], writes=[K('osb')])
                self.s.op('dve', lambda e, ssq=ssq: e.memset(ssq, 0.0), writes=[K('ssq')])
                self.act(onb, osb, AF.Square, accum_out=ssq, reads=[K('osb'), K('ssq')], writes=[K('onb'), K('ssq')])
                self.act(rn1, ssq, AF.Sqrt, bias=self.cst[:, 0:1], reads=[K('ssq')], writes=[K('rn1')])
                self.s.op('dve', lambda e, rn1=rn1: e.reciprocal(out=rn1, in_=rn1), reads=[K('rn1')], writes=[K('rn1')])
                self.stt(onb, osb, rn1[:, 0:1], onw, ALU.mult, ALU.mult, reads=[K('osb'), K('rn1')], writes=[K('onb')])
                b4 = next(bankrot)
                self.tr(ps[b4][:, 0:128], onb, reads=[K('onb')], writes=[('ps', b4)])
                self.tt(ybst[ysl][:, pp * 128:(pp + 1) * 128], ps[b4][:, 0:128], st['zs'][:, pp, :], ALU.mult,
                        reads=[('ps', b4), K('zs', k_)], writes=[K('ybst')])
                yield
            self.s.dma('sp', self.yb_d[h][:, t0:t0 + 512], ybst[ysl], key=f'ybst{hs}', reads=[K('ybst')], writes=[('yb_d', h, sg)])

        def merged(g1, g2):
            gens = [g for g in (g1, g2) if g is not None]
            while gens:
                for g in list(gens):
                    try:
                        next(g)
                        yield
                    except StopIteration:
                        gens.remove(g)

        def chain(h, B):
            hs = B['hs']
            s.dma('pool', B['wu'], w_in_r[l][:, :, OFF_DN + h * 512:OFF_DN + (h + 1) * 512], key=f'dwu{hs}', writes=[(hs, 'wu')])
            self.s.op('dve', lambda e: e.memset(B['S'], 0.0), reads=[(hs, 'S')], writes=[(hs, 'S')])
            nsg = 8 if DN_LIMIT >= 3 else 1
            yield from prep(h, 0, B['sets'][0], B)
            for sg in range(nsg):
                nxt = prep(h, sg + 1, B['sets'][(sg + 1) % 2], B) if sg + 1 < nsg else None
                yield from merged(scan(h, sg, B['sets'][sg % 2], B, sg % 2), nxt)

        NPAR = 2
        bufs = [make_bufs(k) for k in range(NPAR)]
        heads = list(range(8 if DN_LIMIT >= 9 else (NPAR if DN_LIMIT >= 1 else 0)))
        for h0 in range(0, len(heads), NPAR):
            gens = [chain(h0 + k, bufs[k]) for k in range(NPAR) if h0 + k < len(heads)]
            for _ in merged(*gens) if len(gens) == 2 else gens[0]:
                pass
        ar.release(m)

    def ln_tile(self, res, res_key, add_views, add_keys, gam, bet, out, out_key, tbufs, statss, ls):
        tbuf, stats = tbufs[ls], statss[ls]
        kt, kst = ('ln_t', ls), ('ln_st', ls)
        s1, nm, ss, rs = (stats[:, j:j + 1] for j in range(4))
        for hf in range(2):
            cs = slice(hf * 512, (hf + 1) * 512)
            self.stt(tbuf[:, cs], res[:, cs], ALPHA, add_views[hf], ALU.mult, ALU.add,
                     reads=[res_key, add_keys[hf]], writes=[kt])
        self.s.op('dve', lambda e: e.memset(stats[:, 0:4], 0.0), writes=[kst])
        self.act(out, tbuf, AF.Identity, accum_out=s1, reads=[kt, kst], writes=[out_key, kst])
        self.act(nm, s1, AF.Identity, scale=-1.0 / DM, reads=[kst], writes=[kst])
        self.act(out, tbuf, AF.Square, bias=nm, accum_out=ss, reads=[kt, kst], writes=[out_key, kst])
        self.act(rs, ss, AF.Sqrt, bias=self.cst[:, 2:3], scale=1.0 / DM, reads=[kst], writes=[kst])
        self.s.op('dve', lambda e: e.reciprocal(out=rs, in_=rs), reads=[kst], writes=[kst])
        self.ts(tbuf, tbuf, nm, rs, ALU.add, ALU.mult, reads=[kt, kst], writes=[kt])
        self.tt(tbuf, tbuf, gam, ALU.mult, reads=[kt, 'lnp'], writes=[kt])
        self.tt(out, tbuf, bet, ALU.add, reads=[kt, 'lnp'], writes=[out_key])

    def phase_merge(self, l):
        import itertools
        s, ar, ps, xT = self.s, self.ar, self.ps, self.xT
        m = ar.mark()
        moe = (l % 2 == 1)
        w_in_r = self.I('w_in_r')
        Wga = ar.alloc(8 * DM, BF16).rearrange("p (k c) -> p k c", k=8)
        Wgb = ar.alloc(8 * DM, BF16).rearrange("p (k c) -> p k c", k=8)
        Woa = ar.alloc(4 * DM, BF16).rearrange("p (k c) -> p k c", k=4)
        Wob = ar.alloc(8 * DM, BF16).rearrange("p (k c) -> p k c", k=8)
        Wout = ar.alloc(8 * DM, BF16).rearrange("p (k c) -> p k c", k=8)
        gam, bet = ar.alloc(DM), ar.alloc(DM)
        s.dma('pool', Wga, w_in_r[l][:, :, OFF_GA:OFF_GA + DM], key='mw0', writes=['Wga'])
        s.dma('pool', Woa, self.I('w_oa_r')[l], key='mw1', writes=['Woa'])
        s.dma('pool', Wgb, w_in_r[l][:, :, OFF_GB:OFF_GB + DM], key='mw2', writes=['Wgb'])
        s.dma('pool', Wob, self.I('w_ob_r')[l], key='mw3', writes=['Wob'])
        s.dma('pool', Wout, self.I('w_out_r')[l], key='mw4', writes=['Wout'])
        s.dma('sp', gam, self.I('ln_r')[l][0], key='c0', writes=['lnp'])
        s.dma('sp', bet, self.I('ln_r')[l][1], key='c0', writes=['lnp'])
        TB = 512
        yaT = [ar.alloc(4 * TB, BF16).rearrange("p (k t) -> p k t", k=4)] * 2
        ybT = [ar.alloc(8 * TB, BF16).rearrange("p (k t) -> p k t", k=8) for _ in range(2)]
        mT = ar.alloc(8 * TB, BF16).rearrange("p (k t) -> p k t", k=8)
        sg = [ar.alloc(TB) for _ in range(2)]
        xres = [ar.alloc(DM)] * 2
        tbufs = [ar.alloc(DM) for _ in range(2)]
        xo = [ar.alloc(DM) for _ in range(2)]
        statss = [ar.alloc(8) for _ in range(2)]
        if moe:
            x1Tf = ar.alloc(8 * 128).rearrange("p (k t) -> p k t", k=8)
            wr = ar.alloc(8 * NEXP).rearrange("p (k e) -> p k e", k=8)
            s.dma('sp', wr, self.I('moe_router_r'), key='c0', writes=['wr'])
            L, Lm, mk, E = ar.alloc(8), ar.alloc(8), ar.alloc(8), ar.alloc(8)
            gst = ar.alloc(8)
        src_res = self.I('x') if l == 0 else self.x2_d
        brot = itertools.cycle(range(0, 4))
        ya_v = self.ya_d.rearrange("k p t -> p k t")
        yb_v = self.yb_d.rearrange("k p t -> p k t")
        nblk = S_LEN // TB
        for blk in range(nblk):
            bs = slice(blk * TB, (blk + 1) * TB)
            sl = blk % 2
            s.dma('sp', yaT[sl], ya_v[:, :, bs], key='ya0', writes=[('yaT', 0)])
            s.dma('sp', ybT[sl], yb_v[:, :, bs], key=f'yb{sl}', writes=[('ybT', sl)])
            for oc in range(8):
                ocs = slice(oc * 128, (oc + 1) * 128)
                for br in range(2):
                    Wg, Wo, yT, nk = (Wga, Woa, yaT[sl], 4) if br == 0 else (Wgb, Wob, ybT[sl], 8)
                    wgk, wok, yk = (('Wga', 'Woa', ('yaT', 0)) if br == 0 else ('Wgb', 'Wob', ('ybT', sl)))
                    b1 = next(brot)
                    for kc in range(8):
                        self.mm(ps[b1][:, 0:TB], Wg[:, kc, ocs], xT[:, kc, bs], kc == 0, kc == 7,
                                reads=[wgk, ('xTb', blk)], writes=[('ps', b1)])
                    self.act(sg[br], ps[b1][:, 0:TB], AF.Sigmoid, reads=[('ps', b1)], writes=[('sg', br)])
                    b2 = next(brot)
                    for kc in range(nk):
                        self.mm(ps[b2][:, 0:TB], Wo[:, kc, ocs], yT[:, kc, :], kc == 0, kc == nk - 1,
                                reads=[wok, yk], writes=[('ps', b2)])
                    self.tt(sg[br], sg[br], ps[b2][:, 0:TB], ALU.mult, reads=[('sg', br), ('ps', b2)], writes=[('sg', br)])
                    if br == 1:
                        self.tt(mT[:, oc, :], sg[0], sg[1], ALU.add, reads=[('sg', 0), ('sg', 1)], writes=['mT'])
            for sub in range(TB // 128):
                i = blk * (TB // 128) + sub
                xs = i % 2
                s.dma('sp', xres[xs], src_res[i * 128:(i + 1) * 128, :], key='xr0', writes=[('xres', 0)])
                for hf in range(2):
                    for kc in range(8):
                        self.mm(ps[4 + hf][:, :], mT[:, kc, sub * 128:(sub + 1) * 128], Wout[:, kc, hf * 512:(hf + 1) * 512],
                                kc == 0, kc == 7, reads=['mT', 'Wout'], writes=[('ps', 4 + hf)])
                self.ln_tile(xres[xs], ('xres', 0), [ps[4][:, :], ps[5][:, :]], [('ps', 4), ('ps', 5)], gam, bet,
                             xo[xs], ('xo', xs), tbufs, statss, xs)
                s.dma('sp', self.x1_d[i * 128:(i + 1) * 128, :], xo[xs], key=f'xo{xs}', reads=[('xo', xs)], writes=[('x1_d', i)])
                for hf in range(2):
                    b = 6 + hf
                    for q in range(4):
                        kc = hf * 4 + q
                        self.tr(ps[b][:, q * 128:(q + 1) * 128], xo[xs][:, kc * 128:(kc + 1) * 128], reads=[('xo', xs)], writes=[('ps', b)])
                    self.copy('act' if hf == 0 else 'dve', xT[:, hf * 4:(hf + 1) * 4, i * 128:(i + 1) * 128],
                              ps[b][:, :].rearrange("p (k t) -> p k t", k=4), reads=[('ps', b)], writes=[('xTb', blk)])
                    if moe:
                        self.copy('dve' if hf == 0 else 'act', x1Tf[:, hf * 4:(hf + 1) * 4, :],
                                  ps[b][:, :].rearrange("p (k t) -> p k t", k=4), reads=[('ps', b)], writes=['x1Tf'])
                if moe:
                    for kc in range(8):
                        self.mm(ps[6][:, 0:NEXP], x1Tf[:, kc, :], wr[:, kc, :], kc == 0, kc == 7, reads=['x1Tf', 'wr'], writes=[('ps', 6)])
                    self.copy('dve', L, ps[6][:, 0:NEXP], reads=[('ps', 6)], writes=['L'])
                    m1, m2, den = gst[:, 0:1], gst[:, 1:2], gst[:, 2:3]
                    self.s.op('dve', lambda e, m1=m1: e.tensor_reduce(out=m1, in_=L, axis=AX.X, op=ALU.max), reads=['L'], writes=['gst'])
                    self.ts(Lm, L, m1, None, ALU.subtract, reads=['L', 'gst'], writes=['Lm'])
                    self.ts(mk, Lm, 0.0, None, ALU.is_equal, reads=['Lm'], writes=['mk'])
                    self.stt(mk, mk, -1e30, Lm, ALU.mult, ALU.add, reads=['mk', 'Lm'], writes=['mk'])
                    self.s.op('dve', lambda e, m2=m2: e.tensor_reduce(out=m2, in_=mk, axis=AX.X, op=ALU.max), reads=['mk'], writes=['gst'])
                    self.ts(mk, Lm, m2, None, ALU.is_ge, reads=['Lm', 'gst'], writes=['mk'])
                    self.act(E, Lm, AF.Exp, reads=['Lm'], writes=['E'])
                    self.tt(E, E, mk, ALU.mult, reads=['E', 'mk'], writes=['E'])
                    self.s.op('dve', lambda e, den=den: e.tensor_reduce(out=den, in_=E, axis=AX.X, op=ALU.add), reads=['E'], writes=['gst'])
                    self.s.op('dve', lambda e, den=den: e.reciprocal(out=den, in_=den), reads=['gst'], writes=['gst'])
                    self.ts(self.Gt[:, i, :], E, den, None, ALU.mult, reads=['E', 'gst'], writes=['Gt'])
        ar.release(m)

    def phase_ffn(self, l):
        import itertools
        s, ar, ps, xT = self.s, self.ar, self.ps, self.xT
        m = ar.mark()
        moe = (l % 2 == 1)
        last = (l == DEPTH - 1)
        if moe:
            nexp, nch = NEXP, D_FFE // 128
            gsrc = lambda e, c0, n: self.I('moe_g_r')[e][c0:c0 + n].rearrange("c p k j -> p c (k j)")
            usrc = lambda e, c0, n: self.I('moe_u_r')[e][c0:c0 + n].rearrange("c p k j -> p c (k j)")
            dsrc = lambda e, c0, n: self.I('moe_d')[e][c0 * 128:(c0 + n) * 128, :].rearrange("(c p) n -> p c n", p=128)
        else:
            nexp, nch = 1, D_FF // 128
            gsrc = lambda e, c0, n: self.I('ffn_g_r')[c0:c0 + n].rearrange("c p k j -> p c (k j)")
            usrc = lambda e, c0, n: self.I('ffn_u_r')[c0:c0 + n].rearrange("c p k j -> p c (k j)")
            dsrc = lambda e, c0, n: self.I('ffn_d')[c0 * 128:(c0 + n) * 128, :].rearrange("(c p) n -> p c n", p=128)
        GC = 4
        TBK = 1024
        acc = ar.alloc(8 * DM).rearrange("p (a n) -> p a n", a=8)
        hT = [ar.alloc(GC * TBK, BF16).rearrange("p (c t) -> p c t", c=GC) for _ in range(2)]
        Wg = [ar.alloc(GC * 1024, BF16).rearrange("p (c k j) -> p c k j", c=GC, k=8) for _ in range(2)]
        Wu = [ar.alloc(GC * 1024, BF16).rearrange("p (c k j) -> p c k j", c=GC, k=8) for _ in range(2)]
        Wd = [ar.alloc(GC * DM, BF16).rearrange("p (c n) -> p c n", c=GC) for _ in range(2)]
        sgb = [ar.alloc(512) for _ in range(2)]
        gam, bet = ar.alloc(DM), ar.alloc(DM)
        xres = [ar.alloc(DM) for _ in range(2)]
        tbufs = [ar.alloc(DM) for _ in range(2)]
        xo = [ar.alloc(DM) for _ in range(2)]
        statss = [ar.alloc(8) for _ in range(2)]
        s.dma('sp', gam, self.I('ln_r')[l][2], key='c0', writes=['lnp'])
        s.dma('sp', bet, self.I('ln_r')[l][3], key='c0', writes=['lnp'])
        groups = [(c0, min(GC, nch - c0)) for c0 in range(0, nch, GC)]
        gurot = itertools.cycle([(0, 1), (2, 3)])
        drot = itertools.cycle([4, 5])
        dst = self.y_out if last else self.x2_d
        nslot = 0
        for tb in range(S_LEN // TBK if FFN_LIMIT >= 9 else 1):
            first = True
            for e in range(nexp):
                for (c0, n) in (groups if FFN_LIMIT >= 2 else groups[:1]):
                    sl = nslot % 2
                    nslot += 1
                    s.dma('pool', Wg[sl][:, 0:n].rearrange("p c k j -> p c (k j)"), gsrc(e, c0, n), key=f'fg{sl}', writes=[('Wg', sl)])
                    s.dma('pool', Wu[sl][:, 0:n].rearrange("p c k j -> p c (k j)"), usrc(e, c0, n), key=f'fu{sl}', writes=[('Wu', sl)])
                    s.dma('pool', Wd[sl][:, 0:n], dsrc(e, c0, n), key=f'fd{sl}', writes=[('Wd', sl)])
                    for c in range(n):
                        for hf in range(2):
                            ts_ = slice(tb * TBK + hf * 512, tb * TBK + (hf + 1) * 512)
                            bg, bu = next(gurot)
                            for kc in range(8):
                                self.mm(ps[bg][:, :], Wg[sl][:, c, kc, :], xT[:, kc, ts_], kc == 0, kc == 7,
                                        reads=[('Wg', sl), ('xTb', tb)], writes=[('ps', bg)])
                            for kc in range(8):
                                self.mm(ps[bu][:, :], Wu[sl][:, c, kc, :], xT[:, kc, ts_], kc == 0, kc == 7,
                                        reads=[('Wu', sl), ('xTb', tb)], writes=[('ps', bu)])
                            k2 = (c * 2 + hf) % 2
                            self.act(sgb[k2], ps[bg][:, :], AF.Silu, reads=[('ps', bg)], writes=[('sgb', k2)])
                            self.tt(hT[sl][:, c, hf * 512:(hf + 1) * 512], sgb[k2], ps[bu][:, :], ALU.mult,
                                    reads=[('sgb', k2), ('ps', bu)], writes=[('hT', sl)])
                    for sub in range(8 if FFN_LIMIT >= 1 else 0):
                        i = tb * 8 + sub
                        for hf in range(2):
                            bd = next(drot)
                            for c in range(n):
                                self.mm(ps[bd][:, :], hT[sl][:, c, sub * 128:(sub + 1) * 128], Wd[sl][:, c, hf * 512:(hf + 1) * 512],
                                        c == 0, c == n - 1, reads=[('hT', sl), ('Wd', sl)], writes=[('ps', bd)])
                            av = acc[:, sub, hf * 512:(hf + 1) * 512]
                            ak = ('acc', sub, hf)
                            if moe:
                                gcol = self.Gt[:, i, e:e + 1]
                                if first:
                                    self.ts(av, ps[bd][:, :], gcol, None, ALU.mult, reads=[('ps', bd)], writes=[ak])
                                else:
                                    self.stt(av, ps[bd][:, :], gcol, av, ALU.mult, ALU.add, reads=[('ps', bd), ak], writes=[ak])
                            else:
                                if first:
                                    self.copy('dve', av, ps[bd][:, :], reads=[('ps', bd)], writes=[ak])
                                else:
                                    self.tt(av, av, ps[bd][:, :], ALU.add, reads=[('ps', bd), ak], writes=[ak])
                    first = False
            for sub in range(8 if FFN_LIMIT >= 3 else 0):
                i = tb * 8 + sub
                xs = i % 2
                s.dma('sp', xres[xs], self.x1_d[i * 128:(i + 1) * 128, :], key=f'xr{xs}', writes=[('xres', xs)])
                self.ln_tile(xres[xs], ('xres', xs), [acc[:, sub, 0:512], acc[:, sub, 512:1024]], [('acc', sub, 0), ('acc', sub, 1)],
                             gam, bet, xo[xs], ('xo', xs), tbufs, statss, xs)
                s.dma('sp', dst[i * 128:(i + 1) * 128, :], xo[xs], key=f'xo{xs}', reads=[('xo', xs)], writes=[('dst', i)])
                if not last:
                    for hf in range(2):
                        b = 6 + hf
                        for q in range(4):
                            kc = hf * 4 + q
                            self.tr(ps[b][:, q * 128:(q + 1) * 128], xo[xs][:, kc * 128:(kc + 1) * 128], reads=[('xo', xs)], writes=[('ps', b)])
                        self.copy('act' if hf == 0 else 'dve', xT[:, hf * 4:(hf + 1) * 4, i * 128:(i + 1) * 128],
                                  ps[b][:, :].rearrange("p (k t) -> p k t", k=4), reads=[('ps', b)], writes=[('xTb', tb)])
        ar.release(m)


def _prep_shared(inp):
    f = lambda a: np.ascontiguousarray(a, dtype=np.float32)
    perm = _w_in_perm()
    w_in = inp["w_in"]
    w_in_r = np.stack([w_in[l][:, perm].reshape(8, 128, N_IN).transpose(1, 0, 2) for l in range(DEPTH)])
    cw = inp["conv_w"]
    conv_r = np.stack([cw[l].reshape(4, 3, 8, 128).transpose(3, 2, 1, 0).reshape(128, 96) for l in range(DEPTH)])
    headp = np.stack([np.broadcast_to(np.concatenate([inp["a_log"][l], inp["dt_bias"][l]])[None, :], (128, 16)) for l in range(DEPTH)])
    onw = np.stack([np.broadcast_to(inp["o_norm_w"][l][None, :], (128, 128)) for l in range(DEPTH)])
    kt = lambda w, kc: w.reshape(kc, 128, w.shape[1]).transpose(1, 0, 2)
    w_oa_r = np.stack([kt(inp["w_oa"][l], 4) for l in range(DEPTH)])
    w_ob_r = np.stack([kt(inp["w_ob"][l], 8) for l in range(DEPTH)])
    w_out_r = np.stack([kt(inp["w_out"][l], 8) for l in range(DEPTH)])
    ln_r = np.stack([np.stack([np.broadcast_to(inp[k][l][None, :], (128, DM)) for k in ("ln1_g", "ln1_b", "ln2_g", "ln2_b")]) for l in range(DEPTH)])
    ct = lambda w: w.reshape(8, 128, w.shape[1] // 128, 128).transpose(2, 1, 0, 3)
    shared = {
        "cmat": _const_mats(),
        "biasT": _bias_tables(np.asarray(inp["rel_bias"], np.float32)),
        "w_in_r": w_in_r, "conv_r": conv_r, "headp": headp, "onw": onw,
        "w_oa_r": w_oa_r, "w_ob_r": w_ob_r, "w_out_r": w_out_r, "ln_r": ln_r,
        "ffn_g_r": ct(inp["ffn_w_gate"][0]), "ffn_u_r": ct(inp["ffn_w_up"][0]), "ffn_d": inp["ffn_w_down"][0],
        "moe_router_r": inp["moe_router"][0].reshape(8, 128, NEXP).transpose(1, 0, 2),
        "moe_g_r": np.stack([ct(inp["moe_w_gate"][0][e]) for e in range(NEXP)]),
        "moe_u_r": np.stack([ct(inp["moe_w_up"][0][e]) for e in range(NEXP)]),
        "moe_d": inp["moe_w_down"][0],
    }
    return {k: f(v) for k, v in shared.items()}


def kernel(**inputs):
    inp = {k: np.asarray(v) for k, v in inputs.items()}
    shared = _prep_shared(inp)
    prog = Prog()
    x = np.ascontiguousarray(inp["x"], dtype=np.float32)
    shared = {k: v for k, v in shared.items() if k in prog.ins}
    in_maps = [dict(shared, x=x[b]) for b in range(8)]
    res = run_bass_kernel_spmd(prog.nc, in_maps, core_ids=list(range(8)))
    return np.stack([np.asarray(res.results[b]["y"], dtype=np.float32) for b in range(8)])
```

```python
import numpy as np
from contextlib import ExitStack
import concourse.bass as bass
import concourse.mybir as mybir
from concourse.bass_utils import run_bass_kernel_spmd

F32 = mybir.dt.float32
BF16 = mybir.dt.bfloat16
AF = mybir.ActivationFunctionType
ALU = mybir.AluOpType
AX = mybir.AxisListType

S_LEN = 4096
DM = 1024
NTILE = 32
N_IN = 10768
DEPTH = 2
D_FF = 2816
D_FFE = 3584
NEXP = 8
ALPHA = (2 * DEPTH) ** 0.25
LN_EPS = 1e-5
RMS_EPS = 1e-6
NEG = -30000.0
ENGS = ('pe', 'dve', 'act', 'pool', 'sp')
import os
ATT_LIMIT = int(os.environ.get('ATT_LIMIT', '9'))
DN_LIMIT = int(os.environ.get('DN_LIMIT', '9'))
FFN_LIMIT = int(os.environ.get('FFN_LIMIT', '9'))
SKIP_MIXER = int(os.environ.get('SKIP_MIXER', '0'))


class Sched:
    def __init__(self, nc, ctx):
        self.nc = nc
        self.ctx = ctx
        self.ops = {e: [] for e in ENGS}
        self.cnt = {e: 0 for e in ENGS}
        self.seen = {e: {} for e in ENGS}
        self.res = {}
        self.sems = {}
        for e in ('pe', 'dve', 'act', 'pool'):
            self.sems[e] = ctx.enter_context(nc.semaphore("sem_" + e))
        self.dcnt = {}

    def _sem(self, key):
        if key not in self.sems:
            self.sems[key] = self.ctx.enter_context(self.nc.semaphore("semd_" + str(key)))
            self.dcnt[key] = 0
        return self.sems[key]

    def _deps(self, eng, reads, writes):
        need = {}

        def add(k, v):
            if need.get(k, 0) < v:
                need[k] = v
        for r in reads:
            st = self.res.get(r)
            if st and st['w']:
                add(*st['w'])
        for w in writes:
            st = self.res.get(w)
            if st:
                if st['w'] and st['w'][0] != eng:
                    add(*st['w'])
                for k, v in st['r'].items():
                    if k != eng:
                        add(k, v)
        out = []
        for k, v in need.items():
            if self.seen[eng].get(k, 0) < v:
                self.seen[eng][k] = v
                out.append((k, v))
        return out

    def op(self, eng, fn, reads=(), writes=()):
        psr = [r for r in reads if isinstance(r, tuple) and r[0] == 'ps' and r not in writes]
        if psr:
            writes = list(writes) + psr
        waits = self._deps(eng, reads, writes)
        self.cnt[eng] += 1
        n = self.cnt[eng]
        self.ops[eng].append((waits, fn, eng, 1))
        for r in reads:
            st = self.res.setdefault(r, {'w': None, 'r': {}})
            st['r'][eng] = n
        for w in writes:
            self.res[w] = {'w': (eng, n), 'r': {}}
        return n

    def dma(self, eng, out, in_, key, reads=(), writes=(), **kw):
        self._sem(key)
        waits = self._deps(eng, reads, writes)
        self.dcnt[key] += 16
        n = self.dcnt[key]
        self.ops[eng].append((waits, lambda e: e.dma_start(out=out, in_=in_, **kw), key, 16))
        for r in reads:
            st = self.res.setdefault(r, {'w': None, 'r': {}})
            st['r'][key] = n
        for w in writes:
            self.res[w] = {'w': (key, n), 'r': {}}
        return n

    def dma_fn(self, eng, fn, key, reads=(), writes=()):
        self._sem(key)
        waits = self._deps(eng, reads, writes)
        self.dcnt[key] += 16
        n = self.dcnt[key]
        self.ops[eng].append((waits, fn, key, 16))
        for r in reads:
            st = self.res.setdefault(r, {'w': None, 'r': {}})
            st['r'][key] = n
        for w in writes:
            self.res[w] = {'w': (key, n), 'r': {}}
        return n

    def barrier(self):
        cur = {e: self.cnt[e] for e in ('pe', 'dve', 'act', 'pool')}
        cur.update(self.dcnt)
        for e in ENGS:
            waits = []
            for k, v in cur.items():
                if k != e and v > 0 and self.seen[e].get(k, 0) < v:
                    self.seen[e][k] = v
                    waits.append((k, v))
            if waits:
                self.ops[e].append((waits, None, None, 0))
        self.res = {}

    def emit(self):
        nc = self.nc
        sems = self.sems
        ops = self.ops

        def run(e, lst):
            for waits, fn, sk, inc in lst:
                for k, v in waits:
                    e.wait_ge(sems[k], v)
                if fn is not None:
                    fn(e).then_inc(sems[sk], inc)
        with nc.Block() as block:
            @block.tensor
            def _(e):
                run(e, ops['pe'])

            @block.vector
            def _(e):
                run(e, ops['dve'])

            @block.scalar
            def _(e):
                run(e, ops['act'])

            @block.gpsimd
            def _(e):
                run(e, ops['pool'])

            @block.sync
            def _(e):
                run(e, ops['sp'])


class Arena:
    def __init__(self, t, nwords):
        self.t = t
        self.n = nwords
        self.off = 0

    def alloc(self, nelem, dtype=F32):
        nw = nelem if dtype == F32 else (nelem + 1) // 2
        assert self.off + nw <= self.n, f"arena overflow {self.off}+{nw}>{self.n}"
        ap = self.t[:, self.off:self.off + nw]
        self.off += nw
        return ap if dtype == F32 else ap.bitcast(dtype)

    def mark(self):
        return self.off

    def release(self, m):
        self.off = m


def _w_in_perm():
    cols = []
    for hs in range(4):
        for g in range(3):
            h = g * 4 + hs
            for base in (0, 1536, 3072):
                cols.extend(range(base + h * 128, base + (h + 1) * 128))
    for h in range(8):
        for base in (4608, 5632, 6656, 7680):
            cols.extend(range(base + h * 128, base + (h + 1) * 128))
    cols.extend(range(8704, 8720))
    cols.extend(range(8720, 10768))
    assert len(cols) == N_IN
    return np.asarray(cols)


OFF_DN = 12 * 384
OFF_BA = OFF_DN + 8 * 512
OFF_GA = OFF_BA + 16
OFF_GB = OFF_GA + 1024


def _t5_bucket(dist):
    dist = np.asarray(dist, np.int64)
    d = np.maximum(dist, 1).astype(np.float32)
    large = 16 + (np.log(d / np.float32(16)) / np.float32(np.log(2048 / 16)) * np.float32(16)).astype(np.int32)
    large = np.minimum(large, 31)
    return np.where(dist < 16, dist, large)


def _bias_tables(rel_bias):
    out = np.full((128, 12, 256), NEG, np.float32)
    kj = np.arange(128)[:, None]
    qi = np.arange(128)[None, :]
    for h in range(12):
        d = (1, 4, 16)[h // 4]
        delta0 = qi - kj
        b0 = rel_bias[_t5_bucket(np.maximum(delta0, 0) * d), h]
        out[:, h, 0:128] = np.where(delta0 >= 0, b0, NEG)
        delta1 = qi + 128 - kj
        b1 = rel_bias[_t5_bucket(delta1 * d), h]
        out[:, h, 128:256] = np.where(delta1 <= 128, b1, NEG)
    return out


def _const_mats():
    r = np.arange(128)[:, None]
    c = np.arange(128)[None, :]
    same = (r // 64) == (c // 64)
    m = np.zeros((128, 9, 128), np.float32)
    m[:, 0] = (r == c)
    m[:, 1] = 1.0
    m[:, 2] = (r <= c) & same
    m[:, 3] = (r > c)
    m[:, 4] = np.where((r > c) & same, 0.0, NEG)
    m[:, 5] = np.where((c >= r) & same, 0.0, NEG)
    m[:, 6] = same
    m[:, 7] = (r < 64) * np.ones((1, 128))
    m[:, 8] = (r >= 64) * np.ones((1, 128))
    return m


class Prog:
    def __init__(self, dbg=(), stop_after=None):
        self.dbg = set(dbg)
        self.stop_after = stop_after
        self.nc = nc = bass.Bass("TRN2", target_bir_lowering=False)
        nc.allow_low_precision("bf16 matmul operands with fp32 PSUM accumulation")
        self.ctx = ExitStack()
        with self.ctx:
            self._build()

    def I(self, name):
        if name not in self.ins:
            self.ins[name] = self.nc.dram_tensor(name, list(self.in_shapes[name]), F32, kind="ExternalInput").ap()
        return self.ins[name]

    def mm(self, out, lhsT, rhs, start, stop, reads=(), writes=()):
        self.s.op('pe', lambda e: e.matmul(out, lhsT=lhsT, rhs=rhs, start=start, stop=stop), reads=reads, writes=writes)

    def tr(self, out, in_, reads=(), writes=()):
        ident = self.cm[:, 0, :]
        self.s.op('pe', lambda e: e.transpose(out, in_, ident), reads=reads, writes=writes)

    def copy(self, eng, out, in_, reads=(), writes=()):
        if eng == 'act':
            self.s.op('act', lambda e: e.activation(out=out, in_=in_, func=AF.Copy), reads=reads, writes=writes)
        else:
            self.s.op(eng, lambda e: e.tensor_copy(out=out, in_=in_), reads=reads, writes=writes)

    def act(self, out, in_, func, reads=(), writes=(), **kw):
        self.s.op('act', lambda e: e.activation(out=out, in_=in_, func=func, **kw), reads=reads, writes=writes)

    def tt(self, out, in0, in1, op, reads=(), writes=(), eng='dve'):
        self.s.op(eng, lambda e: e.tensor_tensor(out=out, in0=in0, in1=in1, op=op), reads=reads, writes=writes)

    def ts(self, out, in0, s1, s2, op0, op1=None, reads=(), writes=(), eng='dve', **kw):
        if op1 is None:
            self.s.op(eng, lambda e: e.tensor_scalar(out=out, in0=in0, scalar1=s1, scalar2=None, op0=op0, **kw), reads=reads, writes=writes)
        else:
            self.s.op(eng, lambda e: e.tensor_scalar(out=out, in0=in0, scalar1=s1, scalar2=s2, op0=op0, op1=op1, **kw), reads=reads, writes=writes)

    def stt(self, out, in0, scalar, in1, op0, op1, reads=(), writes=(), eng='dve', **kw):
        self.s.op(eng, lambda e: e.scalar_tensor_tensor(out=out, in0=in0, scalar=scalar, in1=in1, op0=op0, op1=op1, **kw), reads=reads, writes=writes)

    def dump(self, name, ap_sb, shape, dt, key, reads):
        o = self.nc.dram_tensor("dbg_" + name, list(shape), dt, kind="ExternalOutput").ap()
        self.s.dma('sp', o, ap_sb, key='dbg', reads=reads, writes=[('dbgout', name)])
        self.dbg_names.append(name)

    def _build(self):
        nc, ctx = self.nc, self.ctx
        self.s = s = Sched(nc, ctx)
        self.dbg_names = []
        self.in_shapes = {
            "x": [S_LEN, DM], "cmat": [128, 9, 128], "biasT": [128, 12, 256], "w_in_r": [DEPTH, 128, 8, N_IN],
            "conv_r": [DEPTH, 128, 96], "headp": [DEPTH, 128, 16], "onw": [DEPTH, 128, 128],
            "w_oa_r": [DEPTH, 128, 4, DM], "w_ob_r": [DEPTH, 128, 8, DM], "w_out_r": [DEPTH, 128, 8, DM],
            "ln_r": [DEPTH, 4, 128, DM], "ffn_g_r": [D_FF // 128, 128, 8, 128], "ffn_u_r": [D_FF // 128, 128, 8, 128],
            "ffn_d": [D_FF, DM], "moe_router_r": [128, 8, NEXP], "moe_g_r": [NEXP, D_FFE // 128, 128, 8, 128],
            "moe_u_r": [NEXP, D_FFE // 128, 128, 8, 128], "moe_d": [NEXP, D_FFE, DM],
        }
        self.ins = {}
        self.y_out = nc.dram_tensor("y", [S_LEN, DM], F32, kind="ExternalOutput").ap()
        self.x1_d = nc.dram_tensor("x1_scr", [S_LEN, DM], F32).ap()
        self.x2_d = nc.dram_tensor("x2_scr", [S_LEN, DM], F32).ap()
        self.ya_d = nc.dram_tensor("ya_scr", [4, 128, S_LEN], BF16).ap()
        self.yb_d = nc.dram_tensor("yb_scr", [8, 128, S_LEN], BF16).ap()
        NW = 53200
        big = ctx.enter_context(nc.sbuf_tensor("arena", [128, NW], F32))
        self.ar = ar = Arena(big, NW)
        self.ps = [ctx.enter_context(nc.psum_tensor(f"ps{i}", [128, 512], F32)) for i in range(8)]
        self.cm = ar.alloc(9 * 128).rearrange("p (k c) -> p k c", k=9)
        self.ones_bf = ar.alloc(128, BF16)
        s.dma('sp', self.cm, self.I('cmat'), key='c0', writes=['cm'])
        self.copy('dve', self.ones_bf, self.cm[:, 1, :], reads=['cm'], writes=['ones_bf'])
        self.Gt = ar.alloc(32 * NEXP).rearrange("p (i e) -> p i e", e=NEXP)
        self.cst = ar.alloc(4)
        self.m_xT = ar.mark()
        self.xT = ar.alloc(8 * S_LEN, BF16).rearrange("p (k t) -> p k t", k=8)
        for j, v in enumerate((128.0 * RMS_EPS, RMS_EPS, LN_EPS, 0.0)):
            s.op('dve', lambda e, j=j, v=v: e.memset(self.cst[:, j:j + 1], v), writes=['cst'])
        s.barrier()

        self.phase_make_xT(self.I('x'))
        s.barrier()
        if self.stop_after == 'xT':
            self.dump("xT", self.xT[:, 0, :], [128, S_LEN], BF16, 'dbg', [])
            self.finish()
            return
        for l in range(DEPTH):
            if SKIP_MIXER:
                self.phase_ffn(l)
                s.barrier()
                break
            self.phase_attention(l)
            s.barrier()
            if self.stop_after == ('att', l):
                break
            self.phase_deltanet(l)
            s.barrier()
            if ('yb', l) in self.dbg:
                for h in range(8):
                    self.dump(f"yb{l}_{h}", self.yb_d[h], [128, S_LEN], BF16, 'dbg', [])
            if self.stop_after == ('dn', l):
                break
            self.phase_merge(l)
            s.barrier()
            if ('x1', l) in self.dbg:
                self.dump(f"x1_{l}", self.x1_d, [S_LEN, DM], F32, 'dbg', [])
            if self.stop_after == ('mix', l):
                break
            self.phase_ffn(l)
            s.barrier()
            if ('x2', l) in self.dbg:
                self.dump(f"x2_{l}", self.x2_d if l == 0 else self.y_out, [S_LEN, DM], F32, 'dbg', [])
            if self.stop_after == ('ffn', l):
                break
        self.finish()

    def finish(self):
        s = self.s
        s.barrier()
        s.emit()

    def transposes_to_xT(self, src, src_key, i, bank0, eng_pair=('act', 'dve'), xf32=None):
        for half in range(2):
            b = bank0 + half
            for q in range(4):
                kc = half * 4 + q
                self.tr(self.ps[b][:, q * 128:(q + 1) * 128], src[:, kc * 128:(kc + 1) * 128],
                        reads=[src_key], writes=[('ps', b)])
            self.copy(eng_pair[half], self.xT[:, half * 4:(half + 1) * 4, i * 128:(i + 1) * 128],
                      self.ps[b][:, :].rearrange("p (k t) -> p k t", k=4),
                      reads=[('ps', b)], writes=[('xT', i)])
            if xf32 is not None:
                self.copy(eng_pair[1 - half], xf32[0][:, half * 4:(half + 1) * 4, :],
                          self.ps[b][:, :].rearrange("p (k t) -> p k t", k=4),
                          reads=[('ps', b)], writes=[xf32[1]])

    def phase_make_xT(self, src):
        s, ar = self.s, self.ar
        m = ar.mark()
        xl = [ar.alloc(DM) for _ in range(2)]
        for i in range(NTILE):
            sl = i % 2
            s.dma('sp', xl[sl], src[i * 128:(i + 1) * 128, :], key=f'xl{sl}', writes=[('xl', sl)])
            self.transposes_to_xT(xl[sl], ('xl', sl), i, bank0=(i % 2) * 2)
        ar.release(m)

    def phase_attention(self, l):
        s, ar, ps, xT = self.s, self.ar, self.ps, self.xT
        m = ar.mark()
        biasT = ar.alloc(12 * 256).rearrange("p (h c) -> p h c", h=12)
        wu = [ar.alloc(8 * 384, BF16).rearrange("p (k c) -> p k c", k=8) for _ in range(2)]
        qT = ar.alloc(S_LEN, BF16)
        kT = ar.alloc(S_LEN, BF16)
        V = ar.alloc(32 * 128, BF16).rearrange("p (b e) -> p b e", b=32)
        OD = ar.alloc(2 * S_LEN).rearrange("p (two t) -> p two t", two=2)
        PT = [ar.alloc(256, BF16) for _ in range(4)]
        tmp = [ar.alloc(256) for _ in range(2)]
        yst = ar.alloc(S_LEN, BF16)
        s.dma('sp', biasT, self.I('biasT'), key='c0', writes=['biasT'])
        scale = 128.0 ** -0.5
        for hs in range(4):
            for g in range(3):
                d = (1, 4, 16)[g]
                nb = 32 // d
                h = g * 4 + hs
                u = hs * 3 + g
                sl = u % 2
                s.dma('pool', wu[sl], self.I('w_in_r')[l][:, :, u * 384:(u + 1) * 384], key=f'wu{sl}', writes=[('wu', sl)])
                for tt in range(8):
                    for X in range(2):
                        bank = X
                        for kc in range(8):
                            self.mm(ps[bank][:, :], wu[sl][:, kc, X * 128:(X + 1) * 128], xT[:, kc, tt * 512:(tt + 1) * 512],
                                    kc == 0, kc == 7, reads=[('wu', sl)], writes=[('ps', bank)])
                        dst = (qT if X == 0 else kT).rearrange("p (r i) -> p r i", r=d)[:, :, (512 // d) * tt:(512 // d) * (tt + 1)]
                        src = ps[bank][:, :].rearrange("p (j r) -> p r j", r=d)
                        self.copy('act' if X == 0 else 'dve', dst, src, reads=[('ps', bank)], writes=['qT' if X == 0 else 'kT'])
                if ATT_LIMIT < 1:
                    continue
                for b4 in range(8):
                    bank = 2 + b4 % 2
                    for bb in range(4):
                        b = b4 * 4 + bb
                        r, c = divmod(b, nb)
                        t0 = 128 * c * d + r
                        for kc in range(8):
                            self.mm(ps[bank][:, bb * 128:(bb + 1) * 128], xT[:, kc, t0:t0 + 127 * d + 1:d], wu[sl][:, kc, 256:384],
                                    kc == 0, kc == 7, reads=[('wu', sl)], writes=[('ps', bank)])
                    self.copy('act' if b4 % 2 == 0 else 'dve', V[:, b4 * 4:(b4 + 1) * 4, :],
                              ps[bank][:, :].rearrange("p (b e) -> p b e", b=4), reads=[('ps', bank)], writes=[('V', b4)])
                if ATT_LIMIT < 2:
                    continue
                ODv = OD.rearrange("p two (i r) -> p two i r", r=d)

                def s_stage(b):
                    r, c = divmod(b, nb)
                    nq = 256 if c + 1 < nb else 128
                    bank = 4 + b % 2
                    sps = ps[bank][:, 0:nq]
                    self.mm(sps, kT[:, b * 128:(b + 1) * 128], qT[:, b * 128:b * 128 + nq], True, True,
                            reads=['qT', 'kT'], writes=[('ps', bank)])
                    tsl = b % 2
                    self.stt(tmp[tsl][:, :nq], sps, scale, biasT[:, h, :nq], ALU.mult, ALU.add,
                             reads=[('ps', bank), 'biasT'], writes=[('tmp', tsl)])
                    self.act(PT[b % 4][:, :nq], tmp[tsl][:, :nq], AF.Exp, reads=[('tmp', tsl)], writes=[('PT', b % 4)])

                def pv_stage(b):
                    r, c = divmod(b, nb)
                    bank = 6 + b % 2
                    ops_ = ps[bank][:, 0:256]
                    for which in range(2):
                        o_ = ops_[:, which * 128:(which + 1) * 128]
                        if c > 0:
                            lh = V[:, b - 1, :] if which == 0 else self.ones_bf
                            self.mm(o_, lh, PT[(b - 1) % 4][:, 128:256], True, False,
                                    reads=[('V', (b - 1) // 4), ('PT', (b - 1) % 4)], writes=[('ps', bank)])
                        lh = V[:, b, :] if which == 0 else self.ones_bf
                        self.mm(o_, lh, PT[b % 4][:, 0:128], c == 0, True,
                                reads=[('V', b // 4), ('PT', b % 4)], writes=[('ps', bank)])
                    view = ODv[:, :, 128 * c:128 * c + 128, r]
                    if g == 0:
                        regs = [('OD', b // 4)]
                    elif g == 1:
                        regs = [('OD', c)]
                    else:
                        regs = [('OD', 4 * c + j) for j in range(4)]
                    src = ops_.rearrange("p (two t) -> p two t", two=2)
                    if g == 0:
                        self.copy('act', view, src, reads=[('ps', bank)], writes=regs)
                    else:
                        self.tt(view, src, view, ALU.add, reads=[('ps', bank)] + regs, writes=regs)

                s_stage(0)
                for b in range(32):
                    if b + 1 < 32:
                        s_stage(b + 1)
                    pv_stage(b)
            if ATT_LIMIT < 3:
                self.dump(f"q{hs}", qT, [128, S_LEN], BF16, 'dbg', ['qT'])
                self.dump(f"v{hs}", V.rearrange("p b e -> p (b e)"), [128, S_LEN], BF16, 'dbg', [('V', i) for i in range(8)])
                continue
            for rg in range(8):
                cs = slice(rg * 512, (rg + 1) * 512)
                self.s.op('dve', lambda e, cs=cs: e.reciprocal(out=OD[:, 1, cs], in_=OD[:, 1, cs]), reads=[('OD', rg)], writes=[('OD', rg)])
                self.tt(yst[:, cs], OD[:, 0, cs], OD[:, 1, cs], ALU.mult, reads=[('OD', rg)], writes=['yst'])
            s.dma('sp', self.ya_d[hs], yst, key='yst', reads=['yst'], writes=[('ya_d', hs)])
            if ('ya', l) in self.dbg:
                self.dump(f"ya{l}_{hs}", yst, [128, S_LEN], BF16, 'dbg', ['yst'])
        ar.release(m)


    def phase_deltanet(self, l):
        import itertools
        s, ar, ps, xT, cm = self.s, self.ar, self.ps, self.xT, self.cm
        m = ar.mark()
        ident, ones, U, Ls, MS, MIT, BD, H0, H1 = (cm[:, k, :] for k in range(9))
        cw = ar.alloc(96)
        hp = ar.alloc(16)
        onw = ar.alloc(128)
        negA = ar.alloc(8)
        s.dma('sp', cw, self.I('conv_r')[l], key='c0', writes=['cw'])
        s.dma('sp', hp, self.I('headp')[l], key='c0', writes=['hp'])
        s.dma('sp', onw, self.I('onw')[l], key='c0', writes=['onw'])
        self.ts(onw, onw, 128.0 ** 0.5, None, ALU.mult, reads=['onw'], writes=['onw'])
        self.act(negA, hp[:, 0:8], AF.Exp, reads=['hp'], writes=['negA'])
        self.ts(negA, negA, -1.0, None, ALU.mult, reads=['negA'], writes=['negA'])
        wba = ar.alloc(8 * 16, BF16).rearrange("p (k c) -> p k c", k=8)
        s.dma('pool', wba, self.I('w_in_r')[l][:, :, OFF_BA:OFF_BA + 16], key='wba', writes=['wba'])
        a8 = lambda: ar.alloc(256).rearrange("p (i h) -> p i h", h=8)
        BETA, NB, Gs, EG, ED, BEG = a8(), a8(), a8(), a8(), a8(), a8()
        GLB = ar.alloc(512).rearrange("p (c h) -> p c h", h=8)
        m_small = ar.mark()
        GG = ar.alloc(512).rearrange("p (i c) -> p i c", c=16)
        ba = ar.alloc(512).rearrange("p (i c) -> p i c", c=16)
        for i in range(32):
            for kc in range(8):
                self.mm(ps[0][:, i * 16:(i + 1) * 16], xT[:, kc, i * 128:(i + 1) * 128], wba[:, kc, :], kc == 0, kc == 7,
                        reads=['wba'], writes=[('ps', 0)])
        self.copy('dve', ba, ps[0][:, :].rearrange("p (i c) -> p i c", c=16), reads=[('ps', 0)], writes=['ba'])
        bc = lambda v: v.unsqueeze(1).to_broadcast([128, 32, 8])
        self.act(BETA, ba[:, :, 0:8], AF.Sigmoid, reads=['ba'], writes=['BETA'])
        self.ts(NB, BETA, -1.0, None, ALU.mult, reads=['BETA'], writes=['NB'])
        self.tt(Gs, ba[:, :, 8:16], bc(hp[:, 8:16]), ALU.add, reads=['ba', 'hp'], writes=['Gs'])
        self.act(Gs, Gs, AF.Exp, reads=['Gs'], writes=['Gs'])
        self.act(Gs, Gs, AF.Ln, bias=1.0, reads=['Gs'], writes=['Gs'])
        self.tt(Gs, Gs, bc(negA), ALU.mult, reads=['Gs', 'negA'], writes=['Gs'])
        for i in range(32):
            self.mm(ps[1][:, i * 16:i * 16 + 8], U, Gs[:, i, :], True, True, reads=['Gs'], writes=[('ps', 1)])
            self.mm(ps[1][:, i * 16 + 8:i * 16 + 16], BD, Gs[:, i, :], True, True, reads=['Gs'], writes=[('ps', 1)])
            self.mm(ps[2][:, (2 * i) * 8:(2 * i) * 8 + 8], H0, Gs[:, i, :], True, True, reads=['Gs'], writes=[('ps', 2)])
            self.mm(ps[2][:, (2 * i + 1) * 8:(2 * i + 1) * 8 + 8], H1, Gs[:, i, :], True, True, reads=['Gs'], writes=[('ps', 2)])
        self.copy('dve', GG, ps[1][:, :].rearrange("p (i c) -> p i c", c=16), reads=[('ps', 1)], writes=['GG'])
        self.act(GLB, ps[2][:, :].rearrange("p (c h) -> p c h", h=8), AF.Exp, reads=[('ps', 2)], writes=['GLB'])
        self.act(EG, GG[:, :, 0:8], AF.Exp, reads=['GG'], writes=['EG'])
        self.tt(ED, GG[:, :, 8:16], GG[:, :, 0:8], ALU.subtract, reads=['GG'], writes=['ED'])
        self.act(ED, ED, AF.Exp, reads=['ED'], writes=['ED'])
        self.tt(BEG, BETA, EG, ALU.mult, reads=['BETA', 'EG'], writes=['BEG'])

        s.barrier()
        ar.release(m_small)
        bankrot = itertools.cycle(range(2, 8))
        w_in_r = self.I('w_in_r')
        evrot = itertools.cycle(('act', 'dve'))
        m4 = lambda: [ar.alloc(128) for _ in range(4)]

        def make_bufs(hs):
            B = dict(hs=hs)
            B['wu'] = ar.alloc(8 * 512, BF16).rearrange("p (k c) -> p k c", k=8)
            B['S'] = ar.alloc(128)
            B['pre'] = [ar.alloc(3 + 512) for _ in range(3)]
            B['hist'] = [ar.alloc(4) for _ in range(3)]
            B['cv'] = [ar.alloc(512) for _ in range(3)]
            B['sets'] = [dict(u0=m4(), wT=m4(), qkD=m4(), Kdec=m4(), QTn=ar.alloc(512),
                              zs=ar.alloc(512).rearrange("p (a e) -> p a e", a=4), idx=k) for k in range(2)]
            for nm in ('gU', 'Ds', 'DTi', 'Za', 'Ya', 'TTa', 'Kbg', 'Vb'):
                B[nm] = m4()
            B['ub'], B['osb'], B['onb'] = ar.alloc(128), ar.alloc(128), ar.alloc(128)
            B['ssq'], B['rn1'] = ar.alloc(1), ar.alloc(1)
            B['ybst'] = [ar.alloc(512, BF16)] * 2
            return B

        def stage(mms, evs):
            b = next(bankrot)
            for pp in range(4):
                mms(pp, ps[b][:, pp * 128:(pp + 1) * 128], ('ps', b))
            for pp in range(4):
                evs(pp, ps[b][:, pp * 128:(pp + 1) * 128], ('ps', b))

        def prep(h, sg, st, B):
            hs = B['hs']
            K = lambda *a: (hs,) + a
            t0 = sg * 512
            k_ = st['idx']
            W = B['wu']
            wk = K('wu')
            pre, cv, hist = B['pre'], B['cv'], B['hist']
            gU, Ds, DTi, Kbg, Vb = B['gU'], B['Ds'], B['DTi'], B['Kbg'], B['Vb']
            for X in range(3):
                bank = X % 2
                for kc in range(8):
                    self.mm(ps[bank][:, :], W[:, kc, X * 128:(X + 1) * 128], xT[:, kc, t0:t0 + 512], kc == 0, kc == 7,
                            reads=[wk], writes=[('ps', bank)])
                if sg > 0:
                    self.copy('dve', pre[X][:, 0:3], hist[X][:, 0:3], reads=[K('hist', X)], writes=[K('pre', X)])
                else:
                    self.s.op('dve', lambda e, X=X: e.memset(pre[X][:, 0:3], 0.0), writes=[K('pre', X)])
                self.copy('act', pre[X][:, 3:515], ps[bank][:, :], reads=[('ps', bank)], writes=[K('pre', X)])
                wc = lambda k: cw[:, h * 12 + X * 4 + k:h * 12 + X * 4 + k + 1]
                self.ts(cv[X], pre[X][:, 0:512], wc(0), None, ALU.mult, reads=[K('pre', X)], writes=[K('cv', X)])
                for k in range(1, 4):
                    self.stt(cv[X], pre[X][:, k:k + 512], wc(k), cv[X], ALU.mult, ALU.add,
                             reads=[K('pre', X), K('cv', X)], writes=[K('cv', X)])
                self.copy('dve', hist[X][:, 0:3], pre[X][:, 512:515], reads=[K('pre', X)], writes=[K('hist', X)])
                self.act(cv[X], cv[X], AF.Silu, reads=[K('cv', X)], writes=[K('cv', X)])
                yield
            for X in range(2):
                sqb = pre[X][:, 3:515]
                sqh = pre[X][:, 3:259].bitcast(BF16)
                self.act(sqh, cv[X], AF.Square, scale=(128.0 ** 0.5 if X == 0 else 1.0), reads=[K('cv', X)], writes=[K('pre', X)])
                self.mm(ps[X][:, :], self.ones_bf, sqh, True, True, reads=[K('pre', X)], writes=[('ps', X)])
                self.act(sqb, ps[X][:, :], AF.Sqrt, bias=self.cst[:, X:X + 1], reads=[('ps', X)], writes=[K('pre', X)])
                self.s.op('dve', lambda e, sqb=sqb: e.reciprocal(out=sqb, in_=sqb), reads=[K('pre', X)], writes=[K('pre', X)])
                if X == 0:
                    self.tt(st['QTn'], cv[0], sqb, ALU.mult, reads=[K('cv', 0), K('pre', 0)], writes=[K('QTn', k_)])
                else:
                    self.tt(cv[1], cv[1], sqb, ALU.mult, reads=[K('cv', 1), K('pre', 1)], writes=[K('cv', 1)])
                yield
            KTn, VTs, QTn = cv[1], cv[2], st['QTn']
            b = next(bankrot)
            for kc in range(8):
                self.mm(ps[b][:, :], W[:, kc, 384:512], xT[:, kc, t0:t0 + 512], kc == 0, kc == 7, reads=[wk], writes=[('ps', b)])
            self.act(st['zs'], ps[b][:, :].rearrange("p (a e) -> p a e", a=4), AF.Silu, reads=[('ps', b)], writes=[K('zs', k_)])
            yield
            ti = lambda pp: sg * 4 + pp
            col = lambda A_, pp: A_[:, ti(pp), h:h + 1]
            pc = lambda pp: slice(pp * 128, (pp + 1) * 128)
            stage(lambda pp, q, bk: self.tr(q, KTn[:, pc(pp)], reads=[K('cv', 1)], writes=[bk]),
                  lambda pp, q, bk: (self.ts(Kbg[pp], q, col(BEG, pp), None, ALU.mult, reads=[bk], writes=[K('Kbg', pp)]),
                                     self.act(st['Kdec'][pp], q, AF.Identity, scale=col(ED, pp), reads=[bk], writes=[K('Kdec', k_, pp)])))
            yield
            stage(lambda pp, q, bk: self.tr(q, VTs[:, pc(pp)], reads=[K('cv', 2)], writes=[bk]),
                  lambda pp, q, bk: self.ts(Vb[pp], q, col(BETA, pp), None, ALU.mult, reads=[bk], writes=[K('Vb', pp)]))
            yield
            for pp in range(4):
                self.ts(gU[pp], U, col(Gs, pp), None, ALU.mult, writes=[K('gU', pp)])
            stage(lambda pp, q, bk: (self.mm(q, gU[pp], Ls, True, False, reads=[K('gU', pp)], writes=[bk]),
                                     self.mm(q, ident, MS, False, True, writes=[bk])),
                  lambda pp, q, bk: self.act(Ds[pp], q, AF.Exp, reads=[bk], writes=[K('Ds', pp)]))
            yield
            stage(lambda pp, q, bk: (self.mm(q, Ls, gU[pp], True, False, reads=[K('gU', pp)], writes=[bk]),
                                     self.mm(q, ident, MIT, False, True, writes=[bk])),
                  lambda pp, q, bk: self.act(DTi[pp], q, AF.Exp, reads=[bk], writes=[K('DTi', pp)]))
            yield
            stage(lambda pp, q, bk: self.mm(q, KTn[:, pc(pp)], QTn[:, pc(pp)], True, True, reads=[K('cv', 1), K('QTn', k_)], writes=[bk]),
                  lambda pp, q, bk: self.tt(st['qkD'][pp], q, DTi[pp], ALU.mult, reads=[bk, K('DTi', pp)], writes=[K('qkD', k_, pp)]))
            yield
            Z, Y, TT = [B['Za'], gU], [B['Ya'], Ds], [B['TTa'], DTi]
            Zk = [lambda pp: K('Za', pp), lambda pp: K('gU', pp)]
            Yk = [lambda pp: K('Ya', pp), lambda pp: K('Ds', pp)]
            TTk = [lambda pp: K('TTa', pp), lambda pp: K('DTi', pp)]
            stage(lambda pp, q, bk: self.mm(q, KTn[:, pc(pp)], KTn[:, pc(pp)], True, True, reads=[K('cv', 1)], writes=[bk]),
                  lambda pp, q, bk: self.stt(Z[0][pp], q, col(NB, pp), Ds[pp], ALU.mult, ALU.mult,
                                             reads=[bk, K('Ds', pp)], writes=[Zk[0](pp)]))
            yield
            stage(lambda pp, q, bk: self.tr(q, Z[0][pp], reads=[Zk[0](pp)], writes=[bk]),
                  lambda pp, q, bk: (self.copy('act', Y[0][pp], q, reads=[bk], writes=[Yk[0](pp)]),
                                     self.tt(TT[0][pp], q, ident, ALU.add, reads=[bk], writes=[TTk[0](pp)])))
            yield
            cur = 0
            for k in range(1, 6):
                nxt = 1 - cur
                stage(lambda pp, q, bk: self.mm(q, Y[cur][pp], Z[cur][pp], True, True, reads=[Yk[cur](pp), Zk[cur](pp)], writes=[bk]),
                      lambda pp, q, bk: self.copy(next(evrot), Z[nxt][pp], q, reads=[bk], writes=[Zk[nxt](pp)]))
                if k < 5:
                    stage(lambda pp, q, bk: self.mm(q, Z[cur][pp], Y[cur][pp], True, True, reads=[Yk[cur](pp), Zk[cur](pp)], writes=[bk]),
                          lambda pp, q, bk: self.copy(next(evrot), Y[nxt][pp], q, reads=[bk], writes=[Yk[nxt](pp)]))
                yield
                stage(lambda pp, q, bk: self.mm(q, Z[nxt][pp], TT[cur][pp], True, True, reads=[Zk[nxt](pp), TTk[cur](pp)], writes=[bk]),
                      lambda pp, q, bk: self.tt(TT[nxt][pp], q, TT[cur][pp], ALU.add, reads=[bk, TTk[cur](pp)], writes=[TTk[nxt](pp)]))
                cur = nxt
                yield
            TTf, TTfk = TT[cur], TTk[cur]
            stage(lambda pp, q, bk: self.mm(q, TTf[pp], Vb[pp], True, True, reads=[TTfk(pp), K('Vb', pp)], writes=[bk]),
                  lambda pp, q, bk: self.copy(next(evrot), st['u0'][pp], q, reads=[bk], writes=[K('u0', k_, pp)]))
            yield
            stage(lambda pp, q, bk: self.mm(q, Kbg[pp], TTf[pp], True, True, reads=[TTfk(pp), K('Kbg', pp)], writes=[bk]),
                  lambda pp, q, bk: self.copy(next(evrot), st['wT'][pp], q, reads=[bk], writes=[K('wT', k_, pp)]))
            yield

        def scan(h, sg, st, B, ysl):
            hs = B['hs']
            K = lambda *a: (hs,) + a
            t0 = sg * 512
            k_ = st['idx']
            Sst, ub, osb, onb, ssq, rn1, ybst = B['S'], B['ub'], B['osb'], B['onb'], B['ssq'], B['rn1'], B['ybst']
            for pp in range(4):
                i = sg * 4 + pp
                for c in range(2):
                    rows = slice(64 * c, 64 * c + 64)
                    b1 = next(bankrot)
                    self.mm(ps[b1][rows, 0:128], st['wT'][pp][:, 64 * c:64 * c + 64], Sst, True, True,
                            reads=[K('wT', k_, pp), K('S')], writes=[('ps', b1)])
                    self.mm(ps[b1][rows, 128:256], st['QTn'][:, pp * 128 + 64 * c:pp * 128 + 64 * c + 64], Sst, True, True,
                            reads=[K('QTn', k_), K('S')], writes=[('ps', b1)])
                    self.tt(ub[rows, :], st['u0'][pp][rows, :], ps[b1][rows, 0:128], ALU.subtract,
                            reads=[('ps', b1), K('u0', k_, pp)], writes=[K('ub')])
                    self.act(osb[rows, :], ps[b1][rows, 128:256], AF.Identity, scale=EG[rows, i, h:h + 1],
                             reads=[('ps', b1)], writes=[K('osb')])
                    yield
                    b2 = next(bankrot)
                    self.mm(ps[b2][:, 0:128], st['Kdec'][pp][rows, :], ub[rows, :], True, True,
                            reads=[K('ub'), K('Kdec', k_, pp)], writes=[('ps', b2)])
                    self.stt(Sst, Sst, GLB[:, 2 * i + c, h:h + 1], ps[b2][:, 0:128], ALU.mult, ALU.add,
                             reads=[('ps', b2), K('S')], writes=[K('S')])
                    yield
                b3 = next(bankrot)
                self.mm(ps[b3][:, 0:128], st['qkD'][pp], ub, True, True, reads=[K('ub'), K('qkD', k_, pp)], writes=[('ps', b3)])
                self.tt(osb, osb, ps[b3][:, 0:128], ALU.add, reads=[('ps', b3), K('osb')], writes=[K('osb')])
                self.s.op('dve', lambda e, ssq=ssq: e.memset(ssq, 0.0), writes=[K('ssq')])
                self.act(onb, osb, AF.Square, accum_out=ssq, reads=[K('osb'), K('ssq')], writes=[K('onb'), K('ssq')])
                self.act(rn1, ssq, AF.Sqrt, bias=self.cst[:, 0:1], reads=[K('ssq')], writes=[K('rn1')])
                self.s.op('dve', lambda e, rn1=rn1: e.reciprocal(out=rn1, in_=rn1), reads=[K('rn1')], writes=[K('rn1')])
                self.stt(onb, osb, rn1[:, 0:1], onw, ALU.mult, ALU.mult, reads=[K('osb'), K('rn1')], writes=[K('onb')])
                b4 = next(bankrot)
                self.tr(ps[b4][:, 0:128], onb, reads=[K('onb')], writes=[('ps', b4)])
                self.tt(ybst[ysl][:, pp * 128:(pp + 1) * 128], ps[b4][:, 0:128], st['zs'][:, pp, :], ALU.mult,
                        reads=[('ps', b4), K('zs', k_)], writes=[K('ybst')])
                yield
            self.s.dma('sp', self.yb_d[h][:, t0:t0 + 512], ybst[ysl], key=f'ybst{hs}', reads=[K('ybst')], writes=[('yb_d', h, sg)])

        def merged(g1, g2):
            gens = [g for g in (g1, g2) if g is not None]
            while gens:
                for g in list(gens):
                    try:
                        next(g)
                        yield
                    except StopIteration:
                        gens.remove(g)

        def chain(h, B):
            hs = B['hs']
            s.dma('pool', B['wu'], w_in_r[l][:, :, OFF_DN + h * 512:OFF_DN + (h + 1) * 512], key=f'dwu{hs}', writes=[(hs, 'wu')])
            self.s.op('dve', lambda e: e.memset(B['S'], 0.0), reads=[(hs, 'S')], writes=[(hs, 'S')])
            nsg = 8 if DN_LIMIT >= 3 else 1
            yield from prep(h, 0, B['sets'][0], B)
            for sg in range(nsg):
                nxt = prep(h, sg + 1, B['sets'][(sg + 1) % 2], B) if sg + 1 < nsg else None
                yield from merged(scan(h, sg, B['sets'][sg % 2], B, sg % 2), nxt)

        NPAR = 2
        bufs = [make_bufs(k) for k in range(NPAR)]
        heads = list(range(8 if DN_LIMIT >= 9 else (NPAR if DN_LIMIT >= 1 else 0)))
        for h0 in range(0, len(heads), NPAR):
            gens = [chain(h0 + k, bufs[k]) for k in range(NPAR) if h0 + k < len(heads)]
            for _ in merged(*gens) if len(gens) == 2 else gens[0]:
                pass
        ar.release(m)

    def ln_tile(self, res, res_key, add_views, add_keys, gam, bet, out, out_key, tbufs, statss, ls):
        tbuf, stats = tbufs[ls], statss[ls]
        kt, kst = ('ln_t', ls), ('ln_st', ls)
        s1, nm, ss, rs = (stats[:, j:j + 1] for j in range(4))
        for hf in range(2):
            cs = slice(hf * 512, (hf + 1) * 512)
            self.stt(tbuf[:, cs], res[:, cs], ALPHA, add_views[hf], ALU.mult, ALU.add,
                     reads=[res_key, add_keys[hf]], writes=[kt])
        self.s.op('dve', lambda e: e.memset(stats[:, 0:4], 0.0), writes=[kst])
        self.act(out, tbuf, AF.Identity, accum_out=s1, reads=[kt, kst], writes=[out_key, kst])
        self.act(nm, s1, AF.Identity, scale=-1.0 / DM, reads=[kst], writes=[kst])
        self.act(out, tbuf, AF.Square, bias=nm, accum_out=ss, reads=[kt, kst], writes=[out_key, kst])
        self.act(rs, ss, AF.Sqrt, bias=self.cst[:, 2:3], scale=1.0 / DM, reads=[kst], writes=[kst])
        self.s.op('dve', lambda e: e.reciprocal(out=rs, in_=rs), reads=[kst], writes=[kst])
        self.ts(tbuf, tbuf, nm, rs, ALU.add, ALU.mult, reads=[kt, kst], writes=[kt])
        self.tt(tbuf, tbuf, gam, ALU.mult, reads=[kt, 'lnp'], writes=[kt])
        self.tt(out, tbuf, bet, ALU.add, reads=[kt, 'lnp'], writes=[out_key])

    def phase_merge(self, l):
        import itertools
        s, ar, ps, xT = self.s, self.ar, self.ps, self.xT
        m = ar.mark()
        moe = (l % 2 == 1)
        w_in_r = self.I('w_in_r')
        Wga = ar.alloc(8 * DM, BF16).rearrange("p (k c) -> p k c", k=8)
        Wgb = ar.alloc(8 * DM, BF16).rearrange("p (k c) -> p k c", k=8)
        Woa = ar.alloc(4 * DM, BF16).rearrange("p (k c) -> p k c", k=4)
        Wob = ar.alloc(8 * DM, BF16).rearrange("p (k c) -> p k c", k=8)
        Wout = ar.alloc(8 * DM, BF16).rearrange("p (k c) -> p k c", k=8)
        gam, bet = ar.alloc(DM), ar.alloc(DM)
        s.dma('pool', Wga, w_in_r[l][:, :, OFF_GA:OFF_GA + DM], key='mw0', writes=['Wga'])
        s.dma('pool', Woa, self.I('w_oa_r')[l], key='mw1', writes=['Woa'])
        s.dma('pool', Wgb, w_in_r[l][:, :, OFF_GB:OFF_GB + DM], key='mw2', writes=['Wgb'])
        s.dma('pool', Wob, self.I('w_ob_r')[l], key='mw3', writes=['Wob'])
        s.dma('pool', Wout, self.I('w_out_r')[l], key='mw4', writes=['Wout'])
        s.dma('sp', gam, self.I('ln_r')[l][0], key='c0', writes=['lnp'])
        s.dma('sp', bet, self.I('ln_r')[l][1], key='c0', writes=['lnp'])
        TB = 512
        yaT = [ar.alloc(4 * TB, BF16).rearrange("p (k t) -> p k t", k=4)] * 2
        ybT = [ar.alloc(8 * TB, BF16).rearrange("p (k t) -> p k t", k=8) for _ in range(2)]
        mT = ar.alloc(8 * TB, BF16).rearrange("p (k t) -> p k t", k=8)
        sg = [ar.alloc(TB) for _ in range(2)]
        xres = [ar.alloc(DM)] * 2
        tbufs = [ar.alloc(DM) for _ in range(2)]
        xo = [ar.alloc(DM) for _ in range(2)]
        statss = [ar.alloc(8) for _ in range(2)]
        if moe:
            x1Tf = ar.alloc(8 * 128).rearrange("p (k t) -> p k t", k=8)
            wr = ar.alloc(8 * NEXP).rearrange("p (k e) -> p k e", k=8)
            s.dma('sp', wr, self.I('moe_router_r'), key='c0', writes=['wr'])
            L, Lm, mk, E = ar.alloc(8), ar.alloc(8), ar.alloc(8), ar.alloc(8)
            gst = ar.alloc(8)
        src_res = self.I('x') if l == 0 else self.x2_d
        brot = itertools.cycle(range(0, 4))
        ya_v = self.ya_d.rearrange("k p t -> p k t")
        yb_v = self.yb_d.rearrange("k p t -> p k t")
        nblk = S_LEN // TB
        for blk in range(nblk):
            bs = slice(blk * TB, (blk + 1) * TB)
            sl = blk % 2
            s.dma('sp', yaT[sl], ya_v[:, :, bs], key='ya0', writes=[('yaT', 0)])
            s.dma('sp', ybT[sl], yb_v[:, :, bs], key=f'yb{sl}', writes=[('ybT', sl)])
            for oc in range(8):
                ocs = slice(oc * 128, (oc + 1) * 128)
                for br in range(2):
                    Wg, Wo, yT, nk = (Wga, Woa, yaT[sl], 4) if br == 0 else (Wgb, Wob, ybT[sl], 8)
                    wgk, wok, yk = (('Wga', 'Woa', ('yaT', 0)) if br == 0 else ('Wgb', 'Wob', ('ybT', sl)))
                    b1 = next(brot)
                    for kc in range(8):
                        self.mm(ps[b1][:, 0:TB], Wg[:, kc, ocs], xT[:, kc, bs], kc == 0, kc == 7,
                                reads=[wgk, ('xTb', blk)], writes=[('ps', b1)])
                    self.act(sg[br], ps[b1][:, 0:TB], AF.Sigmoid, reads=[('ps', b1)], writes=[('sg', br)])
                    b2 = next(brot)
                    for kc in range(nk):
                        self.mm(ps[b2][:, 0:TB], Wo[:, kc, ocs], yT[:, kc, :], kc == 0, kc == nk - 1,
                                reads=[wok, yk], writes=[('ps', b2)])
                    self.tt(sg[br], sg[br], ps[b2][:, 0:TB], ALU.mult, reads=[('sg', br), ('ps', b2)], writes=[('sg', br)])
                    if br == 1:
                        self.tt(mT[:, oc, :], sg[0], sg[1], ALU.add, reads=[('sg', 0), ('sg', 1)], writes=['mT'])
            for sub in range(TB // 128):
                i = blk * (TB // 128) + sub
                xs = i % 2
                s.dma('sp', xres[xs], src_res[i * 128:(i + 1) * 128, :], key='xr0', writes=[('xres', 0)])
                for hf in range(2):
                    for kc in range(8):
                        self.mm(ps[4 + hf][:, :], mT[:, kc, sub * 128:(sub + 1) * 128], Wout[:, kc, hf * 512:(hf + 1) * 512],
                                kc == 0, kc == 7, reads=['mT', 'Wout'], writes=[('ps', 4 + hf)])
                self.ln_tile(xres[xs], ('xres', 0), [ps[4][:, :], ps[5][:, :]], [('ps', 4), ('ps', 5)], gam, bet,
                             xo[xs], ('xo', xs), tbufs, statss, xs)
                s.dma('sp', self.x1_d[i * 128:(i + 1) * 128, :], xo[xs], key=f'xo{xs}', reads=[('xo', xs)], writes=[('x1_d', i)])
                for hf in range(2):
                    b = 6 + hf
                    for q in range(4):
                        kc = hf * 4 + q
                        self.tr(ps[b][:, q * 128:(q + 1) * 128], xo[xs][:, kc * 128:(kc + 1) * 128], reads=[('xo', xs)], writes=[('ps', b)])
                    self.copy('act' if hf == 0 else 'dve', xT[:, hf * 4:(hf + 1) * 4, i * 128:(i + 1) * 128],
                              ps[b][:, :].rearrange("p (k t) -> p k t", k=4), reads=[('ps', b)], writes=[('xTb', blk)])
                    if moe:
                        self.copy('dve' if hf == 0 else 'act', x1Tf[:, hf * 4:(hf + 1) * 4, :],
                                  ps[b][:, :].rearrange("p (k t) -> p k t", k=4), reads=[('ps', b)], writes=['x1Tf'])
                if moe:
                    for kc in range(8):
                        self.mm(ps[6][:, 0:NEXP], x1Tf[:, kc, :], wr[:, kc, :], kc == 0, kc == 7, reads=['x1Tf', 'wr'], writes=[('ps', 6)])
                    self.copy('dve', L, ps[6][:, 0:NEXP], reads=[('ps', 6)], writes=['L'])
                    m1, m2, den = gst[:, 0:1], gst[:, 1:2], gst[:, 2:3]
                    self.s.op('dve', lambda e, m1=m1: e.tensor_reduce(out=m1, in_=L, axis=AX.X, op=ALU.max), reads=['L'], writes=['gst'])
                    self.ts(Lm, L, m1, None, ALU.subtract, reads=['L', 'gst'], writes=['Lm'])
                    self.ts(mk, Lm, 0.0, None, ALU.is_equal, reads=['Lm'], writes=['mk'])
                    self.stt(mk, mk, -1e30, Lm, ALU.mult, ALU.add, reads=['mk', 'Lm'], writes=['mk'])
                    self.s.op('dve', lambda e, m2=m2: e.tensor_reduce(out=m2, in_=mk, axis=AX.X, op=ALU.max), reads=['mk'], writes=['gst'])
                    self.ts(mk, Lm, m2, None, ALU.is_ge, reads=['Lm', 'gst'], writes=['mk'])
                    self.act(E, Lm, AF.Exp, reads=['Lm'], writes=['E'])
                    self.tt(E, E, mk, ALU.mult, reads=['E', 'mk'], writes=['E'])
                    self.s.op('dve', lambda e, den=den: e.tensor_reduce(out=den, in_=E, axis=AX.X, op=ALU.add), reads=['E'], writes=['gst'])
                    self.s.op('dve', lambda e, den=den: e.reciprocal(out=den, in_=den), reads=['gst'], writes=['gst'])
                    self.ts(self.Gt[:, i, :], E, den, None, ALU.mult, reads=['E', 'gst'], writes=['Gt'])
        ar.release(m)

    def phase_ffn(self, l):
        import itertools
        s, ar, ps, xT = self.s, self.ar, self.ps, self.xT
        m = ar.mark()
        moe = (l % 2 == 1)
        last = (l == DEPTH - 1)
        if moe:
            nexp, nch = NEXP, D_FFE // 128
            gsrc = lambda e, c0, n: self.I('moe_g_r')[e][c0:c0 + n].rearrange("c p k j -> p c (k j)")
            usrc = lambda e, c0, n: self.I('moe_u_r')[e][c0:c0 + n].rearrange("c p k j -> p c (k j)")
            dsrc = lambda e, c0, n: self.I('moe_d')[e][c0 * 128:(c0 + n) * 128, :].rearrange("(c p) n -> p c n", p=128)
        else:
            nexp, nch = 1, D_FF // 128
            gsrc = lambda e, c0, n: self.I('ffn_g_r')[c0:c0 + n].rearrange("c p k j -> p c (k j)")
            usrc = lambda e, c0, n: self.I('ffn_u_r')[c0:c0 + n].rearrange("c p k j -> p c (k j)")
            dsrc = lambda e, c0, n: self.I('ffn_d')[c0 * 128:(c0 + n) * 128, :].rearrange("(c p) n -> p c n", p=128)
        GC = 4
        TBK = 1024
        acc = ar.alloc(8 * DM).rearrange("p (a n) -> p a n", a=8)
        hT = [ar.alloc(GC * TBK, BF16).rearrange("p (c t) -> p c t", c=GC) for _ in range(2)]
        Wg = [ar.alloc(GC * 1024, BF16).rearrange("p (c k j) -> p c k j", c=GC, k=8) for _ in range(2)]
        Wu = [ar.alloc(GC * 1024, BF16).rearrange("p (c k j) -> p c k j", c=GC, k=8) for _ in range(2)]
        Wd = [ar.alloc(GC * DM, BF16).rearrange("p (c n) -> p c n", c=GC) for _ in range(2)]
        sgb = [ar.alloc(512) for _ in range(2)]
        gam, bet = ar.alloc(DM), ar.alloc(DM)
        xres = [ar.alloc(DM) for _ in range(2)]
        tbufs = [ar.alloc(DM) for _ in range(2)]
        xo = [ar.alloc(DM) for _ in range(2)]
        statss = [ar.alloc(8) for _ in range(2)]
        s.dma('sp', gam, self.I('ln_r')[l][2], key='c0', writes=['lnp'])
        s.dma('sp', bet, self.I('ln_r')[l][3], key='c0', writes=['lnp'])
        groups = [(c0, min(GC, nch - c0)) for c0 in range(0, nch, GC)]
        gurot = itertools.cycle([(0, 1), (2, 3)])
        drot = itertools.cycle([4, 5])
        dst = self.y_out if last else self.x2_d
        nslot = 0
        for tb in range(S_LEN // TBK if FFN_LIMIT >= 9 else 1):
            first = True
            for e in range(nexp):
                for (c0, n) in (groups if FFN_LIMIT >= 2 else groups[:1]):
                    sl = nslot % 2
                    nslot += 1
                    s.dma('pool', Wg[sl][:, 0:n].rearrange("p c k j -> p c (k j)"), gsrc(e, c0, n), key=f'fg{sl}', writes=[('Wg', sl)])
                    s.dma('pool', Wu[sl][:, 0:n].rearrange("p c k j -> p c (k j)"), usrc(e, c0, n), key=f'fu{sl}', writes=[('Wu', sl)])
                    s.dma('pool', Wd[sl][:, 0:n], dsrc(e, c0, n), key=f'fd{sl}', writes=[('Wd', sl)])
                    for c in range(n):
                        for hf in range(2):
                            ts_ = slice(tb * TBK + hf * 512, tb * TBK + (hf + 1) * 512)
                            bg, bu = next(gurot)
                            for kc in range(8):
                                self.mm(ps[bg][:, :], Wg[sl][:, c, kc, :], xT[:, kc, ts_], kc == 0, kc == 7,
                                        reads=[('Wg', sl), ('xTb', tb)], writes=[('ps', bg)])
                            for kc in range(8):
                                self.mm(ps[bu][:, :], Wu[sl][:, c, kc, :], xT[:, kc, ts_], kc == 0, kc == 7,
                                        reads=[('Wu', sl), ('xTb', tb)], writes=[('ps', bu)])
                            k2 = (c * 2 + hf) % 2
                            self.act(sgb[k2], ps[bg][:, :], AF.Silu, reads=[('ps', bg)], writes=[('sgb', k2)])
                            self.tt(hT[sl][:, c, hf * 512:(hf + 1) * 512], sgb[k2], ps[bu][:, :], ALU.mult,
                                    reads=[('sgb', k2), ('ps', bu)], writes=[('hT', sl)])
                    for sub in range(8 if FFN_LIMIT >= 1 else 0):
                        i = tb * 8 + sub
                        for hf in range(2):
                            bd = next(drot)
                            for c in range(n):
                                self.mm(ps[bd][:, :], hT[sl][:, c, sub * 128:(sub + 1) * 128], Wd[sl][:, c, hf * 512:(hf + 1) * 512],
                                        c == 0, c == n - 1, reads=[('hT', sl), ('Wd', sl)], writes=[('ps', bd)])
                            av = acc[:, sub, hf * 512:(hf + 1) * 512]
                            ak = ('acc', sub, hf)
                            if moe:
                                gcol = self.Gt[:, i, e:e + 1]
                                if first:
                                    self.ts(av, ps[bd][:, :], gcol, None, ALU.mult, reads=[('ps', bd)], writes=[ak])
                                else:
                                    self.stt(av, ps[bd][:, :], gcol, av, ALU.mult, ALU.add, reads=[('ps', bd), ak], writes=[ak])
                            else:
                                if first:
                                    self.copy('dve', av, ps[bd][:, :], reads=[('ps', bd)], writes=[ak])
                                else:
                                    self.tt(av, av, ps[bd][:, :], ALU.add, reads=[('ps', bd), ak], writes=[ak])
                    first = False
            for sub in range(8 if FFN_LIMIT >= 3 else 0):
                i = tb * 8 + sub
                xs = i % 2
                s.dma('sp', xres[xs], self.x1_d[i * 128:(i + 1) * 128, :], key=f'xr{xs}', writes=[('xres', xs)])
                self.ln_tile(xres[xs], ('xres', xs), [acc[:, sub, 0:512], acc[:, sub, 512:1024]], [('acc', sub, 0), ('acc', sub, 1)],
                             gam, bet, xo[xs], ('xo', xs), tbufs, statss, xs)
                s.dma('sp', dst[i * 128:(i + 1) * 128, :], xo[xs], key=f'xo{xs}', reads=[('xo', xs)], writes=[('dst', i)])
                if not last:
                    for hf in range(2):
                        b = 6 + hf
                        for q in range(4):
                            kc = hf * 4 + q
                            self.tr(ps[b][:, q * 128:(q + 1) * 128], xo[xs][:, kc * 128:(kc + 1) * 128], reads=[('xo', xs)], writes=[('ps', b)])
                        self.copy('act' if hf == 0 else 'dve', xT[:, hf * 4:(hf + 1) * 4, i * 128:(i + 1) * 128],
                                  ps[b][:, :].rearrange("p (k t) -> p k t", k=4), reads=[('ps', b)], writes=[('xTb', tb)])
        ar.release(m)


def _prep_shared(inp):
    f = lambda a: np.ascontiguousarray(a, dtype=np.float32)
    perm = _w_in_perm()
    w_in = inp["w_in"]
    w_in_r = np.stack([w_in[l][:, perm].reshape(8, 128, N_IN).transpose(1, 0, 2) for l in range(DEPTH)])
    cw = inp["conv_w"]
    conv_r = np.stack([cw[l].reshape(4, 3, 8, 128).transpose(3, 2, 1, 0).reshape(128, 96) for l in range(DEPTH)])
    headp = np.stack([np.broadcast_to(np.concatenate([inp["a_log"][l], inp["dt_bias"][l]])[None, :], (128, 16)) for l in range(DEPTH)])
    onw = np.stack([np.broadcast_to(inp["o_norm_w"][l][None, :], (128, 128)) for l in range(DEPTH)])
    kt = lambda w, kc: w.reshape(kc, 128, w.shape[1]).transpose(1, 0, 2)
    w_oa_r = np.stack([kt(inp["w_oa"][l], 4) for l in range(DEPTH)])
    w_ob_r = np.stack([kt(inp["w_ob"][l], 8) for l in range(DEPTH)])
    w_out_r = np.stack([kt(inp["w_out"][l], 8) for l in range(DEPTH)])
    ln_r = np.stack([np.stack([np.broadcast_to(inp[k][l][None, :], (128, DM)) for k in ("ln1_g", "ln1_b", "ln2_g", "ln2_b")]) for l in range(DEPTH)])
    ct = lambda w: w.reshape(8, 128, w.shape[1] // 128, 128).transpose(2, 1, 0, 3)
    shared = {
        "cmat": _const_mats(),
        "biasT": _bias_tables(np.asarray(inp["rel_bias"], np.float32)),
        "w_in_r": w_in_r, "conv_r": conv_r, "headp": headp, "onw": onw,
        "w_oa_r": w_oa_r, "w_ob_r": w_ob_r, "w_out_r": w_out_r, "ln_r": ln_r,
        "ffn_g_r": ct(inp["ffn_w_gate"][0]), "ffn_u_r": ct(inp["ffn_w_up"][0]), "ffn_d": inp["ffn_w_down"][0],
        "moe_router_r": inp["moe_router"][0].reshape(8, 128, NEXP).transpose(1, 0, 2),
        "moe_g_r": np.stack([ct(inp["moe_w_gate"][0][e]) for e in range(NEXP)]),
        "moe_u_r": np.stack([ct(inp["moe_w_up"][0][e]) for e in range(NEXP)]),
        "moe_d": inp["moe_w_down"][0],
    }
    return {k: f(v) for k, v in shared.items()}


def kernel(**inputs):
    inp = {k: np.asarray(v) for k, v in inputs.items()}
    shared = _prep_shared(inp)
    prog = Prog()
    x = np.ascontiguousarray(inp["x"], dtype=np.float32)
    shared = {k: v for k, v in shared.items() if k in prog.ins}
    in_maps = [dict(shared, x=x[b]) for b in range(8)]
    res = run_bass_kernel_spmd(prog.nc, in_maps, core_ids=list(range(8)))
    return np.stack([np.asarray(res.results[b]["y"], dtype=np.float32) for b in range(8)])
```

```python
import numpy as np
from contextlib import ExitStack
import concourse.bass as bass
import concourse.mybir as mybir
from concourse.bass_utils import run_bass_kernel_spmd

F32 = mybir.dt.float32
BF16 = mybir.dt.bfloat16
AF = mybir.ActivationFunctionType
ALU = mybir.AluOpType
AX = mybir.AxisListType

S_LEN = 4096
DM = 1024
NTILE = 32
N_IN = 10768
DEPTH = 2
D_FF = 2816
D_FFE = 3584
NEXP = 8
ALPHA = (2 * DEPTH) ** 0.25
LN_EPS = 1e-5
RMS_EPS = 1e-6
NEG = -30000.0
ENGS = ('pe', 'dve', 'act', 'pool', 'sp')
import os
ATT_LIMIT = int(os.environ.get('ATT_LIMIT', '9'))
DN_LIMIT = int(os.environ.get('DN_LIMIT', '9'))
FFN_LIMIT = int(os.environ.get('FFN_LIMIT', '9'))
SKIP_MIXER = int(os.environ.get('SKIP_MIXER', '0'))


class Sched:
    def __init__(self, nc, ctx):
        self.nc = nc
        self.ctx = ctx
        self.ops = {e: [] for e in ENGS}
        self.cnt = {e: 0 for e in ENGS}
        self.seen = {e: {} for e in ENGS}
        self.res = {}
        self.sems = {}
        for e in ('pe', 'dve', 'act', 'pool'):
            self.sems[e] = ctx.enter_context(nc.semaphore("sem_" + e))
        self.dcnt = {}

    def _sem(self, key):
        if key not in self.sems:
            self.sems[key] = self.ctx.enter_context(self.nc.semaphore("semd_" + str(key)))
            self.dcnt[key] = 0
        return self.sems[key]

    def _deps(self, eng, reads, writes):
        need = {}

        def add(k, v):
            if need.get(k, 0) < v:
                need[k] = v
        for r in reads:
            st = self.res.get(r)
            if st and st['w']:
                add(*st['w'])
        for w in writes:
            st = self.res.get(w)
            if st:
                if st['w'] and st['w'][0] != eng:
                    add(*st['w'])
                for k, v in st['r'].items():
                    if k != eng:
                        add(k, v)
        out = []
        for k, v in need.items():
            if self.seen[eng].get(k, 0) < v:
                self.seen[eng][k] = v
                out.append((k, v))
        return out

    def op(self, eng, fn, reads=(), writes=()):
        psr = [r for r in reads if isinstance(r, tuple) and r[0] == 'ps' and r not in writes]
        if psr:
            writes = list(writes) + psr
        waits = self._deps(eng, reads, writes)
        self.cnt[eng] += 1
        n = self.cnt[eng]
        self.ops[eng].append((waits, fn, eng, 1))
        for r in reads:
            st = self.res.setdefault(r, {'w': None, 'r': {}})
            st['r'][eng] = n
        for w in writes:
            self.res[w] = {'w': (eng, n), 'r': {}}
        return n

    def dma(self, eng, out, in_, key, reads=(), writes=(), **kw):
        self._sem(key)
        waits = self._deps(eng, reads, writes)
        self.dcnt[key] += 16
        n = self.dcnt[key]
        self.ops[eng].append((waits, lambda e: e.dma_start(out=out, in_=in_, **kw), key, 16))
        for r in reads:
            st = self.res.setdefault(r, {'w': None, 'r': {}})
            st['r'][key] = n
        for w in writes:
            self.res[w] = {'w': (key, n), 'r': {}}
        return n

    def dma_fn(self, eng, fn, key, reads=(), writes=()):
        self._sem(key)
        waits = self._deps(eng, reads, writes)
        self.dcnt[key] += 16
        n = self.dcnt[key]
        self.ops[eng].append((waits, fn, key, 16))
        for r in reads:
            st = self.res.setdefault(r, {'w': None, 'r': {}})
            st['r'][key] = n
        for w in writes:
            self.res[w] = {'w': (key, n), 'r': {}}
        return n

    def barrier(self):
        cur = {e: self.cnt[e] for e in ('pe', 'dve', 'act', 'pool')}
        cur.update(self.dcnt)
        for e in ENGS:
            waits = []
            for k, v in cur.items():
                if k != e and v > 0 and self.seen[e].get(k, 0) < v:
                    self.seen[e][k] = v
                    waits.append((k, v))
            if waits:
                self.ops[e].append((waits, None, None, 0))
        self.res = {}

    def emit(self):
        nc = self.nc
        sems = self.sems
        ops = self.ops

        def run(e, lst):
            for waits, fn, sk, inc in lst:
                for k, v in waits:
                    e.wait_ge(sems[k], v)
                if fn is not None:
                    fn(e).then_inc(sems[sk], inc)
        with nc.Block() as block:
            @block.tensor
            def _(e):
                run(e, ops['pe'])

            @block.vector
            def _(e):
                run(e, ops['dve'])

            @block.scalar
            def _(e):
                run(e, ops['act'])

            @block.gpsimd
            def _(e):
                run(e, ops['pool'])

            @block.sync
            def _(e):
                run(e, ops['sp'])


class Arena:
    def __init__(self, t, nwords):
        self.t = t
        self.n = nwords
        self.off = 0

    def alloc(self, nelem, dtype=F32):
        nw = nelem if dtype == F32 else (nelem + 1) // 2
        assert self.off + nw <= self.n, f"arena overflow {self.off}+{nw}>{self.n}"
        ap = self.t[:, self.off:self.off + nw]
        self.off += nw
        return ap if dtype == F32 else ap.bitcast(dtype)

    def mark(self):
        return self.off

    def release(self, m):
        self.off = m


def _w_in_perm():
    cols = []
    for hs in range(4):
        for g in range(3):
            h = g * 4 + hs
            for base in (0, 1536, 3072):
                cols.extend(range(base + h * 128, base + (h + 1) * 128))
    for h in range(8):
        for base in (4608, 5632, 6656, 7680):
            cols.extend(range(base + h * 128, base + (h + 1) * 128))
    cols.extend(range(8704, 8720))
    cols.extend(range(8720, 10768))
    assert len(cols) == N_IN
    return np.asarray(cols)


OFF_DN = 12 * 384
OFF_BA = OFF_DN + 8 * 512
OFF_GA = OFF_BA + 16
OFF_GB = OFF_GA + 1024


def _t5_bucket(dist):
    dist = np.asarray(dist, np.int64)
    d = np.maximum(dist, 1).astype(np.float32)
    large = 16 + (np.log(d / np.float32(16)) / np.float32(np.log(2048 / 16)) * np.float32(16)).astype(np.int32)
    large = np.minimum(large, 31)
    return np.where(dist < 16, dist, large)


def _bias_tables(rel_bias):
    out = np.full((128, 12, 256), NEG, np.float32)
    kj = np.arange(128)[:, None]
    qi = np.arange(128)[None, :]
    for h in range(12):
        d = (1, 4, 16)[h // 4]
        delta0 = qi - kj
        b0 = rel_bias[_t5_bucket(np.maximum(delta0, 0) * d), h]
        out[:, h, 0:128] = np.where(delta0 >= 0, b0, NEG)
        delta1 = qi + 128 - kj
        b1 = rel_bias[_t5_bucket(delta1 * d), h]
        out[:, h, 128:256] = np.where(delta1 <= 128, b1, NEG)
    return out


def _const_mats():
    r = np.arange(128)[:, None]
    c = np.arange(128)[None, :]
    same = (r // 64) == (c // 64)
    m = np.zeros((128, 9, 128), np.float32)
    m[:, 0] = (r == c)
    m[:, 1] = 1.0
    m[:, 2] = (r <= c) & same
    m[:, 3] = (r > c)
    m[:, 4] = np.where((r > c) & same, 0.0, NEG)
    m[:, 5] = np.where((c >= r) & same, 0.0, NEG)
    m[:, 6] = same
    m[:, 7] = (r < 64) * np.ones((1, 128))
    m[:, 8] = (r >= 64) * np.ones((1, 128))
    return m


class Prog:
    def __init__(self, dbg=(), stop_after=None):
        self.dbg = set(dbg)
        self.stop_after = stop_after
        self.nc = nc = bass.Bass("TRN2", target_bir_lowering=False)
        nc.allow_low_precision("bf16 matmul operands with fp32 PSUM accumulation")
        self.ctx = ExitStack()
        with self.ctx:
            self._build()

    def I(self, name):
        if name not in self.ins:
            self.ins[name] = self.nc.dram_tensor(name, list(self.in_shapes[name]), F32, kind="ExternalInput").ap()
        return self.ins[name]

    def mm(self, out, lhsT, rhs, start, stop, reads=(), writes=()):
        self.s.op('pe', lambda e: e.matmul(out, lhsT=lhsT, rhs=rhs, start=start, stop=stop), reads=reads, writes=writes)

    def tr(self, out, in_, reads=(), writes=()):
        ident = self.cm[:, 0, :]
        self.s.op('pe', lambda e: e.transpose(out, in_, ident), reads=reads, writes=writes)

    def copy(self, eng, out, in_, reads=(), writes=()):
        if eng == 'act':
            self.s.op('act', lambda e: e.activation(out=out, in_=in_, func=AF.Copy), reads=reads, writes=writes)
        else:
            self.s.op(eng, lambda e: e.tensor_copy(out=out, in_=in_), reads=reads, writes=writes)

    def act(self, out, in_, func, reads=(), writes=(), **kw):
        self.s.op('act', lambda e: e.activation(out=out, in_=in_, func=func, **kw), reads=reads, writes=writes)

    def tt(self, out, in0, in1, op, reads=(), writes=(), eng='dve'):
        self.s.op(eng, lambda e: e.tensor_tensor(out=out, in0=in0, in1=in1, op=op), reads=reads, writes=writes)

    def ts(self, out, in0, s1, s2, op0, op1=None, reads=(), writes=(), eng='dve', **kw):
        if op1 is None:
            self.s.op(eng, lambda e: e.tensor_scalar(out=out, in0=in0, scalar1=s1, scalar2=None, op0=op0, **kw), reads=reads, writes=writes)
        else:
            self.s.op(eng, lambda e: e.tensor_scalar(out=out, in0=in0, scalar1=s1, scalar2=s2, op0=op0, op1=op1, **kw), reads=reads, writes=writes)

    def stt(self, out, in0, scalar, in1, op0, op1, reads=(), writes=(), eng='dve', **kw):
        self.s.op(eng, lambda e: e.scalar_tensor_tensor(out=out, in0=in0, scalar=scalar, in1=in1, op0=op0, op1=op1, **kw), reads=reads, writes=writes)

    def dump(self, name, ap_sb, shape, dt, key, reads):
        o = self.nc.dram_tensor("dbg_" + name, list(shape), dt, kind="ExternalOutput").ap()
        self.s.dma('sp', o, ap_sb, key='dbg', reads=reads, writes=[('dbgout', name)])
        self.dbg_names.append(name)

    def _build(self):
        nc, ctx = self.nc, self.ctx
        self.s = s = Sched(nc, ctx)
        self.dbg_names = []
        self.in_shapes = {
            "x": [S_LEN, DM], "cmat": [128, 9, 128], "biasT": [128, 12, 256], "w_in_r": [DEPTH, 128, 8, N_IN],
            "conv_r": [DEPTH, 128, 96], "headp": [DEPTH, 128, 16], "onw": [DEPTH, 128, 128],
            "w_oa_r": [DEPTH, 128, 4, DM], "w_ob_r": [DEPTH, 128, 8, DM], "w_out_r": [DEPTH, 128, 8, DM],
            "ln_r": [DEPTH, 4, 128, DM], "ffn_g_r": [D_FF // 128, 128, 8, 128], "ffn_u_r": [D_FF // 128, 128, 8, 128],
            "ffn_d": [D_FF, DM], "moe_router_r": [128, 8, NEXP], "moe_g_r": [NEXP, D_FFE // 128, 128, 8, 128],
            "moe_u_r": [NEXP, D_FFE // 128, 128, 8, 128], "moe_d": [NEXP, D_FFE, DM],
        }
        self.ins = {}
        self.y_out = nc.dram_tensor("y", [S_LEN, DM], F32, kind="ExternalOutput").ap()
        self.x1_d = nc.dram_tensor("x1_scr", [S_LEN, DM], F32).ap()
        self.x2_d = nc.dram_tensor("x2_scr", [S_LEN, DM], F32).ap()
        self.ya_d = nc.dram_tensor("ya_scr", [4, 128, S_LEN], BF16).ap()
        self.yb_d = nc.dram_tensor("yb_scr", [8, 128, S_LEN], BF16).ap()
        NW = 53200
        big = ctx.enter_context(nc.sbuf_tensor("arena", [128, NW], F32))
        self.ar = ar = Arena(big, NW)
        self.ps = [ctx.enter_context(nc.psum_tensor(f"ps{i}", [128, 512], F32)) for i in range(8)]
        self.cm = ar.alloc(9 * 128).rearrange("p (k c) -> p k c", k=9)
        self.ones_bf = ar.alloc(128, BF16)
        s.dma('sp', self.cm, self.I('cmat'), key='c1', writes=['cm'])
        self.copy('dve', self.ones_bf, self.cm[:, 1, :], reads=['cm'], writes=['ones_bf'])
        self.Gt = ar.alloc(32 * NEXP).rearrange("p (i e) -> p i e", e=NEXP)
        self.cst = ar.alloc(4)
        self.m_xT = ar.mark()
        self.xT = ar.alloc(8 * S_LEN, BF16).rearrange("p (k t) -> p k t", k=8)
        for j, v in enumerate((128.0 * RMS_EPS, RMS_EPS, LN_EPS, 0.0)):
            s.op('dve', lambda e, j=j, v=v: e.memset(self.cst[:, j:j + 1], v), writes=['cst'])
        s.barrier()

        self.phase_make_xT(self.I('x'))
        s.barrier()
        if self.stop_after == 'xT':
            self.dump("xT", self.xT[:, 0, :], [128, S_LEN], BF16, 'dbg', [])
            self.finish()
            return
        for l in range(DEPTH):
            if SKIP_MIXER:
                self.phase_ffn(l)
                s.barrier()
                break
            self.phase_attention(l)
            s.barrier()
            if self.stop_after == ('att', l):
                break
            self.phase_deltanet(l)
            s.barrier()
            if ('yb', l) in self.dbg:
                for h in range(8):
                    self.dump(f"yb{l}_{h}", self.yb_d[h], [128, S_LEN], BF16, 'dbg', [])
            if self.stop_after == ('dn', l):
                break
            self.phase_merge(l)
            s.barrier()
            if ('x1', l) in self.dbg:
                self.dump(f"x1_{l}", self.x1_d, [S_LEN, DM], F32, 'dbg', [])
            if self.stop_after == ('mix', l):
                break
            self.phase_ffn(l)
            s.barrier()
            if ('x2', l) in self.dbg:
                self.dump(f"x2_{l}", self.x2_d if l == 0 else self.y_out, [S_LEN, DM], F32, 'dbg', [])
            if self.stop_after == ('ffn', l):
                break
        self.finish()

    def finish(self):
        s = self.s
        s.barrier()
        s.emit()

    def transposes_to_xT(self, src, src_key, i, bank0, eng_pair=('act', 'dve'), xf32=None):
        for half in range(2):
            b = bank0 + half
            for q in range(4):
                kc = half * 4 + q
                self.tr(self.ps[b][:, q * 128:(q + 1) * 128], src[:, kc * 128:(kc + 1) * 128],
                        reads=[src_key], writes=[('ps', b)])
            self.copy(eng_pair[half], self.xT[:, half * 4:(half + 1) * 4, i * 128:(i + 1) * 128],
                      self.ps[b][:, :].rearrange("p (k t) -> p k t", k=4),
                      reads=[('ps', b)], writes=[('xT', i)])
            if xf32 is not None:
                self.copy(eng_pair[1 - half], xf32[0][:, half * 4:(half + 1) * 4, :],
                          self.ps[b][:, :].rearrange("p (k t) -> p k t", k=4),
                          reads=[('ps', b)], writes=[xf32[1]])

    def phase_make_xT(self, src):
        s, ar = self.s, self.ar
        m = ar.mark()
        xl = [ar.alloc(DM) for _ in range(2)]
        for i in range(NTILE):
            sl = i % 2
            s.dma('sp', xl[sl], src[i * 128:(i + 1) * 128, :], key=f'xl{sl}', writes=[('xl', sl)])
            self.transposes_to_xT(xl[sl], ('xl', sl), i, bank0=(i % 2) * 2)
        ar.release(m)

    def phase_attention(self, l):
        s, ar, ps, xT = self.s, self.ar, self.ps, self.xT
        m = ar.mark()
        biasT = ar.alloc(12 * 256).rearrange("p (h c) -> p h c", h=12)
        wu = [ar.alloc(8 * 384, BF16).rearrange("p (k c) -> p k c", k=8) for _ in range(2)]
        qT = ar.alloc(S_LEN, BF16)
        kT = ar.alloc(S_LEN, BF16)
        V = ar.alloc(32 * 128, BF16).rearrange("p (b e) -> p b e", b=32)
        OD = ar.alloc(2 * S_LEN).rearrange("p (two t) -> p two t", two=2)
        PT = [ar.alloc(256, BF16) for _ in range(4)]
        tmp = [ar.alloc(256) for _ in range(2)]
        yst = ar.alloc(S_LEN, BF16)
        s.dma('sp', biasT, self.I('biasT'), key='c2', writes=['biasT'])
        scale = 128.0 ** -0.5
        for hs in range(4):
            for g in range(3):
                d = (1, 4, 16)[g]
                nb = 32 // d
                h = g * 4 + hs
                u = hs * 3 + g
                sl = u % 2
                s.dma('pool', wu[sl], self.I('w_in_r')[l][:, :, u * 384:(u + 1) * 384], key=f'wu{sl}', writes=[('wu', sl)])
                for tt in range(8):
                    for X in range(2):
                        bank = X
                        for kc in range(8):
                            self.mm(ps[bank][:, :], wu[sl][:, kc, X * 128:(X + 1) * 128], xT[:, kc, tt * 512:(tt + 1) * 512],
                                    kc == 0, kc == 7, reads=[('wu', sl)], writes=[('ps', bank)])
                        dst = (qT if X == 0 else kT).rearrange("p (r i) -> p r i", r=d)[:, :, (512 // d) * tt:(512 // d) * (tt + 1)]
                        src = ps[bank][:, :].rearrange("p (j r) -> p r j", r=d)
                        self.copy('act' if X == 0 else 'dve', dst, src, reads=[('ps', bank)], writes=['qT' if X == 0 else 'kT'])
                if ATT_LIMIT < 1:
                    continue
                for b4 in range(8):
                    bank = 2 + b4 % 2
                    for bb in range(4):
                        b = b4 * 4 + bb
                        r, c = divmod(b, nb)
                        t0 = 128 * c * d + r
                        for kc in range(8):
                            self.mm(ps[bank][:, bb * 128:(bb + 1) * 128], xT[:, kc, t0:t0 + 127 * d + 1:d], wu[sl][:, kc, 256:384],
                                    kc == 0, kc == 7, reads=[('wu', sl)], writes=[('ps', bank)])
                    self.copy('act' if b4 % 2 == 0 else 'dve', V[:, b4 * 4:(b4 + 1) * 4, :],
                              ps[bank][:, :].rearrange("p (b e) -> p b e", b=4), reads=[('ps', bank)], writes=[('V', b4)])
                if ATT_LIMIT < 2:
                    continue
                ODv = OD.rearrange("p two (i r) -> p two i r", r=d)

                def s_stage(b):
                    r, c = divmod(b, nb)
                    nq = 256 if c + 1 < nb else 128
                    bank = 4 + b % 2
                    sps = ps[bank][:, 0:nq]
                    self.mm(sps, kT[:, b * 128:(b + 1) * 128], qT[:, b * 128:b * 128 + nq], True, True,
                            reads=['qT', 'kT'], writes=[('ps', bank)])
                    tsl = b % 2
                    self.stt(tmp[tsl][:, :nq], sps, scale, biasT[:, h, :nq], ALU.mult, ALU.add,
                             reads=[('ps', bank), 'biasT'], writes=[('tmp', tsl)])
                    self.act(PT[b % 4][:, :nq], tmp[tsl][:, :nq], AF.Exp, reads=[('tmp', tsl)], writes=[('PT', b % 4)])

                def pv_stage(b):
                    r, c = divmod(b, nb)
                    bank = 6 + b % 2
                    ops_ = ps[bank][:, 0:256]
                    for which in range(2):
                        o_ = ops_[:, which * 128:(which + 1) * 128]
                        if c > 0:
                            lh = V[:, b - 1, :] if which == 0 else self.ones_bf
                            self.mm(o_, lh, PT[(b - 1) % 4][:, 128:256], True, False,
                                    reads=[('V', (b - 1) // 4), ('PT', (b - 1) % 4)], writes=[('ps', bank)])
                        lh = V[:, b, :] if which == 0 else self.ones_bf
                        self.mm(o_, lh, PT[b % 4][:, 0:128], c == 0, True,
                                reads=[('V', b // 4), ('PT', b % 4)], writes=[('ps', bank)])
                    view = ODv[:, :, 128 * c:128 * c + 128, r]
                    if g == 0:
                        regs = [('OD', b // 4)]
                    elif g == 1:
                        regs = [('OD', c)]
                    else:
                        regs = [('OD', 4 * c + j) for j in range(4)]
                    src = ops_.rearrange("p (two t) -> p two t", two=2)
                    if g == 0:
                        self.copy('act', view, src, reads=[('ps', bank)], writes=regs)
                    else:
                        self.tt(view, src, view, ALU.add, reads=[('ps', bank)] + regs, writes=regs)

                s_stage(0)
                for b in range(32):
                    if b + 1 < 32:
                        s_stage(b + 1)
                    pv_stage(b)
            if ATT_LIMIT < 3:
                self.dump(f"q{hs}", qT, [128, S_LEN], BF16, 'dbg', ['qT'])
                self.dump(f"v{hs}", V.rearrange("p b e -> p (b e)"), [128, S_LEN], BF16, 'dbg', [('V', i) for i in range(8)])
                continue
            for rg in range(8):
                cs = slice(rg * 512, (rg + 1) * 512)
                self.s.op('dve', lambda e, cs=cs: e.reciprocal(out=OD[:, 1, cs], in_=OD[:, 1, cs]), reads=[('OD', rg)], writes=[('OD', rg)])
                self.tt(yst[:, cs], OD[:, 0, cs], OD[:, 1, cs], ALU.mult, reads=[('OD', rg)], writes=['yst'])
            s.dma('sp', self.ya_d[hs], yst, key='yst', reads=['yst'], writes=[('ya_d', hs)])
            if ('ya', l) in self.dbg:
                self.dump(f"ya{l}_{hs}", yst, [128, S_LEN], BF16, 'dbg', ['yst'])
        ar.release(m)


    def phase_deltanet(self, l):
        import itertools
        s, ar, ps, xT, cm = self.s, self.ar, self.ps, self.xT, self.cm
        m = ar.mark()
        ident, ones, U, Ls, MS, MIT, BD, H0, H1 = (cm[:, k, :] for k in range(9))
        cw = ar.alloc(96)
        hp = ar.alloc(16)
        onw = ar.alloc(128)
        negA = ar.alloc(8)
        s.dma('sp', cw, self.I('conv_r')[l], key='c3', writes=['cw'])
        s.dma('sp', hp, self.I('headp')[l], key='c4', writes=['hp'])
        s.dma('sp', onw, self.I('onw')[l], key='c5', writes=['onw'])
        self.ts(onw, onw, 128.0 ** 0.5, None, ALU.mult, reads=['onw'], writes=['onw'])
        self.act(negA, hp[:, 0:8], AF.Exp, reads=['hp'], writes=['negA'])
        self.ts(negA, negA, -1.0, None, ALU.mult, reads=['negA'], writes=['negA'])
        wba = ar.alloc(8 * 16, BF16).rearrange("p (k c) -> p k c", k=8)
        s.dma('pool', wba, self.I('w_in_r')[l][:, :, OFF_BA:OFF_BA + 16], key='wba', writes=['wba'])
        a8 = lambda: ar.alloc(256).rearrange("p (i h) -> p i h", h=8)
        BETA, NB, Gs, EG, ED, BEG = a8(), a8(), a8(), a8(), a8(), a8()
        GLB = ar.alloc(512).rearrange("p (c h) -> p c h", h=8)
        m_small = ar.mark()
        GG = ar.alloc(512).rearrange("p (i c) -> p i c", c=16)
        ba = ar.alloc(512).rearrange("p (i c) -> p i c", c=16)
        for i in range(32):
            for kc in range(8):
                self.mm(ps[0][:, i * 16:(i + 1) * 16], xT[:, kc, i * 128:(i + 1) * 128], wba[:, kc, :], kc == 0, kc == 7,
                        reads=['wba'], writes=[('ps', 0)])
        self.copy('dve', ba, ps[0][:, :].rearrange("p (i c) -> p i c", c=16), reads=[('ps', 0)], writes=['ba'])
        bc = lambda v: v.unsqueeze(1).to_broadcast([128, 32, 8])
        self.act(BETA, ba[:, :, 0:8], AF.Sigmoid, reads=['ba'], writes=['BETA'])
        self.ts(NB, BETA, -1.0, None, ALU.mult, reads=['BETA'], writes=['NB'])
        self.tt(Gs, ba[:, :, 8:16], bc(hp[:, 8:16]), ALU.add, reads=['ba', 'hp'], writes=['Gs'])
        self.act(Gs, Gs, AF.Exp, reads=['Gs'], writes=['Gs'])
        self.act(Gs, Gs, AF.Ln, bias=1.0, reads=['Gs'], writes=['Gs'])
        self.tt(Gs, Gs, bc(negA), ALU.mult, reads=['Gs', 'negA'], writes=['Gs'])
        for i in range(32):
            self.mm(ps[1][:, i * 16:i * 16 + 8], U, Gs[:, i, :], True, True, reads=['Gs'], writes=[('ps', 1)])
            self.mm(ps[1][:, i * 16 + 8:i * 16 + 16], BD, Gs[:, i, :], True, True, reads=['Gs'], writes=[('ps', 1)])
            self.mm(ps[2][:, (2 * i) * 8:(2 * i) * 8 + 8], H0, Gs[:, i, :], True, True, reads=['Gs'], writes=[('ps', 2)])
            self.mm(ps[2][:, (2 * i + 1) * 8:(2 * i + 1) * 8 + 8], H1, Gs[:, i, :], True, True, reads=['Gs'], writes=[('ps', 2)])
        self.copy('dve', GG, ps[1][:, :].rearrange("p (i c) -> p i c", c=16), reads=[('ps', 1)], writes=['GG'])
        self.act(GLB, ps[2][:, :].rearrange("p (c h) -> p c h", h=8), AF.Exp, reads=[('ps', 2)], writes=['GLB'])
        self.act(EG, GG[:, :, 0:8], AF.Exp, reads=['GG'], writes=['EG'])
        self.tt(ED, GG[:, :, 8:16], GG[:, :, 0:8], ALU.subtract, reads=['GG'], writes=['ED'])
        self.act(ED, ED, AF.Exp, reads=['ED'], writes=['ED'])
        self.tt(BEG, BETA, EG, ALU.mult, reads=['BETA', 'EG'], writes=['BEG'])

        s.barrier()
        ar.release(m_small)
        bankrot = itertools.cycle(range(2, 8))
        w_in_r = self.I('w_in_r')
        evrot = itertools.cycle(('act', 'dve'))
        m4 = lambda: [ar.alloc(128) for _ in range(4)]

        def make_bufs(hs):
            B = dict(hs=hs)
            B['wu'] = ar.alloc(8 * 512, BF16).rearrange("p (k c) -> p k c", k=8)
            B['S'] = ar.alloc(128)
            B['pre'] = [ar.alloc(3 + 512) for _ in range(3)]
            B['hist'] = [ar.alloc(4) for _ in range(3)]
            B['cv'] = [ar.alloc(512) for _ in range(3)]
            B['sets'] = [dict(u0=m4(), wT=m4(), qkD=m4(), Kdec=m4(), QTn=ar.alloc(512),
                              zs=ar.alloc(512).rearrange("p (a e) -> p a e", a=4), idx=k) for k in range(2)]
            for nm in ('gU', 'Ds', 'DTi', 'Za', 'Ya', 'TTa', 'Kbg', 'Vb'):
                B[nm] = m4()
            B['ub'], B['osb'], B['onb'] = ar.alloc(128), ar.alloc(128), ar.alloc(128)
            B['ssq'], B['rn1'] = ar.alloc(1), ar.alloc(1)
            B['ybst'] = [ar.alloc(512, BF16)] * 2
            return B

        def stage(mms, evs):
            b = next(bankrot)
            for pp in range(4):
                mms(pp, ps[b][:, pp * 128:(pp + 1) * 128], ('ps', b))
            for pp in range(4):
                evs(pp, ps[b][:, pp * 128:(pp + 1) * 128], ('ps', b))

        def prep(h, sg, st, B):
            hs = B['hs']
            K = lambda *a: (hs,) + a
            t0 = sg * 512
            k_ = st['idx']
            W = B['wu']
            wk = K('wu')
            pre, cv, hist = B['pre'], B['cv'], B['hist']
            gU, Ds, DTi, Kbg, Vb = B['gU'], B['Ds'], B['DTi'], B['Kbg'], B['Vb']
            for X in range(3):
                bank = X % 2
                for kc in range(8):
                    self.mm(ps[bank][:, :], W[:, kc, X * 128:(X + 1) * 128], xT[:, kc, t0:t0 + 512], kc == 0, kc == 7,
                            reads=[wk], writes=[('ps', bank)])
                if sg > 0:
                    self.copy('dve', pre[X][:, 0:3], hist[X][:, 0:3], reads=[K('hist', X)], writes=[K('pre', X)])
                else:
                    self.s.op('dve', lambda e, X=X: e.memset(pre[X][:, 0:3], 0.0), writes=[K('pre', X)])
                self.copy('act', pre[X][:, 3:515], ps[bank][:, :], reads=[('ps', bank)], writes=[K('pre', X)])
                wc = lambda k: cw[:, h * 12 + X * 4 + k:h * 12 + X * 4 + k + 1]
                self.ts(cv[X], pre[X][:, 0:512], wc(0), None, ALU.mult, reads=[K('pre', X)], writes=[K('cv', X)])
                for k in range(1, 4):
                    self.stt(cv[X], pre[X][:, k:k + 512], wc(k), cv[X], ALU.mult, ALU.add,
                             reads=[K('pre', X), K('cv', X)], writes=[K('cv', X)])
                self.copy('dve', hist[X][:, 0:3], pre[X][:, 512:515], reads=[K('pre', X)], writes=[K('hist', X)])
                self.act(cv[X], cv[X], AF.Silu, reads=[K('cv', X)], writes=[K('cv', X)])
                yield
            for X in range(2):
                sqb = pre[X][:, 3:515]
                sqh = pre[X][:, 3:259].bitcast(BF16)
                self.act(sqh, cv[X], AF.Square, scale=(128.0 ** 0.5 if X == 0 else 1.0), reads=[K('cv', X)], writes=[K('pre', X)])
                self.mm(ps[X][:, :], self.ones_bf, sqh, True, True, reads=[K('pre', X)], writes=[('ps', X)])
                self.act(sqb, ps[X][:, :], AF.Sqrt, bias=self.cst[:, X:X + 1], reads=[('ps', X)], writes=[K('pre', X)])
                self.s.op('dve', lambda e, sqb=sqb: e.reciprocal(out=sqb, in_=sqb), reads=[K('pre', X)], writes=[K('pre', X)])
                if X == 0:
                    self.tt(st['QTn'], cv[0], sqb, ALU.mult, reads=[K('cv', 0), K('pre', 0)], writes=[K('QTn', k_)])
                else:
                    self.tt(cv[1], cv[1], sqb, ALU.mult, reads=[K('cv', 1), K('pre', 1)], writes=[K('cv', 1)])
                yield
            KTn, VTs, QTn = cv[1], cv[2], st['QTn']
            b = next(bankrot)
            for kc in range(8):
                self.mm(ps[b][:, :], W[:, kc, 384:512], xT[:, kc, t0:t0 + 512], kc == 0, kc == 7, reads=[wk], writes=[('ps', b)])
            self.act(st['zs'], ps[b][:, :].rearrange("p (a e) -> p a e", a=4), AF.Silu, reads=[('ps', b)], writes=[K('zs', k_)])
            yield
            ti = lambda pp: sg * 4 + pp
            col = lambda A_, pp: A_[:, ti(pp), h:h + 1]
            pc = lambda pp: slice(pp * 128, (pp + 1) * 128)
            stage(lambda pp, q, bk: self.tr(q, KTn[:, pc(pp)], reads=[K('cv', 1)], writes=[bk]),
                  lambda pp, q, bk: (self.ts(Kbg[pp], q, col(BEG, pp), None, ALU.mult, reads=[bk], writes=[K('Kbg', pp)]),
                                     self.act(st['Kdec'][pp], q, AF.Identity, scale=col(ED, pp), reads=[bk], writes=[K('Kdec', k_, pp)])))
            yield
            stage(lambda pp, q, bk: self.tr(q, VTs[:, pc(pp)], reads=[K('cv', 2)], writes=[bk]),
                  lambda pp, q, bk: self.ts(Vb[pp], q, col(BETA, pp), None, ALU.mult, reads=[bk], writes=[K('Vb', pp)]))
            yield
            for pp in range(4):
                self.ts(gU[pp], U, col(Gs, pp), None, ALU.mult, writes=[K('gU', pp)])
            stage(lambda pp, q, bk: (self.mm(q, gU[pp], Ls, True, False, reads=[K('gU', pp)], writes=[bk]),
                                     self.mm(q, ident, MS, False, True, writes=[bk])),
                  lambda pp, q, bk: self.act(Ds[pp], q, AF.Exp, reads=[bk], writes=[K('Ds', pp)]))
            yield
            stage(lambda pp, q, bk: (self.mm(q, Ls, gU[pp], True, False, reads=[K('gU', pp)], writes=[bk]),
                                     self.mm(q, ident, MIT, False, True, writes=[bk])),
                  lambda pp, q, bk: self.act(DTi[pp], q, AF.Exp, reads=[bk], writes=[K('DTi', pp)]))
            yield
            stage(lambda pp, q, bk: self.mm(q, KTn[:, pc(pp)], QTn[:, pc(pp)], True, True, reads=[K('cv', 1), K('QTn', k_)], writes=[bk]),
                  lambda pp, q, bk: self.tt(st['qkD'][pp], q, DTi[pp], ALU.mult, reads=[bk, K('DTi', pp)], writes=[K('qkD', k_, pp)]))
            yield
            Z, Y, TT = [B['Za'], gU], [B['Ya'], Ds], [B['TTa'], DTi]
            Zk = [lambda pp: K('Za', pp), lambda pp: K('gU', pp)]
            Yk = [lambda pp: K('Ya', pp), lambda pp: K('Ds', pp)]
            TTk = [lambda pp: K('TTa', pp), lambda pp: K('DTi', pp)]
            stage(lambda pp, q, bk: self.mm(q, KTn[:, pc(pp)], KTn[:, pc(pp)], True, True, reads=[K('cv', 1)], writes=[bk]),
                  lambda pp, q, bk: self.stt(Z[0][pp], q, col(NB, pp), Ds[pp], ALU.mult, ALU.mult,
                                             reads=[bk, K('Ds', pp)], writes=[Zk[0](pp)]))
            yield
            stage(lambda pp, q, bk: self.tr(q, Z[0][pp], reads=[Zk[0](pp)], writes=[bk]),
                  lambda pp, q, bk: (self.copy('act', Y[0][pp], q, reads=[bk], writes=[Yk[0](pp)]),
                                     self.tt(TT[0][pp], q, ident, ALU.add, reads=[bk], writes=[TTk[0](pp)])))
            yield
            cur = 0
            for k in range(1, 6):
                nxt = 1 - cur
                stage(lambda pp, q, bk: self.mm(q, Y[cur][pp], Z[cur][pp], True, True, reads=[Yk[cur](pp), Zk[cur](pp)], writes=[bk]),
                      lambda pp, q, bk: self.copy(next(evrot), Z[nxt][pp], q, reads=[bk], writes=[Zk[nxt](pp)]))
                if k < 5:
                    stage(lambda pp, q, bk: self.mm(q, Z[cur][pp], Y[cur][pp], True, True, reads=[Yk[cur](pp), Zk[cur](pp)], writes=[bk]),
                          lambda pp, q, bk: self.copy(next(evrot), Y[nxt][pp], q, reads=[bk], writes=[Yk[nxt](pp)]))
                yield
                stage(lambda pp, q, bk: self.mm(q, Z[nxt][pp], TT[cur][pp], True, True, reads=[Zk[nxt](pp), TTk[cur](pp)], writes=[bk]),
                      lambda pp, q, bk: self.tt(TT[nxt][pp], q, TT[cur][pp], ALU.add, reads=[bk, TTk[cur](pp)], writes=[TTk[nxt](pp)]))
                cur = nxt
                yield
            TTf, TTfk = TT[cur], TTk[cur]
            stage(lambda pp, q, bk: self.mm(q, TTf[pp], Vb[pp], True, True, reads=[TTfk(pp), K('Vb', pp)], writes=[bk]),
                  lambda pp, q, bk: self.copy(next(evrot), st['u0'][pp], q, reads=[bk], writes=[K('u0', k_, pp)]))
            yield
            stage(lambda pp, q, bk: self.mm(q, Kbg[pp], TTf[pp], True, True, reads=[TTfk(pp), K('Kbg', pp)], writes=[bk]),
                  lambda pp, q, bk: self.copy(next(evrot), st['wT'][pp], q, reads=[bk], writes=[K('wT', k_, pp)]))
            yield

        def scan(h, sg, st, B, ysl):
            hs = B['hs']
            K = lambda *a: (hs,) + a
            t0 = sg * 512
            k_ = st['idx']
            Sst, ub, osb, onb, ssq, rn1, ybst = B['S'], B['ub'], B['osb'], B['onb'], B['ssq'], B['rn1'], B['ybst']
            for pp in range(4):
                i = sg * 4 + pp
                for c in range(2):
                    rows = slice(64 * c, 64 * c + 64)
                    b1 = next(bankrot)
                    self.mm(ps[b1][rows, 0:128], st['wT'][pp][:, 64 * c:64 * c + 64], Sst, True, True,
                            reads=[K('wT', k_, pp), K('S')], writes=[('ps', b1)])
                    self.mm(ps[b1][rows, 128:256], st['QTn'][:, pp * 128 + 64 * c:pp * 128 + 64 * c + 64], Sst, True, True,
                            reads=[K('QTn', k_), K('S')], writes=[('ps', b1)])
                    self.tt(ub[rows, :], st['u0'][pp][rows, :], ps[b1][rows, 0:128], ALU.subtract,
                            reads=[('ps', b1), K('u0', k_, pp)], writes=[K('ub')])
                    self.act(osb[rows, :], ps[b1][rows, 128:256], AF.Identity, scale=EG[rows, i, h:h + 1],
                             reads=[('ps', b1)], writes=[K('osb')])
                    yield
                    b2 = next(bankrot)
                    self.mm(ps[b2][:, 0:128], st['Kdec'][pp][rows, :], ub[rows, :], True, True,
                            reads=[K('ub'), K('Kdec', k_, pp)], writes=[('ps', b2)])
                    self.stt(Sst, Sst, GLB[:, 2 * i + c, h:h + 1], ps[b2][:, 0:128], ALU.mult, ALU.add,
                             reads=[('ps', b2), K('S')], writes=[K('S')])
                    yield
                b3 = next(bankrot)
                self.mm(ps[b3][:, 0:128], st['qkD'][pp], ub, True, True, reads=[K('ub'), K('qkD', k_, pp)], writes=[('ps', b3)])
                self.tt(osb, osb, ps[b3][:, 0:128], ALU.add, reads=[('ps', b3), K('osb')], writes=[K('osb')])
                self.s.op('dve', lambda e, ssq=ssq: e.memset(ssq, 0.0), writes=[K('ssq')])
                self.act(onb, osb, AF.Square, accum_out=ssq, reads=[K('osb'), K('ssq')], writes=[K('onb'), K('ssq')])
                self.act(rn1, ssq, AF.Sqrt, bias=self.cst[:, 0:1], reads=[K('ssq')], writes=[K('rn1')])
                self.s.op('dve', lambda e, rn1=rn1: e.reciprocal(out=rn1, in_=rn1), reads=[K('rn1')], writes=[K('rn1')])
                self.stt(onb, osb, rn1[:, 0:1], onw, ALU.mult, ALU.mult, reads=[K('osb'), K('rn1')], writes=[K('onb')])
                b4 = next(bankrot)
                self.tr(ps[b4][:, 0:128], onb, reads=[K('onb')], writes=[('ps', b4)])
                self.tt(ybst[ysl][:, pp * 128:(pp + 1) * 128], ps[b4][:, 0:128], st['zs'][:, pp, :], ALU.mult,
                        reads=[('ps', b4), K('zs', k_)], writes=[K('ybst')])
                yield
            self.s.dma('sp', self.yb_d[h][:, t0:t0 + 512], ybst[ysl], key=f'ybst{hs}', reads=[K('ybst')], writes=[('yb_d', h, sg)])

        def merged(g1, g2):
            gens = [g for g in (g1, g2) if g is not None]
            while gens:
                for g in list(gens):
                    try:
                        next(g)
                        yield
                    except StopIteration:
                        gens.remove(g)

        def chain(h, B):
            hs = B['hs']
            s.dma('pool', B['wu'], w_in_r[l][:, :, OFF_DN + h * 512:OFF_DN + (h + 1) * 512], key=f'dwu{hs}', writes=[(hs, 'wu')])
            self.s.op('dve', lambda e: e.memset(B['S'], 0.0), reads=[(hs, 'S')], writes=[(hs, 'S')])
            nsg = 8 if DN_LIMIT >= 3 else 1
            yield from prep(h, 0, B['sets'][0], B)
            for sg in range(nsg):
                nxt = prep(h, sg + 1, B['sets'][(sg + 1) % 2], B) if sg + 1 < nsg else None
                yield from merged(scan(h, sg, B['sets'][sg % 2], B, sg % 2), nxt)

        NPAR = 2
        bufs = [make_bufs(k) for k in range(NPAR)]
        heads = list(range(8 if DN_LIMIT >= 9 else (NPAR if DN_LIMIT >= 1 else 0)))
        for h0 in range(0, len(heads), NPAR):
            gens = [chain(h0 + k, bufs[k]) for k in range(NPAR) if h0 + k < len(heads)]
            for _ in merged(*gens) if len(gens) == 2 else gens[0]:
                pass
        ar.release(m)

    def ln_tile(self, res, res_key, add_views, add_keys, gam, bet, out, out_key, tbufs, statss, ls):
        tbuf, stats = tbufs[ls], statss[ls]
        kt, kst = ('ln_t', ls), ('ln_st', ls)
        s1, nm, ss, rs = (stats[:, j:j + 1] for j in range(4))
        for hf in range(2):
            cs = slice(hf * 512, (hf + 1) * 512)
            self.stt(tbuf[:, cs], res[:, cs], ALPHA, add_views[hf], ALU.mult, ALU.add,
                     reads=[res_key, add_keys[hf]], writes=[kt])
        self.s.op('dve', lambda e: e.memset(stats[:, 0:4], 0.0), writes=[kst])
        self.act(out, tbuf, AF.Identity, accum_out=s1, reads=[kt, kst], writes=[out_key, kst])
        self.act(nm, s1, AF.Identity, scale=-1.0 / DM, reads=[kst], writes=[kst])
        self.act(out, tbuf, AF.Square, bias=nm, accum_out=ss, reads=[kt, kst], writes=[out_key, kst])
        self.act(rs, ss, AF.Sqrt, bias=self.cst[:, 2:3], scale=1.0 / DM, reads=[kst], writes=[kst])
        self.s.op('dve', lambda e: e.reciprocal(out=rs, in_=rs), reads=[kst], writes=[kst])
        self.ts(tbuf, tbuf, nm, rs, ALU.add, ALU.mult, reads=[kt, kst], writes=[kt])
        self.tt(tbuf, tbuf, gam, ALU.mult, reads=[kt, 'lnp'], writes=[kt])
        self.tt(out, tbuf, bet, ALU.add, reads=[kt, 'lnp'], writes=[out_key])

    def phase_merge(self, l):
        import itertools
        s, ar, ps, xT = self.s, self.ar, self.ps, self.xT
        m = ar.mark()
        moe = (l % 2 == 1)
        w_in_r = self.I('w_in_r')
        Wga = ar.alloc(8 * DM, BF16).rearrange("p (k c) -> p k c", k=8)
        Wgb = ar.alloc(8 * DM, BF16).rearrange("p (k c) -> p k c", k=8)
        Woa = ar.alloc(4 * DM, BF16).rearrange("p (k c) -> p k c", k=4)
        Wob = ar.alloc(8 * DM, BF16).rearrange("p (k c) -> p k c", k=8)
        Wout = ar.alloc(8 * DM, BF16).rearrange("p (k c) -> p k c", k=8)
        gam, bet = ar.alloc(DM), ar.alloc(DM)
        s.dma('pool', Wga, w_in_r[l][:, :, OFF_GA:OFF_GA + DM], key='mw0', writes=['Wga'])
        s.dma('pool', Woa, self.I('w_oa_r')[l], key='mw1', writes=['Woa'])
        s.dma('pool', Wgb, w_in_r[l][:, :, OFF_GB:OFF_GB + DM], key='mw2', writes=['Wgb'])
        s.dma('pool', Wob, self.I('w_ob_r')[l], key='mw3', writes=['Wob'])
        s.dma('pool', Wout, self.I('w_out_r')[l], key='mw4', writes=['Wout'])
        s.dma('sp', gam, self.I('ln_r')[l][0], key='c6', writes=['lnp'])
        s.dma('sp', bet, self.I('ln_r')[l][1], key='c7', writes=['lnp'])
        TB = 512
        yaT = [ar.alloc(4 * TB, BF16).rearrange("p (k t) -> p k t", k=4)] * 2
        ybT = [ar.alloc(8 * TB, BF16).rearrange("p (k t) -> p k t", k=8) for _ in range(2)]
        mT = ar.alloc(8 * TB, BF16).rearrange("p (k t) -> p k t", k=8)
        sg = [ar.alloc(TB) for _ in range(2)]
        xres = [ar.alloc(DM)] * 2
        tbufs = [ar.alloc(DM) for _ in range(2)]
        xo = [ar.alloc(DM) for _ in range(2)]
        statss = [ar.alloc(8) for _ in range(2)]
        if moe:
            x1Tf = ar.alloc(8 * 128).rearrange("p (k t) -> p k t", k=8)
            wr = ar.alloc(8 * NEXP).rearrange("p (k e) -> p k e", k=8)
            s.dma('sp', wr, self.I('moe_router_r'), key='c8', writes=['wr'])
            L, Lm, mk, E = ar.alloc(8), ar.alloc(8), ar.alloc(8), ar.alloc(8)
            gst = ar.alloc(8)
        src_res = self.I('x') if l == 0 else self.x2_d
        brot = itertools.cycle(range(0, 4))
        ya_v = self.ya_d.rearrange("k p t -> p k t")
        yb_v = self.yb_d.rearrange("k p t -> p k t")
        nblk = S_LEN // TB
        for blk in range(nblk):
            bs = slice(blk * TB, (blk + 1) * TB)
            sl = blk % 2
            s.dma('sp', yaT[sl], ya_v[:, :, bs], key='ya0', writes=[('yaT', 0)])
            s.dma('sp', ybT[sl], yb_v[:, :, bs], key=f'yb{sl}', writes=[('ybT', sl)])
            for oc in range(8):
                ocs = slice(oc * 128, (oc + 1) * 128)
                for br in range(2):
                    Wg, Wo, yT, nk = (Wga, Woa, yaT[sl], 4) if br == 0 else (Wgb, Wob, ybT[sl], 8)
                    wgk, wok, yk = (('Wga', 'Woa', ('yaT', 0)) if br == 0 else ('Wgb', 'Wob', ('ybT', sl)))
                    b1 = next(brot)
                    for kc in range(8):
                        self.mm(ps[b1][:, 0:TB], Wg[:, kc, ocs], xT[:, kc, bs], kc == 0, kc == 7,
                                reads=[wgk, ('xTb', blk)], writes=[('ps', b1)])
                    self.act(sg[br], ps[b1][:, 0:TB], AF.Sigmoid, reads=[('ps', b1)], writes=[('sg', br)])
                    b2 = next(brot)
                    for kc in range(nk):
                        self.mm(ps[b2][:, 0:TB], Wo[:, kc, ocs], yT[:, kc, :], kc == 0, kc == nk - 1,
                                reads=[wok, yk], writes=[('ps', b2)])
                    self.tt(sg[br], sg[br], ps[b2][:, 0:TB], ALU.mult, reads=[('sg', br), ('ps', b2)], writes=[('sg', br)])
                    if br == 1:
                        self.tt(mT[:, oc, :], sg[0], sg[1], ALU.add, reads=[('sg', 0), ('sg', 1)], writes=['mT'])
            for sub in range(TB // 128):
                i = blk * (TB // 128) + sub
                xs = i % 2
                s.dma('sp', xres[xs], src_res[i * 128:(i + 1) * 128, :], key='xr0', writes=[('xres', 0)])
                for hf in range(2):
                    for kc in range(8):
                        self.mm(ps[4 + hf][:, :], mT[:, kc, sub * 128:(sub + 1) * 128], Wout[:, kc, hf * 512:(hf + 1) * 512],
                                kc == 0, kc == 7, reads=['mT', 'Wout'], writes=[('ps', 4 + hf)])
                self.ln_tile(xres[xs], ('xres', 0), [ps[4][:, :], ps[5][:, :]], [('ps', 4), ('ps', 5)], gam, bet,
                             xo[xs], ('xo', xs), tbufs, statss, xs)
                s.dma('sp', self.x1_d[i * 128:(i + 1) * 128, :], xo[xs], key=f'xo{xs}', reads=[('xo', xs)], writes=[('x1_d', i)])
                for hf in range(2):
                    b = 6 + hf
                    for q in range(4):
                        kc = hf * 4 + q
                        self.tr(ps[b][:, q * 128:(q + 1) * 128], xo[xs][:, kc * 128:(kc + 1) * 128], reads=[('xo', xs)], writes=[('ps', b)])
                    self.copy('act' if hf == 0 else 'dve', xT[:, hf * 4:(hf + 1) * 4, i * 128:(i + 1) * 128],
                              ps[b][:, :].rearrange("p (k t) -> p k t", k=4), reads=[('ps', b)], writes=[('xTb', blk)])
                    if moe:
                        self.copy('dve' if hf == 0 else 'act', x1Tf[:, hf * 4:(hf + 1) * 4, :],
                                  ps[b][:, :].rearrange("p (k t) -> p k t", k=4), reads=[('ps', b)], writes=['x1Tf'])
                if moe:
                    for kc in range(8):
                        self.mm(ps[6][:, 0:NEXP], x1Tf[:, kc, :], wr[:, kc, :], kc == 0, kc == 7, reads=['x1Tf', 'wr'], writes=[('ps', 6)])
                    self.copy('dve', L, ps[6][:, 0:NEXP], reads=[('ps', 6)], writes=['L'])
                    m1, m2, den = gst[:, 0:1], gst[:, 1:2], gst[:, 2:3]
                    self.s.op('dve', lambda e, m1=m1: e.tensor_reduce(out=m1, in_=L, axis=AX.X, op=ALU.max), reads=['L'], writes=['gst'])
                    self.ts(Lm, L, m1, None, ALU.subtract, reads=['L', 'gst'], writes=['Lm'])
                    self.ts(mk, Lm, 0.0, None, ALU.is_equal, reads=['Lm'], writes=['mk'])
                    self.stt(mk, mk, -1e30, Lm, ALU.mult, ALU.add, reads=['mk', 'Lm'], writes=['mk'])
                    self.s.op('dve', lambda e, m2=m2: e.tensor_reduce(out=m2, in_=mk, axis=AX.X, op=ALU.max), reads=['mk'], writes=['gst'])
                    self.ts(mk, Lm, m2, None, ALU.is_ge, reads=['Lm', 'gst'], writes=['mk'])
                    self.act(E, Lm, AF.Exp, reads=['Lm'], writes=['E'])
                    self.tt(E, E, mk, ALU.mult, reads=['E', 'mk'], writes=['E'])
                    self.s.op('dve', lambda e, den=den: e.tensor_reduce(out=den, in_=E, axis=AX.X, op=ALU.add), reads=['E'], writes=['gst'])
                    self.s.op('dve', lambda e, den=den: e.reciprocal(out=den, in_=den), reads=['gst'], writes=['gst'])
                    self.ts(self.Gt[:, i, :], E, den, None, ALU.mult, reads=['E', 'gst'], writes=['Gt'])
        ar.release(m)

    def phase_ffn(self, l):
        import itertools
        s, ar, ps, xT = self.s, self.ar, self.ps, self.xT
        m = ar.mark()
        moe = (l % 2 == 1)
        last = (l == DEPTH - 1)
        if moe:
            nexp, nch = NEXP, D_FFE // 128
            gsrc = lambda e, c0, n: self.I('moe_g_r')[e][c0:c0 + n].rearrange("c p k j -> p c (k j)")
            usrc = lambda e, c0, n: self.I('moe_u_r')[e][c0:c0 + n].rearrange("c p k j -> p c (k j)")
            dsrc = lambda e, c0, n: self.I('moe_d')[e][c0 * 128:(c0 + n) * 128, :].rearrange("(c p) n -> p c n", p=128)
        else:
            nexp, nch = 1, D_FF // 128
            gsrc = lambda e, c0, n: self.I('ffn_g_r')[c0:c0 + n].rearrange("c p k j -> p c (k j)")
            usrc = lambda e, c0, n: self.I('ffn_u_r')[c0:c0 + n].rearrange("c p k j -> p c (k j)")
            dsrc = lambda e, c0, n: self.I('ffn_d')[c0 * 128:(c0 + n) * 128, :].rearrange("(c p) n -> p c n", p=128)
        GC = 4
        TBK = 1024
        acc = ar.alloc(8 * DM).rearrange("p (a n) -> p a n", a=8)
        hT = [ar.alloc(GC * TBK, BF16).rearrange("p (c t) -> p c t", c=GC) for _ in range(2)]
        Wg = [ar.alloc(GC * 1024, BF16).rearrange("p (c k j) -> p c k j", c=GC, k=8) for _ in range(2)]
        Wu = [ar.alloc(GC * 1024, BF16).rearrange("p (c k j) -> p c k j", c=GC, k=8) for _ in range(2)]
        Wd = [ar.alloc(GC * DM, BF16).rearrange("p (c n) -> p c n", c=GC) for _ in range(2)]
        sgb = [ar.alloc(512) for _ in range(2)]
        gam, bet = ar.alloc(DM), ar.alloc(DM)
        xres = [ar.alloc(DM) for _ in range(2)]
        tbufs = [ar.alloc(DM) for _ in range(2)]
        xo = [ar.alloc(DM) for _ in range(2)]
        statss = [ar.alloc(8) for _ in range(2)]
        s.dma('sp', gam, self.I('ln_r')[l][2], key='c9', writes=['lnp'])
        s.dma('sp', bet, self.I('ln_r')[l][3], key='c10', writes=['lnp'])
        groups = [(c0, min(GC, nch - c0)) for c0 in range(0, nch, GC)]
        gurot = itertools.cycle([(0, 1), (2, 3)])
        drot = itertools.cycle([4, 5])
        dst = self.y_out if last else self.x2_d
        nslot = 0
        for tb in range(S_LEN // TBK if FFN_LIMIT >= 9 else 1):
            first = True
            for e in range(nexp):
                for (c0, n) in (groups if FFN_LIMIT >= 2 else groups[:1]):
                    sl = nslot % 2
                    nslot += 1
                    s.dma('pool', Wg[sl][:, 0:n].rearrange("p c k j -> p c (k j)"), gsrc(e, c0, n), key=f'fg{sl}', writes=[('Wg', sl)])
                    s.dma('pool', Wu[sl][:, 0:n].rearrange("p c k j -> p c (k j)"), usrc(e, c0, n), key=f'fu{sl}', writes=[('Wu', sl)])
                    s.dma('pool', Wd[sl][:, 0:n], dsrc(e, c0, n), key=f'fd{sl}', writes=[('Wd', sl)])
                    for c in range(n):
                        for hf in range(2):
                            ts_ = slice(tb * TBK + hf * 512, tb * TBK + (hf + 1) * 512)
                            bg, bu = next(gurot)
                            for kc in range(8):
                                self.mm(ps[bg][:, :], Wg[sl][:, c, kc, :], xT[:, kc, ts_], kc == 0, kc == 7,
                                        reads=[('Wg', sl), ('xTb', tb)], writes=[('ps', bg)])
                            for kc in range(8):
                                self.mm(ps[bu][:, :], Wu[sl][:, c, kc, :], xT[:, kc, ts_], kc == 0, kc == 7,
                                        reads=[('Wu', sl), ('xTb', tb)], writes=[('ps', bu)])
                            k2 = (c * 2 + hf) % 2
                            self.act(sgb[k2], ps[bg][:, :], AF.Silu, reads=[('ps', bg)], writes=[('sgb', k2)])
                            self.tt(hT[sl][:, c, hf * 512:(hf + 1) * 512], sgb[k2], ps[bu][:, :], ALU.mult,
                                    reads=[('sgb', k2), ('ps', bu)], writes=[('hT', sl)])
                    for sub in range(8 if FFN_LIMIT >= 1 else 0):
                        i = tb * 8 + sub
                        for hf in range(2):
                            bd = next(drot)
                            for c in range(n):
                                self.mm(ps[bd][:, :], hT[sl][:, c, sub * 128:(sub + 1) * 128], Wd[sl][:, c, hf * 512:(hf + 1) * 512],
                                        c == 0, c == n - 1, reads=[('hT', sl), ('Wd', sl)], writes=[('ps', bd)])
                            av = acc[:, sub, hf * 512:(hf + 1) * 512]
                            ak = ('acc', sub, hf)
                            if moe:
                                gcol = self.Gt[:, i, e:e + 1]
                                if first:
                                    self.ts(av, ps[bd][:, :], gcol, None, ALU.mult, reads=[('ps', bd)], writes=[ak])
                                else:
                                    self.stt(av, ps[bd][:, :], gcol, av, ALU.mult, ALU.add, reads=[('ps', bd), ak], writes=[ak])
                            else:
                                if first:
                                    self.copy('dve', av, ps[bd][:, :], reads=[('ps', bd)], writes=[ak])
                                else:
                                    self.tt(av, av, ps[bd][:, :], ALU.add, reads=[('ps', bd), ak], writes=[ak])
                    first = False
            for sub in range(8 if FFN_LIMIT >= 3 else 0):
                i = tb * 8 + sub
                xs = i % 2
                s.dma('sp', xres[xs], self.x1_d[i * 128:(i + 1) * 128, :], key=f'xr{xs}', writes=[('xres', xs)])
                self.ln_tile(xres[xs], ('xres', xs), [acc[:, sub, 0:512], acc[:, sub, 512:1024]], [('acc', sub, 0), ('acc', sub, 1)],
                             gam, bet, xo[xs], ('xo', xs), tbufs, statss, xs)
                s.dma('sp', dst[i * 128:(i + 1) * 128, :], xo[xs], key=f'xo{xs}', reads=[('xo', xs)], writes=[('dst', i)])
                if not last:
                    for hf in range(2):
                        b = 6 + hf
                        for q in range(4):
                            kc = hf * 4 + q
                            self.tr(ps[b][:, q * 128:(q + 1) * 128], xo[xs][:, kc * 128:(kc + 1) * 128], reads=[('xo', xs)], writes=[('ps', b)])
                        self.copy('act' if hf == 0 else 'dve', xT[:, hf * 4:(hf + 1) * 4, i * 128:(i + 1) * 128],
                                  ps[b][:, :].rearrange("p (k t) -> p k t", k=4), reads=[('ps', b)], writes=[('xTb', tb)])
        ar.release(m)


def _prep_shared(inp):
    f = lambda a: np.ascontiguousarray(a, dtype=np.float32)
    perm = _w_in_perm()
    w_in = inp["w_in"]
    w_in_r = np.stack([w_in[l][:, perm].reshape(8, 128, N_IN).transpose(1, 0, 2) for l in range(DEPTH)])
    cw = inp["conv_w"]
    conv_r = np.stack([cw[l].reshape(4, 3, 8, 128).transpose(3, 2, 1, 0).reshape(128, 96) for l in range(DEPTH)])
    headp = np.stack([np.broadcast_to(np.concatenate([inp["a_log"][l], inp["dt_bias"][l]])[None, :], (128, 16)) for l in range(DEPTH)])
    onw = np.stack([np.broadcast_to(inp["o_norm_w"][l][None, :], (128, 128)) for l in range(DEPTH)])
    kt = lambda w, kc: w.reshape(kc, 128, w.shape[1]).transpose(1, 0, 2)
    w_oa_r = np.stack([kt(inp["w_oa"][l], 4) for l in range(DEPTH)])
    w_ob_r = np.stack([kt(inp["w_ob"][l], 8) for l in range(DEPTH)])
    w_out_r = np.stack([kt(inp["w_out"][l], 8) for l in range(DEPTH)])
    ln_r = np.stack([np.stack([np.broadcast_to(inp[k][l][None, :], (128, DM)) for k in ("ln1_g", "ln1_b", "ln2_g", "ln2_b")]) for l in range(DEPTH)])
    ct = lambda w: w.reshape(8, 128, w.shape[1] // 128, 128).transpose(2, 1, 0, 3)
    shared = {
        "cmat": _const_mats(),
        "biasT": _bias_tables(np.asarray(inp["rel_bias"], np.float32)),
        "w_in_r": w_in_r, "conv_r": conv_r, "headp": headp, "onw": onw,
        "w_oa_r": w_oa_r, "w_ob_r": w_ob_r, "w_out_r": w_out_r, "ln_r": ln_r,
        "ffn_g_r": ct(inp["ffn_w_gate"][0]), "ffn_u_r": ct(inp["ffn_w_up"][0]), "ffn_d": inp["ffn_w_down"][0],
        "moe_router_r": inp["moe_router"][0].reshape(8, 128, NEXP).transpose(1, 0, 2),
        "moe_g_r": np.stack([ct(inp["moe_w_gate"][0][e]) for e in range(NEXP)]),
        "moe_u_r": np.stack([ct(inp["moe_w_up"][0][e]) for e in range(NEXP)]),
        "moe_d": inp["moe_w_down"][0],
    }
    return {k: f(v) for k, v in shared.items()}


def kernel(**inputs):
    inp = {k: np.asarray(v) for k, v in inputs.items()}
    shared = _prep_shared(inp)
    prog = Prog()
    x = np.ascontiguousarray(inp["x"], dtype=np.float32)
    shared = {k: v for k, v in shared.items() if k in prog.ins}
    in_maps = [dict(shared, x=x[b]) for b in range(8)]
    res = run_bass_kernel_spmd(prog.nc, in_maps, core_ids=list(range(8)))
    return np.stack([np.asarray(res.results[b]["y"], dtype=np.float32) for b in range(8)])
```
